# Optimizing a Trainium2 kernel written in Bass

```python
import jax
import jax.numpy as jnp
from jax import lax
import numpy as np

D_MODEL = 1024
BATCH = 2
SEQ = 8192
DEPTH = 2

GRID_W = 64
CTX_LEN = 256
HEAD_DIM = 64
N_GROUPS = 4
BRANCH_WIDTH = N_GROUPS * HEAD_DIM
N_BRANCH = 4
CHUNK = 128
CONV_WIDTH = 31
WIN_ROWS = 8
WIN_COLS = 16
QB_COLS = 16
KB_COLS = QB_COLS + WIN_COLS
ROPE_BASE = 10000.0
N_EXPERTS = 16
EXPERT_FF = 1024
EC_CAPACITY = 2
EPS = 1e-6
NEG_INF = -1e30
A_END = 2 * BRANCH_WIDTH
B_END = A_END + BRANCH_WIDTH
C_END = B_END + 2 * BRANCH_WIDTH
Q_END = C_END + BRANCH_WIDTH
K_END = Q_END + BRANCH_WIDTH
V_END = K_END + BRANCH_WIDTH
IN_COLS = V_END + N_BRANCH * D_MODEL
SPLITS = [A_END, B_END, C_END, Q_END, K_END, V_END]

kernel_name = 'hybrid_diffusion_trunk'


def rmsnorm(x, g):
    x32 = x.astype(jnp.float32)
    y = x32 * lax.rsqrt(jnp.mean(x32 * x32, axis=-1, keepdims=True) + EPS)
    return (y * g).astype(x.dtype)


def layernorm(x, g, b):
    x32 = x.astype(jnp.float32)
    mu = jnp.mean(x32, axis=-1, keepdims=True)
    var = jnp.mean(jnp.square(x32 - mu), axis=-1, keepdims=True)
    return ((x32 - mu) * lax.rsqrt(var + EPS) * g + b).astype(x.dtype)


def heads(z):
    return z.reshape(*z.shape[:-1], N_GROUPS, HEAD_DIM)


def chunk_gmlp(z, ln_g, ln_b, w_s, b_s):
    z = jax.nn.gelu(z)
    u, v = jnp.split(z, 2, axis=-1)
    v = layernorm(v, ln_g, ln_b)
    bsz, n, _ = v.shape
    v = v.reshape(bsz, n // CHUNK, CHUNK, N_GROUPS, HEAD_DIM)
    mixed = jnp.einsum('bcpgd,gqp->bcqgd', v, w_s) + b_s.T[None, None, :, :, None]
    return u * mixed.reshape(bsz, n, BRANCH_WIDTH)


def fourier_mix(z):
    bsz, n, _ = z.shape
    zg = z.astype(jnp.float32).reshape(bsz, n, N_GROUPS, HEAD_DIM)
    f = jnp.fft.fft2(zg, axes=(1, 3), norm='ortho').real
    return f.reshape(bsz, n, BRANCH_WIDTH).astype(z.dtype)


def conformer_conv(z, conv_w, conv_b, ln_g, ln_b):
    a, gt = jnp.split(z, 2, axis=-1)
    y = a * jax.nn.sigmoid(gt)
    y = lax.conv_general_dilated(
        y, conv_w[:, None, :], window_strides=(1,),
        padding=[(CONV_WIDTH // 2, CONV_WIDTH // 2)],
        dimension_numbers=('NWC', 'WIO', 'NWC'),
        feature_group_count=BRANCH_WIDTH) + conv_b
    return jax.nn.silu(layernorm(y, ln_g, ln_b))


def axial_rope(x, rows, cols):
    half = HEAD_DIM // 2
    quarter = half // 2
    inv = ROPE_BASE ** (-jnp.arange(quarter, dtype=jnp.float32) / quarter)

    def rot(xp, pos):
        ang = pos.astype(jnp.float32)[:, None] * inv[None, :]
        cos = jnp.cos(ang)[None, :, None, :]
        sin = jnp.sin(ang)[None, :, None, :]
        x1, x2 = jnp.split(xp.astype(jnp.float32), 2, axis=-1)
        return jnp.concatenate([x1 * cos - x2 * sin, x1 * sin + x2 * cos], axis=-1)

    out = jnp.concatenate([rot(x[..., :half], rows), rot(x[..., half:], cols)], axis=-1)
    return out.astype(x.dtype)


def neighbourhood_attention(q, k, v, kc, vc, rpb):
    bsz, n, nh, dh = q.shape
    n_rows = n // GRID_W
    wr = min(WIN_ROWS, n_rows)
    n_cb = GRID_W // QB_COLS
    n_win = wr * KB_COLS
    t = jnp.arange(n)
    qs = q * (dh ** -0.5)
    q_rot = axial_rope(qs, t // GRID_W, t % GRID_W)
    k_rot = axial_rope(k, t // GRID_W, t % GRID_W)
    r = jnp.arange(n_rows)
    key_rows = jnp.clip(r - wr // 2, 0, n_rows - wr)[:, None] + jnp.arange(wr)[None, :]
    jb = jnp.arange(n_cb)
    key_cols = (jnp.clip(jb * QB_COLS - WIN_COLS // 2, 0, GRID_W - KB_COLS)[:, None]
                + jnp.arange(KB_COLS)[None, :])
    q_cols = jb[:, None] * QB_COLS + jnp.arange(QB_COLS)[None, :]
    c_start = jnp.clip(q_cols - WIN_COLS // 2, 0, GRID_W - WIN_COLS)
    kcol = key_cols[:, None, :]
    col_ok = (kcol >= c_start[..., None]) & (kcol < c_start[..., None] + WIN_COLS)
    d_row = key_rows - r[:, None] + (WIN_ROWS - 1)
    d_col = jnp.clip(kcol - q_cols[..., None], 1 - WIN_COLS, WIN_COLS - 1) + (WIN_COLS - 1)
    bias = rpb[:, d_row[:, None, None, :, None], d_col[None, :, :, None, :]].astype(jnp.float32)
    bias = jnp.where(col_ok[None, None, :, :, None, :], bias, NEG_INF)
    bias = bias.reshape(nh, n_rows, n_cb, QB_COLS, n_win)
    key_idx = (key_rows[:, None, :, None] * GRID_W + key_cols[None, :, None, :]).reshape(n_rows, n_cb, n_win)
    k_win = k_rot[:, key_idx]
    v_win = v[:, key_idx]
    q_rot = q_rot.reshape(bsz, n_rows, n_cb, QB_COLS, nh, dh)
    q_pl = qs.reshape(bsz, n_rows, n_cb, QB_COLS, nh, dh)
    s_win = jnp.einsum('brjqhd,brjkhd->bhrjqk', q_rot, k_win).astype(jnp.float32) + bias
    s_ctx = jnp.einsum('brjqhd,bchd->bhrjqc', q_pl, kc).astype(jnp.float32)
    p = jax.nn.softmax(jnp.concatenate([s_win, s_ctx], axis=-1), axis=-1).astype(v.dtype)
    out = (jnp.einsum('bhrjqk,brjkhd->brjqhd', p[..., :n_win], v_win)
           + jnp.einsum('bhrjqc,bchd->brjqhd', p[..., n_win:], vc))
    return out.reshape(bsz, n, nh * dh)


def context_attention(q, k, v):
    bsz, n, nh, dh = q.shape
    s = jnp.einsum('bqhd,bkhd->bhqk', q * (dh ** -0.5), k).astype(jnp.float32)
    p = jax.nn.softmax(s, axis=-1).astype(v.dtype)
    return jnp.einsum('bhqk,bkhd->bqhd', p, v).reshape(bsz, n, nh * dh)


def merge_branches(a, b, cc, d, zg, w_a, w_b, w_c, w_d, w_o):
    ga, gb, gc, gd = jnp.split(jax.nn.sigmoid(zg), N_BRANCH, axis=-1)
    m = ga * (a @ w_a) + gb * (b @ w_b) + gc * (cc @ w_c) + gd * (d @ w_d)
    return m @ w_o


def expert_choice_ffn(h, router, w_gate, w_up, w_down):
    bsz, n, d = h.shape
    cap = EC_CAPACITY * n // N_EXPERTS
    aff = jax.nn.softmax((h @ router).astype(jnp.float32), axis=-1)
    gate, idx = lax.top_k(jnp.swapaxes(aff, 1, 2), cap)
    xe = jax.vmap(lambda hb, ib: hb[ib])(h, idx)
    a = jnp.einsum('becd,edf->becf', xe, w_gate)
    u = jnp.einsum('becd,edf->becf', xe, w_up)
    y = jnp.einsum('becf,efd->becd', jax.nn.silu(a) * u, w_down) * gate[..., None].astype(h.dtype)
    return jax.vmap(lambda ib, yb: jnp.zeros((n, d), h.dtype).at[ib.reshape(-1)].add(yb.reshape(-1, d)))(idx, y)


def setup_inputs(seed: int = 0) -> dict:
    key = jax.random.key(seed)
    ks = jax.random.split(key, 28)
    L, D, W = DEPTH, D_MODEL, BRANCH_WIDTH

    def nrm(k, shape, s):
        return jax.random.normal(k, shape, jnp.float32) * s

    return {
        'x': nrm(ks[0], (BATCH, SEQ, D), 1.0),
        'c': nrm(ks[1], (BATCH, D), 1.0),
        'ctx': nrm(ks[2], (BATCH, CTX_LEN, D), 1.0),
        'c_ctx': nrm(ks[3], (D,), 1.0),
        'w_mod': nrm(ks[4], (L, D, 6 * D), 0.5 * D ** -0.5),
        'b_mod': nrm(ks[5], (L, 6 * D), 0.02),
        'norm1_g': 1.0 + nrm(ks[6], (L, D), 0.02),
        'norm2_g': 1.0 + nrm(ks[7], (L, D), 0.02),
        'w_in': nrm(ks[8], (L, D, IN_COLS), D ** -0.5),
        'sgu_ln_g': 1.0 + nrm(ks[9], (L, W), 0.02),
        'sgu_ln_b': nrm(ks[10], (L, W), 0.02),
        'w_spatial': nrm(ks[11], (L, N_GROUPS, CHUNK, CHUNK), CHUNK ** -0.5),
        'b_spatial': 1.0 + nrm(ks[12], (L, N_GROUPS, CHUNK), 0.02),
        'w_a_out': nrm(ks[13], (L, W, D), W ** -0.5),
        'w_b_out': nrm(ks[14], (L, W, D), W ** -0.5),
        'conv_w': nrm(ks[15], (L, CONV_WIDTH, W), CONV_WIDTH ** -0.5),
        'conv_b': nrm(ks[16], (L, W), 0.02),
        'conv_ln_g': 1.0 + nrm(ks[17], (L, W), 0.02),
        'conv_ln_b': nrm(ks[18], (L, W), 0.02),
        'w_c_out': nrm(ks[19], (L, W, D), W ** -0.5),
        'rpb': nrm(ks[20], (L, N_GROUPS, 2 * WIN_ROWS - 1, 2 * WIN_COLS - 1), 0.1),
        'w_d_out': nrm(ks[21], (L, W, D), W ** -0.5),
        'w_out': nrm(ks[22], (L, D, D), D ** -0.5),
        'w_router': nrm(ks[23], (L, D, N_EXPERTS), D ** -0.5),
        'w_gate_e': nrm(ks[24], (L, N_EXPERTS, D, EXPERT_FF), D ** -0.5),
        'w_up_e': nrm(ks[25], (L, N_EXPERTS, D, EXPERT_FF), D ** -0.5),
        'w_down_e': nrm(ks[26], (L, N_EXPERTS, EXPERT_FF, D), EXPERT_FF ** -0.5),
        'final_norm_g': 1.0 + nrm(ks[27], (D,), 0.02),
    }


def reference(x, c, ctx, c_ctx, w_mod, b_mod, norm1_g, norm2_g, w_in, sgu_ln_g, sgu_ln_b,
              w_spatial, b_spatial, w_a_out, w_b_out, conv_w, conv_b, conv_ln_g, conv_ln_b,
              w_c_out, rpb, w_d_out, w_out, w_router, w_gate_e, w_up_e, w_down_e, final_norm_g):
    xc = ctx
    silu_c = jax.nn.silu(c)
    silu_cc = jax.nn.silu(c_ctx)
    for l in range(DEPTH):
        last = l == DEPTH - 1
        mod = (silu_c @ w_mod[l] + b_mod[l])[:, None, :]
        sh1, sc1, gt1, sh2, sc2, gt2 = jnp.split(mod, 6, axis=-1)
        cmod = silu_cc @ w_mod[l] + b_mod[l]
        csh1, csc1, cgt1, csh2, csc2, cgt2 = jnp.split(cmod, 6, axis=-1)

        h = rmsnorm(x, norm1_g[l]) * (1 + sc1) + sh1
        hc = rmsnorm(xc, norm1_g[l]) * (1 + csc1) + csh1
        za, zb, zcv, zq, zk, zv, zg = jnp.split(h @ w_in[l], SPLITS, axis=-1)
        if last:
            zkc, zvc = jnp.split(hc @ w_in[l][:, Q_END:V_END], 2, axis=-1)
        else:
            cza, czb, czcv, czq, zkc, zvc, czg = jnp.split(hc @ w_in[l], SPLITS, axis=-1)
        kc, vc = heads(zkc), heads(zvc)
        d_lat = neighbourhood_attention(heads(zq), heads(zk), heads(zv), kc, vc, rpb[l])
        mix = merge_branches(
            chunk_gmlp(za, sgu_ln_g[l], sgu_ln_b[l], w_spatial[l], b_spatial[l]),
            fourier_mix(zb),
            conformer_conv(zcv, conv_w[l], conv_b[l], conv_ln_g[l], conv_ln_b[l]),
            d_lat, zg, w_a_out[l], w_b_out[l], w_c_out[l], w_d_out[l], w_out[l])
        x = x + gt1 * mix
        if not last:
            cmix = merge_branches(
                chunk_gmlp(cza, sgu_ln_g[l], sgu_ln_b[l], w_spatial[l], b_spatial[l]),
                fourier_mix(czb),
                conformer_conv(czcv, conv_w[l], conv_b[l], conv_ln_g[l], conv_ln_b[l]),
                context_attention(heads(czq), kc, vc),
                czg, w_a_out[l], w_b_out[l], w_c_out[l], w_d_out[l], w_out[l])
            xc = xc + cgt1 * cmix

        h2 = rmsnorm(x, norm2_g[l]) * (1 + sc2) + sh2
        x = x + gt2 * expert_choice_ffn(h2, w_router[l], w_gate_e[l], w_up_e[l], w_down_e[l])
        if not last:
            hc2 = rmsnorm(xc, norm2_g[l]) * (1 + csc2) + csh2
            xc = xc + cgt2 * expert_choice_ffn(hc2, w_router[l], w_gate_e[l], w_up_e[l], w_down_e[l])
    return rmsnorm(x, final_norm_g)
```

```python
import numpy as np
import ml_dtypes
from contextlib import ExitStack
import concourse.bass as bass
import concourse.mybir as mybir
from concourse.bass_utils import run_bass_kernel_spmd

F32 = mybir.dt.float32
BF16 = mybir.dt.bfloat16
I32 = mybir.dt.int32
U32 = mybir.dt.uint32
AF = mybir.ActivationFunctionType
ALU = mybir.AluOpType
AX = mybir.AxisListType

N_DMA_SEMS = 40
N_SW_SEMS = 8


class T:
    def __init__(self, ap, name=""):
        self.ap = ap
        self.name = name
        self.w = []
        self.wb = []
        self.r = []

    def __getitem__(self, idx):
        return self.ap[idx]


class K:
    ENGS = ["pe", "act", "dve", "pool", "sp"]

    def __init__(self, nc, stack):
        self.nc = nc
        self.stack = stack
        self.streams = {e: [] for e in self.ENGS}
        self.sems = {}
        for e in self.ENGS:
            self.sems[e] = stack.enter_context(nc.semaphore("c_" + e))
        self.count = {e: 0 for e in self.ENGS}
        self.dma_sems = [stack.enter_context(nc.semaphore("d%d" % i)) for i in range(N_DMA_SEMS)]
        self.dma_val = [0] * N_DMA_SEMS
        self.dma_rr = 0
        self.dma_rr_sw = 0
        self.known = {e: {} for e in self.ENGS}
        self.same_engine_sync = True
        self.n_inst = 0

    def sbuf(self, name, shape, dtype):
        self.n_alloc = getattr(self, "n_alloc", 0) + 1
        name = "%s_%d" % (name, self.n_alloc)
        t = self.stack.enter_context(self.nc.sbuf_tensor(name, list(shape), dtype))
        return T(t, name)

    def psum(self, name, shape, dtype):
        self.n_alloc = getattr(self, "n_alloc", 0) + 1
        name = "%s_%d" % (name, self.n_alloc)
        t = self.stack.enter_context(self.nc.psum_tensor(name, list(shape), dtype))
        return T(t, name)

    def dram(self, name, shape, dtype, kind="Internal"):
        t = self.nc.dram_tensor(name, list(shape), dtype, kind=kind)
        return T(t, name)

    def _sem_of(self, key):
        return self.sems[key] if isinstance(key, str) else self.dma_sems[key]

    def _wait(self, eng, dep):
        key, val = dep
        if key == eng and (not self.same_engine_sync or eng in ("pe", "sp")):
            return
        kn = self.known[eng]
        if kn.get(key, 0) >= val:
            return
        kn[key] = val
        self.streams[eng].append(("wait", key, val))

    def _deps(self, eng, reads, writes, acc_w=False):
        for t in reads:
            for d in t.w:
                self._wait(eng, d)
        for t in writes:
            for d in (t.wb if acc_w else t.w):
                self._wait(eng, d)
            for d in t.r:
                self._wait(eng, d)

    def _mark(self, dep, reads, writes, acc_w=False):
        for t in reads:
            t.r.append(dep)
            if len(t.r) > 64:
                t.r = self._compress(t.r)
        for t in writes:
            if acc_w:
                t.w.append(dep)
                if len(t.w) > 64:
                    t.w = self._compress(t.w)
            else:
                t.w = [dep]
                t.wb = [dep]
            t.r = []

    @staticmethod
    def _compress(deps):
        m = {}
        for k, v in deps:
            if m.get(k, 0) < v:
                m[k] = v
        return list(m.items())

    def op(self, eng, fn, reads=(), writes=(), signal=True):
        self._deps(eng, reads, writes)
        if signal:
            self.count[eng] += 1
            dep = (eng, self.count[eng])
            self.streams[eng].append(("inst", fn, eng, 1))
        else:
            dep = (eng, self.count[eng] + 1)
            self.streams[eng].append(("inst", fn, None, 0))
        self._mark(dep, reads, writes)
        self.n_inst += 1

    def dma(self, q, out, in_, reads=(), writes=(), acc_w=False, **kw):
        self._deps(q, reads, writes, acc_w=acc_w)
        self._throttle(q)
        s = self._next_sem(q)
        if self.dma_val[s] > 0:
            self._wait(q, (s, self.dma_val[s]))
        self.dma_val[s] += 16
        dep = (s, self.dma_val[s])

        def fn(e, out=out, in_=in_, kw=kw):
            return e.dma_start(out=out, in_=in_, **kw)
        self.streams[q].append(("inst", fn, s, 16))
        self._mark(dep, reads, writes, acc_w=acc_w)
        self.n_inst += 1
        if q == "pool":
            self.__dict__.setdefault("pool_out", []).append(dep)
        return dep

    def _next_sem(self, q):
        if q == "pool":
            s = self.dma_rr_sw
            self.dma_rr_sw = (self.dma_rr_sw + 1) % N_SW_SEMS
            return s
        s = N_SW_SEMS + self.dma_rr
        self.dma_rr = (self.dma_rr + 1) % (N_DMA_SEMS - N_SW_SEMS)
        return s

    def _throttle(self, q):
        if q != "pool":
            return
        po = self.__dict__.setdefault("pool_out", [])
        if len(po) >= 2:
            self._wait(q, po[-2])

    def dma_raw(self, q, fn, reads=(), writes=(), acc_w=False):
        self._deps(q, reads, writes, acc_w=acc_w)
        self._throttle(q)
        s = self._next_sem(q)
        if self.dma_val[s] > 0:
            self._wait(q, (s, self.dma_val[s]))
        self.dma_val[s] += 16
        dep = (s, self.dma_val[s])
        self.streams[q].append(("inst", fn, s, 16))
        self._mark(dep, reads, writes, acc_w=acc_w)
        if q == "pool":
            self.__dict__.setdefault("pool_out", []).append(dep)
        return dep

    def finish(self, out_tiles):
        for t in out_tiles:
            for d in t.w:
                self._wait("sp", d)
        for e in self.ENGS:
            if e != "sp" and self.count[e] > 0:
                self._wait("sp", (e, self.count[e]))
        for s in range(N_DMA_SEMS):
            if self.dma_val[s] > 0:
                self._wait("sp", (s, self.dma_val[s]))

    def emit(self):
        nc = self.nc
        engmap = {"pe": "tensor", "act": "scalar", "dve": "vector", "pool": "gpsimd", "sp": "sync"}
        with nc.Block() as block:
            for e in self.ENGS:
                stream = self.streams[e]
                if not stream:
                    continue

                def body(eng, stream=stream):
                    for item in stream:
                        if item[0] == "wait":
                            eng.wait_ge(self._sem_of(item[1]), item[2])
                        else:
                            _, fn, key, inc = item
                            ins = fn(eng)
                            if key is not None:
                                ins.then_inc(self._sem_of(key), inc)
                getattr(block, engmap[e])(body)


class Pool:
    def __init__(self, k, name, n, shape, dtype, space="sbuf"):
        mk = k.sbuf if space == "sbuf" else k.psum
        self.tiles = [mk("%s%d" % (name, i), shape, dtype) for i in range(n)]
        self.i = 0

    def next(self):
        t = self.tiles[self.i]
        self.i = (self.i + 1) % len(self.tiles)
        return t


def _k_phase(self):
    class _Ph:
        def __init__(s, k):
            s.k = k
        def __enter__(s):
            s.prev = s.k.stack
            s.st = ExitStack()
            s.st.__enter__()
            s.k.stack = s.st
            return s
        def __exit__(s, *a):
            s.k.barrier()
            s.k.stack = s.prev
            return s.st.__exit__(*a)
    return _Ph(self)


def _k_barrier(self):
    for e in self.ENGS:
        for e2 in self.ENGS:
            if e2 != e and self.count[e2] > 0:
                self._wait(e, (e2, self.count[e2]))
        for s in range(N_DMA_SEMS):
            if self.dma_val[s] > 0:
                self._wait(e, (s, self.dma_val[s]))


K.phase = _k_phase
K.barrier = _k_barrier


D = 1024
W = 256
NCTX = 256
NE = 16
GW = 64
bf16_np = ml_dtypes.bfloat16
NEG = -30000.0


def _rope_tables(n):
    t = np.arange(n)
    rows, cols = t // GW, t % GW
    inv = 10000.0 ** (-np.arange(16, dtype=np.float64) / 16)
    d = np.arange(128) % 64
    pos = np.where(d[:, None] < 32, rows[None, :], cols[None, :]).astype(np.float64)
    ang = pos * inv[d % 16][:, None]
    cos, sin = np.cos(ang), np.sin(ang)
    sgn = np.where((d % 32) < 16, -1.0, 1.0)[:, None]
    return np.stack([cos / 8, sin * sgn / 8, cos, sin * sgn]).astype(np.float32)


def _attn_geometry(n):
    R = n // GW
    wr = min(8, R)
    s = lambda r: int(np.clip(r - wr // 2, 0, R - wr))
    cst = np.clip(np.arange(GW) - 8, 0, GW - 16)
    uniq = {}
    ulist = []
    qtiles = []
    kc = np.tile(np.arange(GW), 2)
    ka = np.repeat(np.arange(2), GW)
    for qt in range(R // 2):
        r0 = 2 * qt
        lo, hi = s(r0), s(r0 + 1) + wr - 1
        lst = []
        for kt in range(lo // 2, hi // 2 + 1):
            kr = 2 * kt + ka
            qr = r0 + ka
            qc = kc
            srow = np.array([s(r) for r in qr])
            ok_r = (kr[:, None] >= srow[None, :]) & (kr[:, None] < srow[None, :] + wr)
            ok_c = (kc[:, None] >= cst[qc][None, :]) & (kc[:, None] < cst[qc][None, :] + 16)
            mask = ok_r & ok_c
            if not mask.any():
                continue
            dr = np.clip(kr[:, None] - qr[None, :] + 7, 0, 14)
            dc = np.clip(kc[:, None] - qc[None, :], -15, 15) + 15
            key = (2 * kt - r0, mask.tobytes())
            if key not in uniq:
                uniq[key] = len(ulist)
                ulist.append((dr, dc, mask))
            lst.append((kt, uniq[key]))
        qtiles.append(lst)
    return ulist, qtiles


def _bias_tiles(rpb_l, ulist):
    out = np.empty((4, len(ulist), 128, 128), np.float32)
    for u, (dr, dc, mask) in enumerate(ulist):
        for h in range(4):
            out[h, u] = np.where(mask, rpb_l[h][dr, dc], NEG)
    return np.ascontiguousarray(out.transpose(2, 0, 1, 3).reshape(128, 4 * len(ulist), 128)).astype(bf16_np)


def _dft_consts(n):
    N1 = n // 128
    sc = 1.0 / 8.0
    dd = np.arange(64)
    ph = 2 * np.pi * np.outer(dd, dd) / 64
    Cd, Sd = np.cos(ph) * sc, np.sin(ph) * sc
    cs = np.zeros((128, 256))
    for g in range(2):
        cs[g * 64:(g + 1) * 64, g * 64:(g + 1) * 64] = Cd
        cs[g * 64:(g + 1) * 64, 128 + g * 64:128 + (g + 1) * 64] = -Sd
    c1 = np.arange(N1)
    p1 = 2 * np.pi * np.outer(c1, c1) / N1
    f1 = np.stack([np.cos(p1), np.sin(p1), -np.sin(p1)]) / np.sqrt(float(n))
    pp = np.arange(128)
    p2 = 2 * np.pi * np.outer(pp, pp) / 128
    f2 = np.stack([np.cos(p2), np.sin(p2)])
    pt = 2 * np.pi * np.outer(pp, c1) / n
    tw = np.stack([np.cos(pt), -np.sin(pt)]).astype(np.float32)
    return cs.astype(bf16_np), f1.astype(bf16_np), f2.astype(bf16_np), tw


def _route_consts():
    p = np.arange(128)
    same = (p[:, None] // 8) == (p[None, :] // 8)
    blk = same.astype(np.float32)
    low = (same & (p[:, None] < p[None, :])).astype(np.float32)
    iota = np.broadcast_to(np.arange(32, dtype=np.float32), (128, NE, 32)).copy()
    return blk, low, iota


A_END, B_END, C_END, Q_END, K_END, V_END = 512, 768, 1280, 1536, 1792, 2048
IN_COLS = 6144
GELU_C = 1.5957691216057308


class Stream:
    pass


def build_program(n, L=2, dbg=(), dbg_phases=None):
    nc = bass.Bass("TRN2", target_bir_lowering=False)
    N1 = n // 128
    cap = 2 * n // NE
    capc = 2 * NCTX // NE
    st = ExitStack()
    k = K(nc, st)
    ins = {}

    def ext(name, shape, dtype=F32):
        h = nc.dram_tensor(name, list(shape), dtype, kind="ExternalInput")
        ins[name] = T(h, name)
        return ins[name]

    def scr(name, shape, dtype):
        kind = "ExternalOutput" if name in dbg else "Internal"
        h = nc.dram_tensor(name, list(shape), dtype, kind=kind)
        return T(h, name)

    x_in = ext("x", [n, D]); ctx_in = ext("ctx", [NCTX, D])
    c_in = ext("c_pk", [128, 8]); cc_in = ext("cctx_pk", [128, 8])
    w_mod = ext("w_mod", [L, D, 6 * D]); b_mod = ext("b_mod", [L, 6 * D])
    n1g = ext("norm1_g", [L, D]); n2g = ext("norm2_g", [L, D]); fng = ext("final_norm_g", [1, D])
    w_in = ext("w_in", [L, D, IN_COLS]); w_qkp = ext("w_qkp", [L, D, 512])
    sgu_g = ext("sgu_ln_g", [L, W]); sgu_b = ext("sgu_ln_b", [L, W])
    wsT = ext("wsT", [L, 128, 4, 128]); bsp = ext("b_spatial", [L, 4, 128])
    w_br = [ext(nm, [L, W, D]) for nm in ("w_a_out", "w_b_out", "w_c_out", "w_d_out")]
    conv_wT = ext("conv_wT", [L, W, 31]); conv_b = ext("conv_b_c", [L, W, 1])
    cln_g = ext("conv_ln_g_c", [L, W, 1]); cln_b = ext("conv_ln_b_c", [L, W, 1])
    w_out = ext("w_out", [L, D, D]); w_router = ext("w_router", [L, D, NE])
    w_gate = ext("w_gate_e", [L, NE, D, D]); w_up = ext("w_up_e", [L, NE, D, D]); w_down = ext("w_down_e", [L, NE, D, D])
    id_bf = ext("id_bf", [128, 128], BF16); id_f = ext("id_f", [128, 128])
    rope = ext("rope", [4, 128, n])
    ulist, qtiles = _attn_geometry(n)
    U = len(ulist)
    biasT = ext("biasT", [L, 128, 4 * U, 128], BF16)
    cs_c = ext("dft_cs", [128, 256], BF16)
    f1x = ext("dft_f1x", [3, N1, N1], BF16); f1c = ext("dft_f1c", [3, 2, 2], BF16)
    f2 = ext("dft_f2", [2, 128, 128], BF16)
    twx = ext("dft_twx", [2, 128, N1]); twc = ext("dft_twc", [2, 128, 2])
    r_blk = ext("r_blk", [128, 128]); r_low = ext("r_low", [128, 128]); r_iota = ext("r_iota", [128, NE, 32])
    tidx = ext("tid_x", [128, N1]); tidc = ext("tid_c", [128, 2])
    out_h = nc.dram_tensor("out", [n, D], F32, kind="ExternalOutput")
    OUT = T(out_h, "out")

    def mk_stream(si, nn, name, xin):
        S = Stream()
        S.si, S.n, S.name, S.N1 = si, nn, name, nn // 128
        S.cap = 2 * nn // NE
        S.Xin = xin
        S.XA = scr(name + "_XA", [nn, D], F32); S.XB = scr(name + "_XB", [nn, D], F32)
        S.hT = scr(name + "_hT", [D, nn], BF16)
        for nm in ("uT", "yT", "qrT", "qpT", "krT", "aT", "bT", "cT", "dT"):
            setattr(S, nm, scr(name + "_" + nm, [W, nn], BF16))
        for nm in ("vln", "Zr", "Zi", "v"):
            setattr(S, nm, scr(name + "_" + nm, [nn, W], BF16))
        S.gT = scr(name + "_gT", [4 * D, nn], BF16)
        S.Y1r = scr(name + "_Y1r", [S.N1, 128 * W], BF16); S.Y1i = scr(name + "_Y1i", [S.N1, 128 * W], BF16)
        S.h2D = scr(name + "_h2D", [nn, D], BF16)
        S.affD = scr(name + "_affD", [NE, nn], F32)
        S.slotD = scr(name + "_slotD", [NE, nn], F32)
        S.idxD = scr(name + "_idxD", [NE, S.cap], I32)
        S.gateD = scr(name + "_gateD", [NE, S.cap], F32)
        S.f1 = f1x if si == 0 else f1c
        S.tw = twx if si == 0 else twc
        S.tid = tidx if si == 0 else tidc
        return S

    SX = mk_stream(0, n, "sx", x_in)
    SC = mk_stream(1, NCTX, "sc", ctx_in)
    modD = [scr("modD%d" % l, [2, 6 * D], F32) for l in range(L)]

    def mmul(out, lhsT, rhs, start, stop, reads, writes):
        k.op("pe", lambda e: e.matmul(out, lhsT=lhsT, rhs=rhs, start=start, stop=stop), reads=reads, writes=writes, signal=bool(stop))

    def load_ident(bf=True, f=False):
        r = []
        if bf:
            t = k.sbuf("identb", [128, 128], BF16); k.dma("sp", t[:], id_bf.ap.ap(), reads=[id_bf], writes=[t]); r.append(t)
        if f:
            t = k.sbuf("identf", [128, 128], F32); k.dma("sp", t[:], id_f.ap.ap(), reads=[id_f], writes=[t]); r.append(t)
        return r

    def mod_phase(l):
        with k.phase():
            cs = k.sbuf("mod_cs", [128, 2, 8], F32)
            k.dma("sp", cs[:, 0, :], c_in.ap.ap(), reads=[c_in], writes=[cs])
            k.dma("sp", cs[:, 1, :], cc_in.ap.ap(), reads=[cc_in], writes=[cs])
            scs = k.sbuf("mod_scs", [128, 2, 8], F32)
            k.op("act", lambda e: e.activation(out=scs[:], in_=cs[:], func=AF.Silu), reads=[cs], writes=[scs])
            wp = Pool(k, "mod_w", 4, [128, 8, 512], F32)
            pp = Pool(k, "mod_ps", 2, [2, 512], F32, "psum")
            bp = Pool(k, "mod_b", 4, [2, 512], F32)
            rp = Pool(k, "mod_r", 4, [2, 512], F32)
            ldq = {}
            def mod_loads(blk):
                wm = wp.next(); bm = bp.next()
                cols = slice(blk * 512, (blk + 1) * 512)
                k.dma("sp", wm[:], w_mod.ap[l, :, cols].rearrange("(c p) n -> p c n", p=128), reads=[w_mod], writes=[wm])
                k.dma("sp", bm[:], b_mod.ap[l:l + 1, cols].to_broadcast([2, 512]), reads=[b_mod], writes=[bm])
                ldq[blk] = (wm, bm)
            for b0 in range(3):
                mod_loads(b0)
            for blk in range(12):
                if blk + 3 < 12:
                    mod_loads(blk + 3)
                wm, bm = ldq.pop(blk)
                ps = pp.next(); rs = rp.next()
                cols = slice(blk * 512, (blk + 1) * 512)
                for c in range(8):
                    mmul(ps[:], scs[:, :, c], wm[:, c, :], c == 0, c == 7, [scs, wm], [ps])
                k.op("dve", lambda e, rs=rs, ps=ps, bm=bm: e.tensor_tensor(out=rs[:], in0=ps[:], in1=bm[:], op=ALU.add), reads=[ps, bm], writes=[rs])
                k.dma("sp", modD[l].ap[:, cols], rs[:], reads=[rs], writes=[modD[l]], acc_w=True)

    def bc_load(dst, src_t, row_ap, F):
        k.dma("sp", dst[:], row_ap.to_broadcast([128, F]), reads=[src_t], writes=[dst])

    def norm_phase(S, l, kind):
        nn, si = S.n, S.si
        with k.phase():
            scale_b = k.sbuf("nm_scale", [128, D], F32)
            if kind == 3:
                bc_load(scale_b, fng, fng.ap[0:1, :], D)
            else:
                g = n1g if kind == 1 else n2g
                o = 0 if kind == 1 else 3
                gb = k.sbuf("nm_g", [128, D], F32); scb = k.sbuf("nm_sc", [128, D], F32)
                shift_b = k.sbuf("nm_shift", [128, D], F32)
                bc_load(gb, g, g.ap[l:l + 1, :], D)
                bc_load(scb, modD[l], modD[l].ap[si:si + 1, (o + 1) * D:(o + 2) * D], D)
                bc_load(shift_b, modD[l], modD[l].ap[si:si + 1, o * D:(o + 1) * D], D)
                k.op("dve", lambda e: e.scalar_tensor_tensor(out=scale_b[:], in0=scb[:], scalar=1.0, in1=gb[:], op0=ALU.add, op1=ALU.mult), reads=[scb, gb], writes=[scale_b])
            if kind == 1:
                identb, = load_ident(True, False)
            if kind == 2:
                identf, = load_ident(False, True)
                rt = k.sbuf("nm_router", [128, 8, NE], F32)
                k.dma("sp", rt[:], w_router.ap[l].rearrange("(c p) e -> p c e", p=128), reads=[w_router], writes=[rt])
                ones16 = k.sbuf("nm_ones16", [NE, NE], F32)
                k.op("dve", lambda e: e.memset(ones16[:], 1.0), writes=[ones16])
                aff_all = k.sbuf("nm_aff", [NE, nn], F32)
            xp = Pool(k, "nm_x", 5, [128, D], F32)
            jp = Pool(k, "nm_junk", 2, [128, D], BF16)
            sp_ = Pool(k, "nm_st", 3, [128, 4], F32)
            tp = Pool(k, "nm_tmp", 3, [128, D], F32)
            hp = Pool(k, "nm_h", 3, [128, D], BF16 if kind == 1 else F32)
            if kind == 1:
                psT = Pool(k, "nm_psT", 2, [128, 8, 128], BF16, "psum")
                hTp = Pool(k, "nm_hT", 2, [128, 8, 128], BF16)
            if kind == 2:
                hbp = Pool(k, "nm_hb", 2, [128, D], BF16)
                psT = Pool(k, "nm_psT", 2, [128, 4, 128], F32, "psum")
                hTp = Pool(k, "nm_hT", 2, [128, 8, 128], F32)
                psl = Pool(k, "nm_psl", 2, [NE, 512], F32, "psum")
                rstate = {}
                e_p = Pool(k, "nm_e", 2, [NE, 512], F32)
                r_p = Pool(k, "nm_r", 2, [NE, 512], F32)
            xq = {}
            def stage0(i):
                    xt = xp.next()
                    k.dma("sp", xt[:], S.Xin.ap[i * 128:(i + 1) * 128, :], reads=[S.Xin], writes=[xt])
                    xq[i] = xt
            def stage1(i):
                    rows = slice(i * 128, (i + 1) * 128)
                    xt = xq.pop(i); jk = jp.next(); s4 = sp_.next(); tmp = tp.next(); h = hp.next()
                    k.op("act", lambda e, xt=xt, jk=jk, s4=s4: e.activation(out=jk[:], in_=xt[:], func=AF.Square, accum_out=s4[:, 0:1]), reads=[xt], writes=[jk, s4])
                    xs = tmp
                    if kind != 3:
                        k.op("pool", lambda e, xt=xt, xs=xs: e.tensor_tensor(out=xs[:], in0=xt[:], in1=scale_b[:], op=ALU.mult), reads=[xt, scale_b], writes=[xs])
                    k.op("act", lambda e, s4=s4: e.activation(out=s4[:, 1:2], in_=s4[:, 0:1], func=AF.Sqrt, bias=1e-6, scale=1.0 / D), reads=[s4], writes=[s4])
                    k.op("dve", lambda e, s4=s4: e.reciprocal(out=s4[:, 2:3], in_=s4[:, 1:2]), reads=[s4], writes=[s4])
                    if kind == 3:
                        k.op("dve", lambda e, xt=xt, s4=s4, h=h: e.scalar_tensor_tensor(out=h[:], in0=xt[:], scalar=s4[:, 2:3], in1=scale_b[:], op0=ALU.mult, op1=ALU.mult), reads=[xt, s4, scale_b], writes=[h])
                        k.dma("sp", OUT.ap[rows, :], h[:], reads=[h], writes=[OUT], acc_w=True)
                        return None
                    k.op("dve", lambda e, tmp=tmp, xs=xs, s4=s4, h=h: e.scalar_tensor_tensor(out=h[:], in0=xs[:], scalar=s4[:, 2:3], in1=shift_b[:], op0=ALU.mult, op1=ALU.add), reads=[xs, s4, shift_b], writes=[h])
                    return (rows, h)
            def stage2(ctx_):
                    rows, h = ctx_
                    if kind == 1:
                        pt = psT.next(); hT = hTp.next()
                        for c in range(8):
                            k.op("pe", lambda e, c=c, pt=pt, h=h: e.transpose(out=pt[:, c, :], in_=h[:, c * 128:(c + 1) * 128], identity=identb[:]), reads=[h, identb], writes=[pt], signal=(c == 7))
                        k.op("act", lambda e, pt=pt, hT=hT: e.activation(out=hT[:], in_=pt[:], func=AF.Copy), reads=[pt], writes=[hT])
                        k.dma("sp", S.hT.ap.ap().rearrange("(c p) t -> p c t", p=128)[:, :, rows], hT[:], reads=[hT], writes=[S.hT], acc_w=True)
                    else:
                        hb = hbp.next(); hT = hTp.next()
                        k.op("act", lambda e, hb=hb, h=h: e.activation(out=hb[:], in_=h[:], func=AF.Copy), reads=[h], writes=[hb])
                        k.dma("sp", S.h2D.ap[rows, :], hb[:], reads=[hb], writes=[S.h2D], acc_w=True)
                        for half in range(2):
                            pt = psT.next()
                            for c in range(4):
                                cc = half * 4 + c
                                k.op("pe", lambda e, c=c, cc=cc, pt=pt, h=h: e.transpose(out=pt[:, c, :], in_=h[:, cc * 128:(cc + 1) * 128], identity=identf[:]), reads=[h, identf], writes=[pt], signal=(c == 3))
                            k.op("dve", lambda e, pt=pt, hT=hT, half=half: e.tensor_copy(out=hT[:, half * 4:(half + 1) * 4, :], in_=pt[:]), reads=[pt], writes=[hT])
                        i_ = rows.start // 128
                        if i_ % 4 == 0:
                            rstate["pl"] = psl.next()
                        pl = rstate["pl"]
                        for c in range(8):
                            mmul(pl[:, (i_ % 4) * 128:(i_ % 4 + 1) * 128], rt[:, c, :], hT[:, c, :], c == 0, c == 7, [rt, hT], [pl])
                        if i_ % 4 == 3 or i_ == nn // 128 - 1:
                            nb_ = (i_ % 4 + 1) * 128
                            c0 = (i_ // 4) * 512
                            ee = e_p.next(); rr = r_p.next()
                            k.op("act", lambda e, pl=pl, ee=ee, nb_=nb_: e.activation(out=ee[:, :nb_], in_=pl[:, :nb_], func=AF.Exp), reads=[pl], writes=[ee])
                            pl2 = psl.next()
                            mmul(pl2[:, :nb_], ones16[:], ee[:, :nb_], True, True, [ones16, ee], [pl2])
                            k.op("dve", lambda e, pl2=pl2, rr=rr, nb_=nb_: e.reciprocal(out=rr[:, :nb_], in_=pl2[:, :nb_]), reads=[pl2], writes=[rr])
                            k.op("dve", lambda e, ee=ee, rr=rr, nb_=nb_, c0=c0: e.tensor_tensor(out=aff_all[:, c0:c0 + nb_], in0=ee[:, :nb_], in1=rr[:, :nb_], op=ALU.mult), reads=[ee, rr], writes=[aff_all])

            pend = None
            NTL = nn // 128
            stage0(0)
            if NTL > 1:
                stage0(1)
            for i in range(NTL):
                if i + 2 < NTL:
                    stage0(i + 2)
                cur = stage1(i)
                if pend is not None:
                    stage2(pend)
                pend = cur
            if pend is not None:
                stage2(pend)
            if kind == 2:
                k.dma("sp", S.affD.ap.ap(), aff_all[:], reads=[aff_all], writes=[S.affD])

    def inproj_phase(specs, l):
        with k.phase():
            wb = k.sbuf("ip_w", [128, 8, IN_COLS], BF16)
            for c in range(8):
                k.dma("pool", wb[:, c, :], w_in.ap[l, c * 128:(c + 1) * 128, :], reads=[w_in], writes=[wb], acc_w=True)
            wp_ = k.sbuf("ip_wp", [128, 8, 512], BF16)
            k.dma("pool", wp_[:], w_qkp.ap[l].rearrange("(c p) n -> p c n", p=128), reads=[w_qkp], writes=[wp_])
            csb = k.sbuf("ip_cs", [128, 256], BF16)
            k.dma("sp", csb[:], cs_c.ap.ap(), reads=[cs_c], writes=[csb])
            lg = k.sbuf("ip_lg", [128, W], F32); lb = k.sbuf("ip_lb", [128, W], F32)
            bc_load(lg, sgu_g, sgu_g.ap[l:l + 1, :], W); bc_load(lb, sgu_b, sgu_b.ap[l:l + 1, :], W)
            for S, kv_only in specs:
              nn, si = S.n, S.si
              TT = 512 if nn >= 512 else nn
              with k.phase():
                hp = Pool(k, "ip_h", 2, [128, 8, TT], BF16)
                ps = Pool(k, "ip_ps", 8, [128, 512], F32, "psum")
                ev = Pool(k, "ip_ev", 4, [128, TT], BF16)
                f32p = Pool(k, "ip_f32", 4, [128, TT], F32)
                rp = Pool(k, "ip_rope", 2, [128, 4, TT], F32)
                zbp = Pool(k, "ip_zb", 2, [128, 2, TT], BF16)
                tmv = Pool(k, "ip_tmv", 3, [128, 512], F32)
                tmb = Pool(k, "ip_tmb", 3, [128, 512], BF16)
                stp = Pool(k, "ip_st", 3, [128, 8], F32)

                def fm(wt, col0, h, reads_w):
                    p = ps.next()
                    for c in range(8):
                        mmul(p[:, :TT], wt[:, c, col0:col0 + 128], h[:, c, :], c == 0, c == 7, [reads_w, h], [p])
                    return p

                def gelu_to(dst_ap, p, width, reads_extra, writes):
                    sq = f32p.next(); t2 = f32p.next()
                    k.op("act", lambda e: e.activation(out=sq[:, :width], in_=p[:, :width], func=AF.Square), reads=[p], writes=[sq])
                    k.op("dve", lambda e: e.tensor_scalar(out=sq[:, :width], in0=sq[:, :width], scalar1=0.044715, scalar2=1.0, op0=ALU.mult, op1=ALU.add), reads=[sq], writes=[sq])
                    k.op("dve", lambda e: e.tensor_tensor(out=t2[:, :width], in0=p[:, :width], in1=sq[:, :width], op=ALU.mult), reads=[p, sq], writes=[t2])
                    k.op("act", lambda e: e.activation(out=t2[:, :width], in_=t2[:, :width], func=AF.Sigmoid, scale=GELU_C), reads=[t2], writes=[t2])
                    k.op("dve", lambda e: e.tensor_tensor(out=dst_ap, in0=p[:, :width], in1=t2[:, :width], op=ALU.mult), reads=[p, t2], writes=writes)

                def ip_loads(j):
                    tk = slice(j * TT, (j + 1) * TT)
                    h = hp.next()
                    k.dma("sp", h[:], S.hT.ap.ap().rearrange("(c p) t -> p c t", p=128)[:, :, tk], reads=[S.hT], writes=[h])
                    rt = None
                    if si == 0:
                        rt = rp.next()
                        k.dma("sp", rt[:], rope.ap.ap().rearrange("f p t -> p f t")[:, :, tk], reads=[rope], writes=[rt])
                    return h, rt
                nxt = ip_loads(0)
                for j in range(nn // TT):
                    tk = slice(j * TT, (j + 1) * TT)
                    h, rt = nxt
                    if j + 1 < nn // TT:
                        nxt = ip_loads(j + 1)
                    fmv = lambda T_: T_.ap.ap().rearrange("(c p) t -> p c t", p=128)
                    if not kv_only:
                        zb = zbp.next()
                        for ch in range(2):
                            p = fm(wb, A_END + ch * 128, h, wb)
                            k.op("act", lambda e, p=p, zb=zb, ch=ch, TT=TT: e.activation(out=zb[:, ch, :], in_=p[:, :TT], func=AF.Copy), reads=[p], writes=[zb])
                    for s_ in range(TT // 128):
                        trow = slice(j * TT + s_ * 128, j * TT + (s_ + 1) * 128)
                        hs = lambda c: h[:, c, s_ * 128:(s_ + 1) * 128]
                        p = ps.next()
                        for c in range(8):
                            mmul(p[:, 0:256], hs(c), wb[:, c, K_END:V_END], c == 0, c == 7, [h, wb], [p])
                        if not kv_only:
                            for c in range(8):
                                mmul(p[:, 256:512], hs(c), wb[:, c, 256:512], c == 0, c == 7, [h, wb], [p])
                        ov = tmb.next()
                        k.op("act", lambda e, ov=ov, p=p: e.activation(out=ov[:, 0:256], in_=p[:, 0:256], func=AF.Copy), reads=[p], writes=[ov])
                        k.dma("sp", S.v.ap[trow, :], ov[:, 0:256], reads=[ov], writes=[S.v], acc_w=True)
                        if kv_only:
                            continue
                        gv = tmv.next(); s8 = stp.next(); ol = tmb.next()
                        sq = f32p.next(); t2 = f32p.next()
                        pv = p
                        k.op("act", lambda e, sq=sq, pv=pv: e.activation(out=sq[:, :256], in_=pv[:, 256:512], func=AF.Square), reads=[pv], writes=[sq])
                        k.op("dve", lambda e, sq=sq: e.tensor_scalar(out=sq[:, :256], in0=sq[:, :256], scalar1=0.044715, scalar2=1.0, op0=ALU.mult, op1=ALU.add), reads=[sq], writes=[sq])
                        k.op("dve", lambda e, sq=sq, t2=t2, pv=pv: e.tensor_tensor(out=t2[:, :256], in0=pv[:, 256:512], in1=sq[:, :256], op=ALU.mult), reads=[pv, sq], writes=[t2])
                        k.op("act", lambda e, t2=t2: e.activation(out=t2[:, :256], in_=t2[:, :256], func=AF.Sigmoid, scale=GELU_C), reads=[t2], writes=[t2])
                        k.op("dve", lambda e, gv=gv, t2=t2, pv=pv: e.tensor_tensor(out=gv[:, :256], in0=pv[:, 256:512], in1=t2[:, :256], op=ALU.mult), reads=[pv, t2], writes=[gv])
                        k.op("dve", lambda e, gv=gv, s8=s8: e.bn_stats(out=s8[:, 0:6], in_=gv[:, :256]), reads=[gv], writes=[s8])
                        k.op("dve", lambda e, s8=s8: e.bn_aggr(out=s8[:, 6:8], in_=s8[:, 0:6]), reads=[s8], writes=[s8])
                        k.op("act", lambda e, s8=s8: e.activation(out=s8[:, 0:1], in_=s8[:, 7:8], func=AF.Sqrt, bias=1e-6, scale=1.0), reads=[s8], writes=[s8])
                        k.op("dve", lambda e, s8=s8: e.reciprocal(out=s8[:, 1:2], in_=s8[:, 0:1]), reads=[s8], writes=[s8])
                        k.op("dve", lambda e, gv=gv, s8=s8: e.tensor_scalar(out=gv[:, :256], in0=gv[:, :256], scalar1=s8[:, 6:7], scalar2=s8[:, 1:2], op0=ALU.subtract, op1=ALU.mult), reads=[gv, s8], writes=[gv])
                        k.op("pool", lambda e, gv=gv: e.tensor_tensor(out=gv[:, :256], in0=gv[:, :256], in1=lg[:], op=ALU.mult), reads=[gv, lg], writes=[gv])
                        k.op("pool", lambda e, gv=gv, ol=ol: e.tensor_tensor(out=ol[:, :256], in0=gv[:, :256], in1=lb[:], op=ALU.add), reads=[gv, lb], writes=[ol])
                        k.dma("sp", S.vln.ap[trow, :], ol[:, :256], reads=[ol], writes=[S.vln], acc_w=True)
                        pz = ps.next()
                        for ch in range(2):
                            mmul(pz[:, ch * 256:(ch + 1) * 256], zb[:, ch, s_ * 128:(s_ + 1) * 128], csb[:], True, True, [zb, csb], [pz])
                        oz = tmb.next()
                        k.op("act", lambda e, oz=oz, pz=pz: e.activation(out=oz[:], in_=pz[:], func=AF.Copy), reads=[pz], writes=[oz])
                        ozv = oz[:].rearrange("p (c r f) -> p c r f", c=2, r=2)
                        k.dma("sp", S.Zr.ap[trow, :].rearrange("t (c f) -> t c f", c=2), ozv[:, :, 0, :], reads=[oz], writes=[S.Zr], acc_w=True)
                        k.dma("sp", S.Zi.ap[trow, :].rearrange("t (c f) -> t c f", c=2), ozv[:, :, 1, :], reads=[oz], writes=[S.Zi], acc_w=True)
                    if not kv_only:
                        for ch in range(2):
                            p = fm(wb, ch * 128, h, wb); o = ev.next()
                            gelu_to(o[:], p, TT, [], [o])
                            k.dma("sp", fmv(S.uT)[:, ch, tk], o[:], reads=[o], writes=[S.uT], acc_w=True)
                        for ch in range(2):
                            pa = fm(wb, B_END + ch * 128, h, wb); pg = fm(wb, B_END + 256 + ch * 128, h, wb)
                            sg = f32p.next(); o = ev.next()
                            k.op("act", lambda e, sg=sg, pg=pg, TT=TT: e.activation(out=sg[:], in_=pg[:, :TT], func=AF.Sigmoid), reads=[pg], writes=[sg])
                            k.op("dve", lambda e, o=o, pa=pa, sg=sg, TT=TT: e.tensor_tensor(out=o[:], in0=pa[:, :TT], in1=sg[:], op=ALU.mult), reads=[pa, sg], writes=[o])
                            k.dma("sp", fmv(S.yT)[:, ch, tk], o[:], reads=[o], writes=[S.yT], acc_w=True)
                    for which, col0, pc0, dstr, dstp in ((0, C_END, 0, S.qrT, S.qpT), (1, Q_END, 256, S.krT, None)):
                        if kv_only and which == 0:
                            continue
                        for ch in range(2):
                            p = fm(wb, col0 + ch * 128, h, wb)
                            if si == 0:
                                pp_ = fm(wp_, pc0 + ch * 128, h, wp_)
                                t1 = f32p.next(); t2 = f32p.next(); o = ev.next()
                                k.op("dve", lambda e, t1=t1, p=p, rt=rt, which=which, TT=TT: e.tensor_tensor(out=t1[:], in0=p[:, :TT], in1=rt[:, 2 * which, :], op=ALU.mult), reads=[p, rt], writes=[t1])
                                k.op("dve", lambda e, t2=t2, pp_=pp_, rt=rt, which=which, TT=TT: e.tensor_tensor(out=t2[:], in0=pp_[:, :TT], in1=rt[:, 2 * which + 1, :], op=ALU.mult), reads=[pp_, rt], writes=[t2])
                                k.op("pool", lambda e, o=o, t1=t1, t2=t2: e.tensor_tensor(out=o[:], in0=t1[:], in1=t2[:], op=ALU.add), reads=[t1, t2], writes=[o])
                                k.dma("sp", fmv(dstr)[:, ch, tk], o[:], reads=[o], writes=[dstr], acc_w=True)
                                if which == 0:
                                    o2 = ev.next()
                                    k.op("act", lambda e, o2=o2, p=p, TT=TT: e.activation(out=o2[:], in_=p[:, :TT], func=AF.Copy, scale=0.125), reads=[p], writes=[o2])
                                    k.dma("sp", fmv(dstp)[:, ch, tk], o2[:], reads=[o2], writes=[dstp], acc_w=True)
                            else:
                                o = ev.next()
                                if which == 0:
                                    k.op("act", lambda e, o=o, p=p, TT=TT: e.activation(out=o[:], in_=p[:, :TT], func=AF.Copy, scale=0.125), reads=[p], writes=[o])
                                    k.dma("sp", fmv(S.qpT)[:, ch, tk], o[:], reads=[o], writes=[S.qpT], acc_w=True)
                                else:
                                    k.op("act", lambda e, o=o, p=p, TT=TT: e.activation(out=o[:], in_=p[:, :TT], func=AF.Copy), reads=[p], writes=[o])
                                    k.dma("sp", fmv(S.krT)[:, ch, tk], o[:], reads=[o], writes=[S.krT], acc_w=True)
                    if not kv_only:
                        for ch in range(32):
                            p = fm(wb, V_END + ch * 128, h, wb); o = ev.next()
                            k.op("act", lambda e, p=p, o=o, TT=TT: e.activation(out=o[:], in_=p[:, :TT], func=AF.Sigmoid), reads=[p], writes=[o])
                            k.dma("sp", S.gT.ap.ap().rearrange("(c p) t -> p c t", p=128)[:, ch, tk], o[:], reads=[o], writes=[S.gT], acc_w=True)

    def gmlp_phase(S, l):
        nn = S.n
        TT = 512 if nn >= 512 else nn
        NCH = TT // 128
        fmv = lambda T_: T_.ap.ap().rearrange("(c p) t -> p c t", p=128)
        with k.phase():
            ws = k.sbuf("gm_ws", [128, 4, 128], BF16)
            k.dma("pool", ws[:], wsT.ap[l], reads=[wsT], writes=[ws])
            bs = k.sbuf("gm_bs", [1, 4, 128], BF16)
            k.dma("pool", bs[:], bsp.ap[l:l + 1], reads=[bsp], writes=[bs])
            ones = k.sbuf("gm_ones", [1, 128], BF16)
            k.op("dve", lambda e: e.memset(ones[:], 1.0), writes=[ones])
            vp = Pool(k, "gm_v", 2, [128, NCH, W], BF16)
            up = Pool(k, "gm_u", 2, [128, 2, TT], BF16)
            ap_ = Pool(k, "gm_a", 2, [128, 2, TT], BF16)
            pp = Pool(k, "gm_ps", 8, [128, NCH, 128], F32, "psum")
            def gm_loads(j):
                tk = slice(j * TT, (j + 1) * TT)
                vt = vp.next(); ut = up.next()
                k.dma("sp", vt[:], S.vln.ap[tk, :].rearrange("(c p) f -> p c f", p=128), reads=[S.vln], writes=[vt])
                k.dma("sp", ut[:], fmv(S.uT)[:, :, tk], reads=[S.uT], writes=[ut])
                return vt, ut
            nxt = gm_loads(0)
            for j in range(nn // TT):
                tk = slice(j * TT, (j + 1) * TT)
                vt, ut = nxt
                if j + 1 < nn // TT:
                    nxt = gm_loads(j + 1)
                at = ap_.next()
                for hf in range(2):
                    for gi in range(2):
                        g = 2 * hf + gi
                        p = pp.next()
                        for cc in range(NCH):
                            mmul(p[:, cc, :], vt[:, cc, hf * 128:(hf + 1) * 128], ws[:, g, :], True, False, [vt, ws], [p])
                            mmul(p[:, cc, :], ones[:], bs[:, g, :], False, True, [ones, bs], [p])
                        pr = slice(gi * 64, (gi + 1) * 64)
                        k.op("dve", lambda e, at=at, p=p, ut=ut, pr=pr, hf=hf: e.tensor_tensor(out=at[pr, hf, :].rearrange("p (c q) -> p c q", q=128), in0=p[pr, :, :], in1=ut[pr, hf, :].rearrange("p (c q) -> p c q", q=128), op=ALU.mult), reads=[p, ut], writes=[at])
                k.dma("sp", fmv(S.aT)[:, :, tk], at[:], reads=[at], writes=[S.aT], acc_w=True)

    def conv_phase(S, l):
        nn = S.n
        TT = 512 if nn >= 512 else nn
        fmv = lambda T_: T_.ap.ap().rearrange("(c p) t -> p c t", p=128)
        with k.phase():
            identf, = load_ident(False, True)
            cw = k.sbuf("cv_w", [128, 2, 31], F32)
            k.dma("sp", cw[:], conv_wT.ap[l].rearrange("(h p) j -> p h j", p=128), reads=[conv_wT], writes=[cw])
            prm = k.sbuf("cv_prm", [128, 3, 2], F32)
            for i_, src in enumerate((conv_b, cln_g, cln_b)):
                for hf in range(2):
                    k.dma("sp", prm[:, i_, hf:hf + 1], src.ap[l, hf * 128:(hf + 1) * 128, :], reads=[src], writes=[prm], acc_w=True)
            dg = k.sbuf("cv_diag", [128, 2, 31, 128], BF16)
            for hf in range(2):
                for j in range(31):
                    k.op("dve", lambda e, hf=hf, j=j: e.tensor_scalar(out=dg[:, hf, j, :], in0=identf[:], scalar1=cw[:, hf, j:j + 1], scalar2=None, op0=ALU.mult), reads=[identf, cw], writes=[dg])
            onesf = k.sbuf("cv_ones", [128, 128], F32)
            k.op("dve", lambda e: e.memset(onesf[:], 1.0 / W), writes=[onesf])
            yp = Pool(k, "cv_y", 2, [128, 2, TT + 30], BF16)
            pc = Pool(k, "cv_pc", 4, [128, TT], F32, "psum")
            pst = Pool(k, "cv_pst", 2, [128, 2, TT], F32, "psum")
            y2p = Pool(k, "cv_y2", 3, [128, 2, TT], F32)
            sqp = Pool(k, "cv_sq", 3, [128, 2, TT], F32)
            stp = Pool(k, "cv_st", 2, [128, 2, TT], F32)
            op_ = Pool(k, "cv_o", 2, [128, 2, TT], BF16)
            def cv_a(j):
                    t0 = j * TT
                    yt = yp.next()
                    lo, hi = max(0, t0 - 15), min(nn, t0 + TT + 15)
                    if lo > t0 - 15 or hi < t0 + TT + 15:
                        k.op("pool", lambda e, yt=yt: e.memset(yt[:], 0.0), writes=[yt])
                    k.dma("sp", yt[:, :, lo - (t0 - 15):hi - (t0 - 15)], fmv(S.yT)[:, :, lo:hi], reads=[S.yT], writes=[yt])
                    y2 = y2p.next(); sq = sqp.next()
                    for hf in range(2):
                        p = pc.next()
                        for jj in range(31):
                            mmul(p[:], dg[:, hf, jj, :], yt[:, hf, jj:jj + TT], jj == 0, jj == 30, [dg, yt], [p])
                        k.op("act", lambda e, y2=y2, p=p, hf=hf: e.activation(out=y2[:, hf, :], in_=p[:], func=AF.Identity, bias=prm[:, 0, hf:hf + 1], scale=1.0), reads=[p, prm], writes=[y2])
                        k.op("act", lambda e, y2=y2, sq=sq, hf=hf: e.activation(out=sq[:, hf, :], in_=y2[:, hf, :], func=AF.Square), reads=[y2], writes=[sq])
                    return (t0, y2, sq)
            def cv_b(ctx_):
                    t0, y2, sq = ctx_
                    ps_ = pst.next()
                    for hf in range(2):
                        mmul(ps_[:, 0, :], onesf[:], y2[:, hf, :], hf == 0, hf == 1, [onesf, y2], [ps_])
                    for hf in range(2):
                        mmul(ps_[:, 1, :], onesf[:], sq[:, hf, :], hf == 0, hf == 1, [onesf, sq], [ps_])
                    stt = stp.next(); ot = op_.next()
                    k.op("act", lambda e, stt=stt, ps_=ps_: e.activation(out=stt[:, 0, :], in_=ps_[:, 0, :], func=AF.Copy), reads=[ps_], writes=[stt])
                    k.op("dve", lambda e, stt=stt: e.tensor_tensor(out=stt[:, 1, :], in0=stt[:, 0, :], in1=stt[:, 0, :], op=ALU.mult), reads=[stt], writes=[stt])
                    k.op("dve", lambda e, stt=stt, ps_=ps_: e.tensor_tensor(out=stt[:, 1, :], in0=ps_[:, 1, :], in1=stt[:, 1, :], op=ALU.subtract), reads=[ps_, stt], writes=[stt])
                    k.op("act", lambda e, stt=stt: e.activation(out=stt[:, 1, :], in_=stt[:, 1, :], func=AF.Sqrt, bias=1e-6, scale=1.0), reads=[stt], writes=[stt])
                    k.op("dve", lambda e, stt=stt: e.reciprocal(out=stt[:, 1, :], in_=stt[:, 1, :]), reads=[stt], writes=[stt])
                    for hf in range(2):
                        k.op("dve", lambda e, y2=y2, stt=stt, hf=hf: e.tensor_tensor(out=y2[:, hf, :], in0=y2[:, hf, :], in1=stt[:, 0, :], op=ALU.subtract), reads=[y2, stt], writes=[y2])
                        k.op("pool", lambda e, y2=y2, stt=stt, hf=hf: e.tensor_tensor(out=y2[:, hf, :], in0=y2[:, hf, :], in1=stt[:, 1, :], op=ALU.mult), reads=[y2, stt], writes=[y2])
                        k.op("act", lambda e, y2=y2, ot=ot, hf=hf: e.activation(out=ot[:, hf, :], in_=y2[:, hf, :], func=AF.Silu, bias=prm[:, 2, hf:hf + 1], scale=prm[:, 1, hf:hf + 1]), reads=[y2, prm], writes=[ot])
                    k.dma("sp", fmv(S.cT)[:, :, t0:t0 + TT], ot[:], reads=[ot], writes=[S.cT], acc_w=True)
            pend = None
            for j in range(nn // TT):
                cur = cv_a(j)
                if pend is not None:
                    cv_b(pend)
                pend = cur
            cv_b(pend)

    def fourier_phase(S, l):
        nn, NN1 = S.n, S.N1
        fmv = lambda T_: T_.ap.ap().rearrange("(c p) t -> p c t", p=128)
        CB = 4096
        with k.phase():
            f1sb = k.sbuf("ff_f1", [NN1, 3, NN1], BF16)
            k.dma("sp", f1sb[:], S.f1.ap.ap().rearrange("m c q -> c m q"), reads=[S.f1], writes=[f1sb])
            zp = Pool(k, "ff_z", 4, [NN1, CB], BF16)
            yp = Pool(k, "ff_y", 4, [NN1, CB], BF16)
            pp = Pool(k, "ff_ps", 4, [NN1, 512], F32, "psum")
            Zrv = S.Zr.ap.ap().rearrange("(c p) f -> c (p f)", p=128)
            Ziv = S.Zi.ap.ap().rearrange("(c p) f -> c (p f)", p=128)
            def ff_loads(blk):
                cb = slice(blk * CB, (blk + 1) * CB)
                zr = zp.next(); zi = zp.next()
                k.dma("sp", zr[:], Zrv[:, cb], reads=[S.Zr], writes=[zr])
                k.dma("sp", zi[:], Ziv[:, cb], reads=[S.Zi], writes=[zi])
                return zr, zi
            nxt = ff_loads(0)
            for blk in range(128 * W // CB):
                cb = slice(blk * CB, (blk + 1) * CB)
                zr, zi = nxt
                if blk + 1 < 128 * W // CB:
                    nxt = ff_loads(blk + 1)
                yr = yp.next(); yi = yp.next()
                for sub in range(CB // 512):
                    cs_ = slice(sub * 512, (sub + 1) * 512)
                    pr = pp.next(); pi = pp.next()
                    mmul(pr[:], f1sb[:, 0, :], zr[:, cs_], True, False, [f1sb, zr], [pr])
                    mmul(pr[:], f1sb[:, 1, :], zi[:, cs_], False, True, [f1sb, zi], [pr])
                    mmul(pi[:], f1sb[:, 0, :], zi[:, cs_], True, False, [f1sb, zi], [pi])
                    mmul(pi[:], f1sb[:, 2, :], zr[:, cs_], False, True, [f1sb, zr], [pi])
                    k.op("act", lambda e, yr=yr, pr=pr, cs_=cs_: e.activation(out=yr[:, cs_], in_=pr[:], func=AF.Copy), reads=[pr], writes=[yr])
                    k.op("dve", lambda e, yi=yi, pi=pi, cs_=cs_: e.tensor_copy(out=yi[:, cs_], in_=pi[:]), reads=[pi], writes=[yi])
                k.dma("sp", S.Y1r.ap[:, cb], yr[:], reads=[yr], writes=[S.Y1r], acc_w=True)
                k.dma("sp", S.Y1i.ap[:, cb], yi[:], reads=[yi], writes=[S.Y1i], acc_w=True)
        with k.phase():
            KB = min(8, NN1)
            G = min(4, KB)
            f2sb = k.sbuf("ff_f2", [128, 2, 128], BF16)
            k.dma("sp", f2sb[:], f2.ap.ap().rearrange("m p q -> p m q"), reads=[f2], writes=[f2sb])
            tw = k.sbuf("ff_tw", [128, 2, NN1], F32)
            k.dma("sp", tw[:], S.tw.ap.ap().rearrange("m p q -> p m q"), reads=[S.tw], writes=[tw])
            outT = k.sbuf("ff_out", [128, 2, nn], BF16)
            yrp = Pool(k, "ff_yr", 2, [128, KB, W], BF16)
            yip = Pool(k, "ff_yi", 2, [128, KB, W], BF16)
            ypr = Pool(k, "ff_ypr", 2, [128, KB, W], BF16)
            ypi = Pool(k, "ff_ypi", 2, [128, KB, W], BF16)
            tp = Pool(k, "ff_t", 6, [128, W], F32)
            po = Pool(k, "ff_po", 4, [128, G, 128], F32, "psum")
            Y1rv = S.Y1r.ap.ap().rearrange("q (p f) -> p q f", p=128)
            Y1iv = S.Y1i.ap.ap().rearrange("q (p f) -> p q f", p=128)
            def f2_loads(kb):
                ks = slice(kb * KB, (kb + 1) * KB)
                yr = yrp.next(); yi = yip.next()
                k.dma("sp", yr[:], Y1rv[:, ks, :], reads=[S.Y1r], writes=[yr])
                k.dma("sp", yi[:], Y1iv[:, ks, :], reads=[S.Y1i], writes=[yi])
                return yr, yi
            nxt = f2_loads(0)
            for kb in range(NN1 // KB):
                ks = slice(kb * KB, (kb + 1) * KB)
                yr, yi = nxt
                if kb + 1 < NN1 // KB:
                    nxt = f2_loads(kb + 1)
                qr = ypr.next(); qi = ypi.next()
                for kk in range(KB):
                    k1 = kb * KB + kk
                    t1 = tp.next(); t2 = tp.next()
                    k.op("act", lambda e, t1=t1, yi=yi, kk=kk, k1=k1: e.activation(out=t1[:], in_=yi[:, kk, :], func=AF.Copy, scale=tw[:, 1, k1:k1 + 1]), reads=[yi, tw], writes=[t1])
                    k.op("dve", lambda e, t1=t1, yr=yr, qr=qr, kk=kk, k1=k1: e.scalar_tensor_tensor(out=qr[:, kk, :], in0=yr[:, kk, :], scalar=tw[:, 0, k1:k1 + 1], in1=t1[:], op0=ALU.mult, op1=ALU.subtract), reads=[yr, tw, t1], writes=[qr])
                    k.op("act", lambda e, t2=t2, yi=yi, kk=kk, k1=k1: e.activation(out=t2[:], in_=yi[:, kk, :], func=AF.Copy, scale=tw[:, 0, k1:k1 + 1]), reads=[yi, tw], writes=[t2])
                    k.op("dve", lambda e, t2=t2, yr=yr, qi=qi, kk=kk, k1=k1: e.scalar_tensor_tensor(out=qi[:, kk, :], in0=yr[:, kk, :], scalar=tw[:, 1, k1:k1 + 1], in1=t2[:], op0=ALU.mult, op1=ALU.add), reads=[yr, tw, t2], writes=[qi])
                for fh in range(2):
                    fs = slice(fh * 128, (fh + 1) * 128)
                    for g0 in range(0, KB, G):
                        p = po.next()
                        for gg in range(G):
                            kk = g0 + gg
                            mmul(p[:, gg, :], qr[:, kk, fs], f2sb[:, 0, :], True, False, [qr, f2sb], [p])
                            mmul(p[:, gg, :], qi[:, kk, fs], f2sb[:, 1, :], False, True, [qi, f2sb], [p])
                        k1_0 = kb * KB + g0
                        dst = outT[:, fh, :].rearrange("p (a b) -> p b a", b=NN1)[:, k1_0:k1_0 + G, :]
                        k.op("act", lambda e, dst=dst, p=p: e.activation(out=dst, in_=p[:], func=AF.Copy), reads=[p], writes=[outT])
            k.dma("sp", fmv(S.bT), outT[:], reads=[outT], writes=[S.bT])

    def attention_phase(S, SCx, l):
        nn, si = S.n, S.si
        NT = nn // 128
        fmv = lambda T_: T_.ap.ap().rearrange("(c p) t -> p c t", p=128)
        with k.phase():
            identb, = load_ident(True, False)
            kc = k.sbuf("at_kc", [128, 2, NCTX], BF16)
            k.dma("sp", kc[:], fmv(SCx.krT), reads=[SCx.krT], writes=[kc])
            vc = k.sbuf("at_vc", [128, 2, 4, 65], BF16)
            k.op("dve", lambda e: e.memset(vc[:], 1.0), writes=[vc])
            for t_ in range(2):
                k.dma("sp", vc[:, t_, :, 0:64], SCx.v.ap[t_ * 128:(t_ + 1) * 128, :].rearrange("p (h d) -> p h d", h=4), reads=[SCx.v], writes=[vc], acc_w=True)
            if si == 0:
                kr = k.sbuf("at_kr", [128, 2, nn], BF16)
                k.dma("sp", kr[:], fmv(S.krT), reads=[S.krT], writes=[kr])
                vs = k.sbuf("at_v", [128, NT, 4, 65], BF16)
                k.op("dve", lambda e: e.memset(vs[:], 1.0), writes=[vs])
                for t_ in range(NT):
                    k.dma("sp", vs[:, t_, :, 0:64], S.v.ap[t_ * 128:(t_ + 1) * 128, :].rearrange("p (h d) -> p h d", h=4), reads=[S.v], writes=[vs], acc_w=True)
                bias = k.sbuf("at_bias", [128, 4 * U, 128], BF16)
                k.dma("sp", bias[:], biasT.ap[l], reads=[biasT], writes=[bias])
            qrp = Pool(k, "at_qr", 3, [128, 2, 128], BF16)
            qpp = Pool(k, "at_qp", 3, [128, 2, 128], BF16)
            psS = Pool(k, "at_S", 2, [128, 8, 128], F32, "psum")
            psO = Pool(k, "at_O", 2, [128, 128], F32, "psum")
            psT = Pool(k, "at_T", 1, [128, 2, 128], BF16, "psum")
            Ep = Pool(k, "at_E", 3, [128, 8, 128], BF16)
            rcp = Pool(k, "at_rc", 4, [128, 1], F32)
            otp = Pool(k, "at_ot", 3, [128, W], BF16)
            dtp = Pool(k, "at_dt", 2, [128, 2, 128], BF16)
            items = [(qt, h) for qt in range(NT) for h in range(4)]
            qst = {}

            def stage_s(qt, h):
                qs = slice(qt * 128, (qt + 1) * 128)
                if h == 0:
                    qp_t = qpp.next()
                    k.dma("sp", qp_t[:], fmv(S.qpT)[:, :, qs], reads=[S.qpT], writes=[qp_t])
                    qr_t = None
                    lst = []
                    if si == 0:
                        qr_t = qrp.next()
                        k.dma("sp", qr_t[:], fmv(S.qrT)[:, :, qs], reads=[S.qrT], writes=[qr_t])
                        lst = qtiles[qt]
                    qst[qt] = (qp_t, qr_t, lst)
                qp_t, qr_t, lst = qst[qt]
                nw = len(lst); nt = nw + 2
                ch, pr = h // 2, slice((h % 2) * 64, (h % 2) * 64 + 64)
                Sp = psS.next(); E = Ep.next()
                for j_, (kt, u) in enumerate(lst):
                    mmul(Sp[:, j_, :], identb[:], bias[:, h * U + u, :], True, False, [identb, bias], [Sp])
                    mmul(Sp[:, j_, :], kr[pr, ch, kt * 128:(kt + 1) * 128], qr_t[pr, ch, :], False, True, [kr, qr_t], [Sp])
                for cj in range(2):
                    mmul(Sp[:, nw + cj, :], kc[pr, ch, cj * 128:(cj + 1) * 128], qp_t[pr, ch, :], True, True, [kc, qp_t], [Sp])
                k.op("act", lambda e, E=E, Sp=Sp, nt=nt: e.activation(out=E[:, 0:nt, :], in_=Sp[:, 0:nt, :], func=AF.Exp), reads=[Sp], writes=[E])
                return E

            cur_ot = {}

            def stage_pv(qt, h, E):
                qs = slice(qt * 128, (qt + 1) * 128)
                qp_t, qr_t, lst = qst[qt]
                nw = len(lst); nt = nw + 2
                if h == 0:
                    cur_ot[qt] = otp.next()
                ot = cur_ot[qt]
                O = psO.next(); rc = rcp.next()
                for j_ in range(nt):
                    rhs = vs[:, lst[j_][0], h, :] if j_ < nw else vc[:, j_ - nw, h, :]
                    mmul(O[:, 0:65], E[:, j_, :], rhs, j_ == 0, j_ == nt - 1, [E, vc] + ([vs] if si == 0 else []), [O])
                k.op("dve", lambda e, rc=rc, O=O: e.reciprocal(out=rc[:], in_=O[:, 64:65]), reads=[O], writes=[rc])
                k.op("dve", lambda e, ot=ot, O=O, rc=rc, h=h: e.tensor_scalar(out=ot[:, h * 64:(h + 1) * 64], in0=O[:, 0:64], scalar1=rc[:, 0:1], scalar2=None, op0=ALU.mult), reads=[O, rc], writes=[ot])
                if h == 3:
                    pt = psT.next(); dt = dtp.next()
                    for c in range(2):
                        k.op("pe", lambda e, c=c, pt=pt, ot=ot: e.transpose(out=pt[:, c, :], in_=ot[:, c * 128:(c + 1) * 128], identity=identb[:]), reads=[ot, identb], writes=[pt], signal=(c == 1))
                    k.op("act", lambda e, pt=pt, dt=dt: e.activation(out=dt[:], in_=pt[:], func=AF.Copy), reads=[pt], writes=[dt])
                    k.dma("sp", fmv(S.dT)[:, :, qs], dt[:], reads=[dt], writes=[S.dT], acc_w=True)

            Es = {0: stage_s(*items[0])}
            for i_, (qt, h) in enumerate(items):
                if i_ + 1 < len(items):
                    Es[i_ + 1] = stage_s(*items[i_ + 1])
                stage_pv(qt, h, Es.pop(i_))

    def merge_phase(S, l, Xsrc, Xdst):
        nn, si = S.n, S.si
        TT = 512 if nn >= 512 else nn
        fmv = lambda T_: T_.ap.ap().rearrange("(c p) t -> p c t", p=128)
        with k.phase():
            wbr = []
            for br in range(4):
                t = k.sbuf("mg_wbr", [128, 2, D], BF16)
                k.dma("pool", t[:], w_br[br].ap[l].rearrange("(c p) n -> p c n", p=128), reads=[w_br[br]], writes=[t])
                wbr.append(t)
            wo = k.sbuf("mg_wo", [128, 8, D], BF16)
            k.dma("pool", wo[:], w_out.ap[l].rearrange("(c p) n -> p c n", p=128), reads=[w_out], writes=[wo])
            identb, = load_ident(True, False)
            gtb = k.sbuf("mg_gt", [128, D], F32)
            bc_load(gtb, modD[l], modD[l].ap[si:si + 1, 2 * D:3 * D], D)
            brp = [Pool(k, "mg_br%d" % i, 2, [128, 2, TT], BF16) for i in range(4)]
            gp = Pool(k, "mg_g", 2, [128, 32, TT], BF16)
            ps = Pool(k, "mg_ps", 8, [128, 512], F32, "psum")
            tp = Pool(k, "mg_t", 8, [128, TT], BF16)
            mp = Pool(k, "mg_m", 2, [128, 8, TT], BF16)
            xp = Pool(k, "mg_x", 2, [128, D], F32)
            tmp = Pool(k, "mg_tmp", 2, [128, D], F32)
            xop = Pool(k, "mg_xo", 2, [128, D], F32)
            srcs = [S.aT, S.bT, S.cT, S.dT]
            def out_group(j, mT, g_, st_):
                s_, half = g_ // 2, g_ % 2
                rows = slice(j * TT + s_ * 128, j * TT + (s_ + 1) * 128)
                if half == 0:
                    st_["xt"] = xp.next(); st_["tm"] = tmp.next(); st_["xo"] = xop.next()
                    k.dma("sp", st_["xt"][:], Xsrc.ap[rows, :], reads=[Xsrc], writes=[st_["xt"]])
                xt, tm, xo = st_["xt"], st_["tm"], st_["xo"]
                hs = slice(half * 512, (half + 1) * 512)
                p = ps.next()
                for kc in range(8):
                    mmul(p[:], mT[:, kc, s_ * 128:(s_ + 1) * 128], wo[:, kc, hs], kc == 0, kc == 7, [mT, wo], [p])
                k.op("dve", lambda e, tm=tm, p=p, hs=hs: e.tensor_tensor(out=tm[:, hs], in0=p[:], in1=gtb[:, hs], op=ALU.mult), reads=[p, gtb], writes=[tm])
                if half == 1:
                    k.op("pool", lambda e, xo=xo, tm=tm, xt=xt: e.tensor_tensor(out=xo[:], in0=tm[:], in1=xt[:], op=ALU.add), reads=[tm, xt], writes=[xo])
                    k.dma("sp", Xdst.ap[rows, :], xo[:], reads=[xo], writes=[Xdst], acc_w=True)

            NG = (TT // 128) * 2
            prev = None
            def mg_loads(j):
                tk = slice(j * TT, (j + 1) * TT)
                bts = []
                for br in range(4):
                    t = brp[br].next()
                    k.dma("sp", t[:], fmv(srcs[br])[:, :, tk], reads=[srcs[br]], writes=[t])
                    bts.append(t)
                g = gp.next()
                for q4 in range(4):
                    k.dma("sp", g[:, q4 * 8:(q4 + 1) * 8, :], S.gT.ap.ap().rearrange("(c p) t -> p c t", p=128)[:, q4 * 8:(q4 + 1) * 8, tk], reads=[S.gT], writes=[g], acc_w=True)
                return bts, g
            nxt = mg_loads(0)
            for j in range(nn // TT):
                tk = slice(j * TT, (j + 1) * TT)
                bts, g = nxt
                if j + 1 < nn // TT:
                    nxt = mg_loads(j + 1)
                mT = mp.next()
                st_ = {}
                for oc in range(8):
                    ts = []
                    for br in range(4):
                        p = ps.next()
                        for kc in range(2):
                            mmul(p[:, :TT], wbr[br][:, kc, oc * 128:(oc + 1) * 128], bts[br][:, kc, :], kc == 0, kc == 1, [wbr[br], bts[br]], [p])
                        t = tp.next()
                        k.op("dve", lambda e, t=t, p=p, g=g, br=br, oc=oc: e.tensor_tensor(out=t[:], in0=p[:, :TT], in1=g[:, br * 8 + oc, :], op=ALU.mult), reads=[p, g], writes=[t])
                        ts.append(t)
                    if prev is not None and oc < NG:
                        out_group(prev[0], prev[1], oc, st_)
                    pm = ps.next()
                    for br in range(4):
                        mmul(pm[:, :TT], identb[:], ts[br][:], br == 0, br == 3, [identb, ts[br]], [pm])
                    k.op("act", lambda e, pm=pm, mT=mT, oc=oc: e.activation(out=mT[:, oc, :], in_=pm[:, :TT], func=AF.Copy), reads=[pm], writes=[mT])
                prev = (j, mT)
            st_ = {}
            for g_ in range(NG):
                out_group(prev[0], prev[1], g_, st_)

    def route_phase(S, l):
        nn, cap_ = S.n, S.cap
        Tseg = nn // 8
        NCH = nn // 128
        if not hasattr(S, "slot2D"):
            S.slot2D = scr(S.name + "_slot2D", [NE, nn], F32)
        seg = lambda T_: T_.ap.ap().rearrange("e (s t) -> (e s) t", s=8)
        with k.phase():
            blk = k.sbuf("rt_blk", [128, 128], F32); low = k.sbuf("rt_low", [128, 128], F32)
            k.dma("sp", blk[:], r_blk.ap.ap(), reads=[r_blk], writes=[blk])
            k.dma("sp", low[:], r_low.ap.ap(), reads=[r_low], writes=[low])
            A = k.sbuf("rt_A", [128, Tseg], F32)
            k.dma("sp", A[:], seg(S.affD), reads=[S.affD], writes=[A])
            junk = k.sbuf("rt_junk", [128, Tseg], F32)
            sv = k.sbuf("rt_sv", [128, 8], F32)
            k.op("dve", lambda e: e.memset(sv[:], 0.0), writes=[sv])
            k.op("dve", lambda e: e.memset(sv[:, 1:2], 1.0), reads=[sv], writes=[sv])
            pc = Pool(k, "rt_pc", 2, [128, 1], F32, "psum")
            for it in range(30):
                step = 2.0 ** -(it + 1)
                k.op("dve", lambda e, step=step: e.tensor_scalar(out=sv[:, 2:3], in0=sv[:, 0:1], scalar1=step, scalar2=None, op0=ALU.add), reads=[sv], writes=[sv])
                k.op("dve", lambda e: e.tensor_scalar(out=junk[:], in0=A[:], scalar1=sv[:, 2:3], scalar2=0.0, op0=ALU.is_ge, op1=ALU.add, accum_out=sv[:, 3:4]), reads=[A, sv], writes=[junk, sv])
                p = pc.next()
                mmul(p[:], blk[:], sv[:, 3:4], True, True, [blk, sv], [p])
                k.op("dve", lambda e, p=p, step=step: e.tensor_scalar(out=sv[:, 4:5], in0=p[:], scalar1=float(cap_), scalar2=step, op0=ALU.is_ge, op1=ALU.mult), reads=[p, sv], writes=[sv])
                k.op("dve", lambda e: e.tensor_tensor(out=sv[:, 0:1], in0=sv[:, 0:1], in1=sv[:, 4:5], op=ALU.add), reads=[sv], writes=[sv])
            mask = k.sbuf("rt_mask", [128, Tseg], F32)
            k.op("dve", lambda e: e.tensor_scalar(out=mask[:], in0=A[:], scalar1=sv[:, 0:1], scalar2=None, op0=ALU.is_ge), reads=[A, sv], writes=[mask])
            k.op("pool", lambda e: e.memset(junk[:], 0.0), writes=[junk])
            cs = k.sbuf("rt_cs", [128, Tseg], F32)
            k.op("dve", lambda e: e.tensor_tensor_scan(out=cs[:], data0=mask[:], data1=junk[:], initial=0.0, op0=ALU.add, op1=ALU.add), reads=[mask, junk], writes=[cs])
            k.op("dve", lambda e: e.tensor_copy(out=sv[:, 6:7], in_=cs[:, Tseg - 1:Tseg]), reads=[cs, sv], writes=[sv])
            p = pc.next()
            mmul(p[:], low[:], sv[:, 6:7], True, True, [low, sv], [p])
            k.op("dve", lambda e, p=p: e.tensor_copy(out=sv[:, 7:8], in_=p[:]), reads=[p, sv], writes=[sv])
            INV = 2016.0
            k.op("dve", lambda e: e.tensor_scalar(out=cs[:], in0=cs[:], scalar1=sv[:, 7:8], scalar2=-(INV + 1.0), op0=ALU.add, op1=ALU.add), reads=[cs, sv], writes=[cs])
            k.op("dve", lambda e: e.tensor_tensor(out=cs[:], in0=cs[:], in1=mask[:], op=ALU.mult), reads=[cs, mask], writes=[cs])
            k.op("dve", lambda e: e.tensor_scalar(out=cs[:], in0=cs[:], scalar1=INV, scalar2=None, op0=ALU.add), reads=[cs], writes=[cs])
            si_ = k.sbuf("rt_si", [128, Tseg], I32); s2 = k.sbuf("rt_s2", [128, Tseg], I32)
            k.op("dve", lambda e: e.tensor_copy(out=si_[:], in_=cs[:]), reads=[cs], writes=[si_])
            k.op("dve", lambda e: e.tensor_scalar(out=s2[:], in0=si_[:], scalar1=5, scalar2=None, op0=ALU.arith_shift_right), reads=[si_], writes=[s2])
            k.op("dve", lambda e: e.tensor_copy(out=mask[:], in_=s2[:]), reads=[s2], writes=[mask])
            k.dma("sp", seg(S.slotD), mask[:], reads=[mask], writes=[S.slotD])
            s3 = k.sbuf("rt_s3", [128, Tseg], I32)
            k.op("dve", lambda e: e.tensor_scalar(out=s3[:], in0=si_[:], scalar1=31, scalar2=None, op0=ALU.bitwise_and), reads=[si_], writes=[s3])
            k.op("dve", lambda e: e.tensor_copy(out=junk[:], in_=s3[:]), reads=[s3], writes=[junk])
            k.dma("sp", seg(S.slot2D), junk[:], reads=[junk], writes=[S.slot2D])
        with k.phase():
            identf, = load_ident(False, True)
            iota = k.sbuf("rt_iota", [128, NE, 32], F32)
            k.dma("sp", iota[:], r_iota.ap.ap(), reads=[r_iota], writes=[iota])
            tid = k.sbuf("rt_tid", [128, NCH], F32)
            k.dma("sp", tid[:], S.tid.ap.ap(), reads=[S.tid], writes=[tid])
            rows3 = []
            for nm, src in (("hi", S.slotD), ("lo", S.slot2D), ("af", S.affD)):
                t = k.sbuf("rt_" + nm, [NE, nn], F32)
                k.dma("sp", t[:], src.ap.ap(), reads=[src], writes=[t])
                rows3.append(t)
            pT = Pool(k, "rt_pT", 3, [128, 3, NE], F32, "psum")
            tmp_ = Pool(k, "rt_tm", 3, [128, 3, NE], F32)
            Ap = Pool(k, "rt_Ap", 3, [128, NE, 32], F32)
            Bp = Pool(k, "rt_Bp", 3, [128, NE, 32], F32)
            AGp = Pool(k, "rt_AG", 3, [128, NE, 2, 32], F32)
            accp = Pool(k, "rt_acc", 2, [64, NE, 32], F32, "psum")
            res = k.sbuf("rt_res", [64, NE, 32], F32)
            k.op("dve", lambda e: e.memset(res[:], 0.0), writes=[res])
            for c in range(NCH):
                cs_ = slice(c * 128, (c + 1) * 128)
                pt = pT.next(); tm = tmp_.next(); Aa = Ap.next(); Bb = Bp.next(); AG = AGp.next()
                for i_ in range(3):
                    k.op("pe", lambda e, i_=i_, pt=pt, cs_=cs_: e.transpose(out=pt[:, i_, :], in_=rows3[i_][:, cs_], identity=identf[0:NE, 0:NE]), reads=[rows3[i_], identf], writes=[pt], signal=(i_ == 2))
                k.op("act", lambda e, pt=pt, tm=tm: e.activation(out=tm[:], in_=pt[:], func=AF.Copy), reads=[pt], writes=[tm])
                b0, b1, b2 = [tm[:, i_, :].unsqueeze(2).to_broadcast([128, NE, 32]) for i_ in range(3)]
                k.op("dve", lambda e, Aa=Aa, b0=b0: e.tensor_tensor(out=Aa[:], in0=iota[:], in1=b0, op=ALU.is_equal), reads=[iota, tm], writes=[Aa])
                k.op("dve", lambda e, Bb=Bb, b1=b1: e.tensor_tensor(out=Bb[:], in0=iota[:], in1=b1, op=ALU.is_equal), reads=[iota, tm], writes=[Bb])
                k.op("dve", lambda e, Aa=Aa, AG=AG, b2=b2: e.tensor_tensor(out=AG[:, :, 1, :], in0=Aa[:], in1=b2, op=ALU.mult), reads=[Aa, tm], writes=[AG])
                k.op("act", lambda e, Aa=Aa, AG=AG, c=c: e.activation(out=AG[:, :, 0, :], in_=Aa[:], func=AF.Copy, scale=tid[:, c:c + 1]), reads=[Aa, tid], writes=[AG])
                acc = accp.next()
                for e_ in range(NE):
                    mmul(acc[:, e_, :], AG[:, e_, :, :], Bb[:, e_, :], True, True, [AG, Bb], [acc])
                k.op("dve", lambda e, acc=acc: e.tensor_tensor(out=res[:], in0=acc[:], in1=res[:], op=ALU.add), reads=[acc, res], writes=[res])
            resi = k.sbuf("rt_resi", [32, NE, 32], I32)
            k.op("dve", lambda e: e.tensor_copy(out=resi[:], in_=res[0:32]), reads=[res], writes=[resi])
            na = max(1, cap_ // 32)
            k.dma("sp", S.idxD.ap.ap().rearrange("e (a b) -> a e b", b=32), resi[0:na], reads=[resi], writes=[S.idxD])
            k.dma("sp", S.gateD.ap.ap().rearrange("e (a b) -> a e b", b=32), res[32:32 + na], reads=[res], writes=[S.gateD])

    def experts_phase(l, streams):
        with k.phase():
            identb, = load_ident(True, False)
            gtb = {}
            for S in streams:
                t = k.sbuf("ex_gt", [128, D], F32)
                bc_load(t, modD[l], modD[l].ap[S.si:S.si + 1, 5 * D:6 * D], D)
                gtb[S.si] = t
            wgp = Pool(k, "ex_wg", 2, [128, 8, D], BF16)
            wup = Pool(k, "ex_wu", 2, [128, 8, D], BF16)
            wdp = Pool(k, "ex_wd", 2, [128, 8, D], BF16)
            CAPM = max(S.cap for S in streams)
            xeTp = Pool(k, "ex_xeT", 2, [128, 8, CAPM], BF16)
            hT = k.sbuf("ex_hT", [128, 8, CAPM], BF16)
            xep = Pool(k, "ex_xe", 3, [128, D], BF16)
            yp = Pool(k, "ex_y", 3, [128, D], F32)
            sap = Pool(k, "ex_sa", 2, [128, 512], F32)
            ixp = Pool(k, "ex_ix", 3, [128, 8], I32)
            gap = Pool(k, "ex_ga", 3, [128, 8], F32)
            psT = Pool(k, "ex_psT", 1, [128, 8, 128], BF16, "psum")
            psA = Pool(k, "ex_psA", 2, [128, 512], F32, "psum")
            psU = Pool(k, "ex_psU", 2, [128, 512], F32, "psum")
            psY = Pool(k, "ex_psY", 3, [128, 512], F32, "psum")
            items = [(e_, S) for e_ in range(NE) for S in streams]
            wts = {}

            def load_w(e_):
                wg = wgp.next(); wu = wup.next(); wd = wdp.next()
                for wt, src in ((wg, w_gate), (wu, w_up), (wd, w_down)):
                    k.dma("pool", wt[:], src.ap[l, e_].rearrange("(c p) f -> p c f", p=128), reads=[src], writes=[wt])
                wts[e_] = (wg, wu, wd)

            def stage_g(e_, S):
                cap_ = S.cap
                P_ = min(128, cap_)
                ntl = max(1, cap_ // 128)
                ix = ixp.next(); ga = gap.next(); xeT = xeTp.next()
                k.dma("sp", ix[:P_, :ntl], S.idxD.ap[e_, :].rearrange("(p t) -> p t", t=ntl), reads=[S.idxD], writes=[ix])
                k.dma("sp", ga[:P_, :ntl], S.gateD.ap[e_, :].rearrange("(p t) -> p t", t=ntl), reads=[S.gateD], writes=[ga])
                k.op("dve", lambda e, ix=ix, P_=P_, ntl=ntl, hi_=S.n - 1: e.tensor_scalar(out=ix[:P_, :ntl], in0=ix[:P_, :ntl], scalar1=hi_, scalar2=0, op0=ALU.min, op1=ALU.max), reads=[ix], writes=[ix])
                for t_ in range(ntl):
                    xe = xep.next()
                    k.dma_raw("pool", lambda e, xe=xe, ix=ix, t_=t_, S=S, P_=P_: e.indirect_dma_start(out=xe[:P_, :], out_offset=None, in_=S.h2D.ap.ap(), in_offset=bass.IndirectOffsetOnAxis(ap=ix[:P_, t_:t_ + 1], axis=0)), reads=[S.h2D, ix], writes=[xe])
                    pt = psT.next()
                    for c in range(8):
                        k.op("pe", lambda e, c=c, pt=pt, xe=xe, P_=P_: e.transpose(out=pt[:, c, :P_], in_=xe[:P_, c * 128:(c + 1) * 128], identity=identb[:P_, :P_]), reads=[xe, identb], writes=[pt], signal=(c == 7))
                    k.op("act", lambda e, pt=pt, t_=t_, P_=P_, xeT=xeT: e.activation(out=xeT[:, :, t_ * 128:t_ * 128 + P_], in_=pt[:, :, :P_], func=AF.Copy), reads=[pt], writes=[xeT])
                return (ix, ga, xeT)

            def stage_u(e_, S, ctx_):
                ix, ga, xeT = ctx_
                wg, wu, wd = wts[e_]
                cap_ = S.cap
                NB = min(512, cap_)
                for fc in range(8):
                    fs = slice(fc * 128, (fc + 1) * 128)
                    for nb in range(cap_ // NB):
                        ns = slice(nb * NB, (nb + 1) * NB)
                        pa = psA.next(); pu = psU.next(); sa = sap.next()
                        for c in range(8):
                            mmul(pa[:, :NB], wg[:, c, fs], xeT[:, c, ns], c == 0, c == 7, [wg, xeT], [pa])
                        for c in range(8):
                            mmul(pu[:, :NB], wu[:, c, fs], xeT[:, c, ns], c == 0, c == 7, [wu, xeT], [pu])
                        k.op("act", lambda e, sa=sa, pa=pa, NB=NB: e.activation(out=sa[:, :NB], in_=pa[:, :NB], func=AF.Silu), reads=[pa], writes=[sa])
                        k.op("dve", lambda e, sa=sa, pu=pu, fc=fc, ns=ns, NB=NB: e.tensor_tensor(out=hT[:, fc, ns], in0=pu[:, :NB], in1=sa[:, :NB], op=ALU.mult), reads=[pu, sa], writes=[hT])

            def stage_d(e_, S, ctx_):
                ix, ga, xeT = ctx_
                wg, wu, wd = wts[e_]
                cap_ = S.cap
                P_ = min(128, cap_)
                ntl = max(1, cap_ // 128)
                Xn = S.Xnext
                for t_ in range(ntl):
                    y = yp.next()
                    for half in range(2):
                        hs = slice(half * 512, (half + 1) * 512)
                        py = psY.next()
                        for fc in range(8):
                            mmul(py[:P_, :], hT[:, fc, t_ * 128:t_ * 128 + P_], wd[:, fc, hs], fc == 0, fc == 7, [hT, wd], [py])
                        k.op("dve", lambda e, y=y, py=py, ga=ga, t_=t_, hs=hs, S=S, P_=P_: e.scalar_tensor_tensor(out=y[:P_, hs], in0=py[:P_, :], scalar=ga[:P_, t_:t_ + 1], in1=gtb[S.si][:P_, hs], op0=ALU.mult, op1=ALU.mult), reads=[py, ga, gtb[S.si]], writes=[y])
                    k.dma_raw("pool", lambda e, y=y, ix=ix, t_=t_, Xn=Xn, P_=P_: e.indirect_dma_start(out=Xn.ap.ap(), out_offset=bass.IndirectOffsetOnAxis(ap=ix[:P_, t_:t_ + 1], axis=0), in_=y[:P_, :], in_offset=None, compute_op=ALU.add), reads=[y, ix], writes=[Xn], acc_w=(t_ > 0))

            load_w(0)
            ctxs = {0: stage_g(*items[0])}
            for i_, (e_, S) in enumerate(items):
                if S is streams[0] and e_ + 1 < NE:
                    load_w(e_ + 1)
                stage_u(e_, S, ctxs[i_])
                if i_ + 1 < len(items):
                    ctxs[i_ + 1] = stage_g(*items[i_ + 1])
                stage_d(e_, S, ctxs[i_])

    PH = set(dbg_phases) if dbg_phases is not None else None
    def on(name):
        return PH is None or name in PH
    SX.Xcur, SC.Xcur = x_in, ctx_in
    for l in range(L):
        last = l == L - 1
        for S_ in (SX, SC):
            S_.Xin = S_.Xcur
            S_.Xnext = S_.XA if S_.Xcur is not S_.XA else S_.XB
        if on("mod"):
            mod_phase(l)
        if on("norm1"):
            norm_phase(SX, l, 1); norm_phase(SC, l, 1)
        if on("inproj"):
            inproj_phase([(SX, False), (SC, last)], l)
        if on("gmlp"):
            gmlp_phase(SX, l)
            if not last:
                gmlp_phase(SC, l)
        if on("conv"):
            conv_phase(SX, l)
            if not last:
                conv_phase(SC, l)
        if on("fourier"):
            fourier_phase(SX, l)
            if not last:
                fourier_phase(SC, l)
        if on("attn"):
            attention_phase(SX, SC, l)
            if not last:
                attention_phase(SC, SC, l)
        if on("merge"):
            merge_phase(SX, l, SX.Xcur, SX.Xnext)
            if not last:
                merge_phase(SC, l, SC.Xcur, SC.Xnext)
        if on("ffn"):
            strs = [SX] if last else [SX, SC]
            for S_ in strs:
                S_.Xin = S_.Xnext
                norm_phase(S_, l, 2)
                route_phase(S_, l)
            if on("experts"):
                experts_phase(l, strs)
        for S_ in (SX, SC):
            S_.Xcur = S_.Xnext
        if PH is not None:
            break
    SX.Xin = SX.Xcur if PH is None else x_in
    norm_phase(SX, 0, 3)
    k.finish([OUT])
    k.emit()
    st.close()
    return nc, list(ins.keys())


def _prep_inputs(inp, n, L=2):
    f = lambda a: np.ascontiguousarray(np.asarray(a, dtype=np.float32))
    ulist, qtiles = _attn_geometry(n)
    cs, f1x, f2, twx = _dft_consts(n)
    _, f1c, _, twc = _dft_consts(NCTX)
    blk, low, iota = _route_consts()
    w_in = f(inp["w_in"])
    i = np.arange(256)
    perm = np.where((i % 32) < 16, i + 16, i - 16)
    w_qkp = np.concatenate([w_in[:, :, C_END:Q_END][:, :, perm], w_in[:, :, Q_END:K_END][:, :, perm]], axis=2)
    pp = np.arange(128)
    common = {
        "w_mod": f(inp["w_mod"]), "b_mod": f(inp["b_mod"]), "norm1_g": f(inp["norm1_g"]), "norm2_g": f(inp["norm2_g"]),
        "final_norm_g": f(inp["final_norm_g"]).reshape(1, D), "w_in": w_in, "w_qkp": np.ascontiguousarray(w_qkp),
        "sgu_ln_g": f(inp["sgu_ln_g"]), "sgu_ln_b": f(inp["sgu_ln_b"]),
        "wsT": np.ascontiguousarray(f(inp["w_spatial"]).transpose(0, 3, 1, 2)),
        "b_spatial": f(inp["b_spatial"]),
        "w_a_out": f(inp["w_a_out"]), "w_b_out": f(inp["w_b_out"]), "w_c_out": f(inp["w_c_out"]), "w_d_out": f(inp["w_d_out"]),
        "conv_wT": np.ascontiguousarray(f(inp["conv_w"]).transpose(0, 2, 1)), "conv_b_c": f(inp["conv_b"])[:, :, None].copy(),
        "conv_ln_g_c": f(inp["conv_ln_g"])[:, :, None].copy(), "conv_ln_b_c": f(inp["conv_ln_b"])[:, :, None].copy(),
        "w_out": f(inp["w_out"]), "w_router": f(inp["w_router"]),
        "w_gate_e": f(inp["w_gate_e"]), "w_up_e": f(inp["w_up_e"]), "w_down_e": f(inp["w_down_e"]),
        "id_bf": np.eye(128).astype(bf16_np), "id_f": np.eye(128, dtype=np.float32),
        "rope": _rope_tables(n),
        "biasT": np.stack([_bias_tiles(f(inp["rpb"])[l], ulist) for l in range(L)]),
        "dft_cs": cs, "dft_f1x": f1x, "dft_f1c": f1c, "dft_f2": f2, "dft_twx": twx, "dft_twc": twc,
        "r_blk": blk, "r_low": low, "r_iota": iota,
        "tid_x": (np.arange(n // 128)[None, :] * 128 + pp[:, None]).astype(np.float32),
        "tid_c": (np.arange(2)[None, :] * 128 + pp[:, None]).astype(np.float32),
        "cctx_pk": np.ascontiguousarray(f(inp["c_ctx"]).reshape(8, 128).T),
    }
    maps = []
    B = inp["x"].shape[0]
    for b in range(B):
        m = dict(common)
        m["x"] = f(inp["x"][b]); m["ctx"] = f(inp["ctx"][b])
        m["c_pk"] = np.ascontiguousarray(f(inp["c"][b]).reshape(8, 128).T)
        maps.append(m)
    return maps


_CACHE = {}


def kernel(**inputs):
    n = inputs["x"].shape[1]
    B = inputs["x"].shape[0]
    if n not in _CACHE:
        _CACHE[n] = build_program(n)
    nc, names = _CACHE[n]
    maps = _prep_inputs(inputs, n)
    maps = [{kk: m[kk] for kk in names} for m in maps]
    res = run_bass_kernel_spmd(nc, maps, core_ids=list(range(B)))
    return np.stack([np.asarray(r["out"], dtype=np.float32) for r in res.results], axis=0)
```

```python
import numpy as np
import ml_dtypes
from contextlib import ExitStack
import concourse.bass as bass
import concourse.mybir as mybir
from concourse.bass_utils import run_bass_kernel_spmd

F32 = mybir.dt.float32
BF16 = mybir.dt.bfloat16
I32 = mybir.dt.int32
U32 = mybir.dt.uint32
AF = mybir.ActivationFunctionType
ALU = mybir.AluOpType
AX = mybir.AxisListType

N_DMA_SEMS = 40
N_SW_SEMS = 8


class T:
    def __init__(self, ap, name=""):
        self.ap = ap
        self.name = name
        self.w = []
        self.wb = []
        self.r = []

    def __getitem__(self, idx):
        return self.ap[idx]


class K:
    ENGS = ["pe", "act", "dve", "pool", "sp"]

    def __init__(self, nc, stack):
        self.nc = nc
        self.stack = stack
        self.streams = {e: [] for e in self.ENGS}
        self.sems = {}
        for e in self.ENGS:
            self.sems[e] = stack.enter_context(nc.semaphore("c_" + e))
        self.count = {e: 0 for e in self.ENGS}
        self.dma_sems = [stack.enter_context(nc.semaphore("d%d" % i)) for i in range(N_DMA_SEMS)]
        self.dma_val = [0] * N_DMA_SEMS
        self.dma_rr = 0
        self.dma_rr_sw = 0
        self.known = {e: {} for e in self.ENGS}
        self.same_engine_sync = True
        self.n_inst = 0

    def sbuf(self, name, shape, dtype):
        self.n_alloc = getattr(self, "n_alloc", 0) + 1
        name = "%s_%d" % (name, self.n_alloc)
        t = self.stack.enter_context(self.nc.sbuf_tensor(name, list(shape), dtype))
        return T(t, name)

    def psum(self, name, shape, dtype):
        self.n_alloc = getattr(self, "n_alloc", 0) + 1
        name = "%s_%d" % (name, self.n_alloc)
        t = self.stack.enter_context(self.nc.psum_tensor(name, list(shape), dtype))
        return T(t, name)

    def dram(self, name, shape, dtype, kind="Internal"):
        t = self.nc.dram_tensor(name, list(shape), dtype, kind=kind)
        return T(t, name)

    def _sem_of(self, key):
        return self.sems[key] if isinstance(key, str) else self.dma_sems[key]

    def _wait(self, eng, dep):
        key, val = dep
        if key == eng and (not self.same_engine_sync or eng in ("pe", "sp")):
            return
        kn = self.known[eng]
        if kn.get(key, 0) >= val:
            return
        kn[key] = val
        self.streams[eng].append(("wait", key, val))

    def _deps(self, eng, reads, writes, acc_w=False):
        for t in reads:
            for d in t.w:
                self._wait(eng, d)
        for t in writes:
            for d in (t.wb if acc_w else t.w):
                self._wait(eng, d)
            for d in t.r:
                self._wait(eng, d)

    def _mark(self, dep, reads, writes, acc_w=False):
        for t in reads:
            t.r.append(dep)
            if len(t.r) > 64:
                t.r = self._compress(t.r)
        for t in writes:
            if acc_w:
                t.w.append(dep)
                if len(t.w) > 64:
                    t.w = self._compress(t.w)
            else:
                t.w = [dep]
                t.wb = [dep]
            t.r = []

    @staticmethod
    def _compress(deps):
        m = {}
        for k, v in deps:
            if m.get(k, 0) < v:
                m[k] = v
        return list(m.items())

    def op(self, eng, fn, reads=(), writes=(), signal=True):
        self._deps(eng, reads, writes)
        if signal:
            self.count[eng] += 1
            dep = (eng, self.count[eng])
            self.streams[eng].append(("inst", fn, eng, 1))
        else:
            dep = (eng, self.count[eng] + 1)
            self.streams[eng].append(("inst", fn, None, 0))
        self._mark(dep, reads, writes)
        self.n_inst += 1

    def dma(self, q, out, in_, reads=(), writes=(), acc_w=False, **kw):
        self._deps(q, reads, writes, acc_w=acc_w)
        self._throttle(q)
        s = self._next_sem(q)
        if self.dma_val[s] > 0:
            self._wait(q, (s, self.dma_val[s]))
        self.dma_val[s] += 16
        dep = (s, self.dma_val[s])

        def fn(e, out=out, in_=in_, kw=kw):
            return e.dma_start(out=out, in_=in_, **kw)
        self.streams[q].append(("inst", fn, s, 16))
        self._mark(dep, reads, writes, acc_w=acc_w)
        self.n_inst += 1
        if q == "pool":
            self.__dict__.setdefault("pool_out", []).append(dep)
        return dep

    def _next_sem(self, q):
        if q == "pool":
            s = self.dma_rr_sw
            self.dma_rr_sw = (self.dma_rr_sw + 1) % N_SW_SEMS
            return s
        s = N_SW_SEMS + self.dma_rr
        self.dma_rr = (self.dma_rr + 1) % (N_DMA_SEMS - N_SW_SEMS)
        return s

    def _throttle(self, q):
        if q != "pool":
            return
        po = self.__dict__.setdefault("pool_out", [])
        if len(po) >= 2:
            self._wait(q, po[-2])

    def dma_raw(self, q, fn, reads=(), writes=(), acc_w=False):
        self._deps(q, reads, writes, acc_w=acc_w)
        self._throttle(q)
        s = self._next_sem(q)
        if self.dma_val[s] > 0:
            self._wait(q, (s, self.dma_val[s]))
        self.dma_val[s] += 16
        dep = (s, self.dma_val[s])
        self.streams[q].append(("inst", fn, s, 16))
        self._mark(dep, reads, writes, acc_w=acc_w)
        if q == "pool":
            self.__dict__.setdefault("pool_out", []).append(dep)
        return dep

    def finish(self, out_tiles):
        for t in out_tiles:
            for d in t.w:
                self._wait("sp", d)
        for e in self.ENGS:
            if e != "sp" and self.count[e] > 0:
                self._wait("sp", (e, self.count[e]))
        for s in range(N_DMA_SEMS):
            if self.dma_val[s] > 0:
                self._wait("sp", (s, self.dma_val[s]))

    def emit(self):
        nc = self.nc
        engmap = {"pe": "tensor", "act": "scalar", "dve": "vector", "pool": "gpsimd", "sp": "sync"}
        with nc.Block() as block:
            for e in self.ENGS:
                stream = self.streams[e]
                if not stream:
                    continue

                def body(eng, stream=stream):
                    for item in stream:
                        if item[0] == "wait":
                            eng.wait_ge(self._sem_of(item[1]), item[2])
                        else:
                            _, fn, key, inc = item
                            ins = fn(eng)
                            if key is not None:
                                ins.then_inc(self._sem_of(key), inc)
                getattr(block, engmap[e])(body)


class Pool:
    def __init__(self, k, name, n, shape, dtype, space="sbuf"):
        mk = k.sbuf if space == "sbuf" else k.psum
        self.tiles = [mk("%s%d" % (name, i), shape, dtype) for i in range(n)]
        self.i = 0

    def next(self):
        t = self.tiles[self.i]
        self.i = (self.i + 1) % len(self.tiles)
        return t


def _k_phase(self):
    class _Ph:
        def __init__(s, k):
            s.k = k
        def __enter__(s):
            s.prev = s.k.stack
            s.st = ExitStack()
            s.st.__enter__()
            s.k.stack = s.st
            return s
        def __exit__(s, *a):
            s.k.barrier()
            s.k.stack = s.prev
            return s.st.__exit__(*a)
    return _Ph(self)


def _k_barrier(self):
    for e in self.ENGS:
        for e2 in self.ENGS:
            if e2 != e and self.count[e2] > 0:
                self._wait(e, (e2, self.count[e2]))
        for s in range(N_DMA_SEMS):
            if self.dma_val[s] > 0:
                self._wait(e, (s, self.dma_val[s]))


K.phase = _k_phase
K.barrier = _k_barrier


D = 1024
W = 256
NCTX = 256
NE = 16
GW = 64
bf16_np = ml_dtypes.bfloat16
NEG = -30000.0


def _rope_tables(n):
    t = np.arange(n)
    rows, cols = t // GW, t % GW
    inv = 10000.0 ** (-np.arange(16, dtype=np.float64) / 16)
    d = np.arange(128) % 64
    pos = np.where(d[:, None] < 32, rows[None, :], cols[None, :]).astype(np.float64)
    ang = pos * inv[d % 16][:, None]
    cos, sin = np.cos(ang), np.sin(ang)
    sgn = np.where((d % 32) < 16, -1.0, 1.0)[:, None]
    return np.stack([cos / 8, sin * sgn / 8, cos, sin * sgn]).astype(np.float32)


def _attn_geometry(n):
    R = n // GW
    wr = min(8, R)
    s = lambda r: int(np.clip(r - wr // 2, 0, R - wr))
    cst = np.clip(np.arange(GW) - 8, 0, GW - 16)
    uniq = {}
    ulist = []
    qtiles = []
    kc = np.tile(np.arange(GW), 2)
    ka = np.repeat(np.arange(2), GW)
    for qt in range(R // 2):
        r0 = 2 * qt
        lo, hi = s(r0), s(r0 + 1) + wr - 1
        lst = []
        for kt in range(lo // 2, hi // 2 + 1):
            kr = 2 * kt + ka
            qr = r0 + ka
            qc = kc
            srow = np.array([s(r) for r in qr])
            ok_r = (kr[:, None] >= srow[None, :]) & (kr[:, None] < srow[None, :] + wr)
            ok_c = (kc[:, None] >= cst[qc][None, :]) & (kc[:, None] < cst[qc][None, :] + 16)
            mask = ok_r & ok_c
            if not mask.any():
                continue
            dr = np.clip(kr[:, None] - qr[None, :] + 7, 0, 14)
            dc = np.clip(kc[:, None] - qc[None, :], -15, 15) + 15
            key = (2 * kt - r0, mask.tobytes())
            if key not in uniq:
                uniq[key] = len(ulist)
                ulist.append((dr, dc, mask))
            lst.append((kt, uniq[key]))
        qtiles.append(lst)
    return ulist, qtiles


def _bias_tiles(rpb_l, ulist):
    out = np.empty((4, len(ulist), 128, 128), np.float32)
    for u, (dr, dc, mask) in enumerate(ulist):
        for h in range(4):
            out[h, u] = np.where(mask, rpb_l[h][dr, dc], NEG)
    return np.ascontiguousarray(out.transpose(2, 0, 1, 3).reshape(128, 4 * len(ulist), 128)).astype(bf16_np)


def _dft_consts(n):
    N1 = n // 128
    sc = 1.0 / 8.0
    dd = np.arange(64)
    ph = 2 * np.pi * np.outer(dd, dd) / 64
    Cd, Sd = np.cos(ph) * sc, np.sin(ph) * sc
    cs = np.zeros((128, 256))
    for g in range(2):
        cs[g * 64:(g + 1) * 64, g * 64:(g + 1) * 64] = Cd
        cs[g * 64:(g + 1) * 64, 128 + g * 64:128 + (g + 1) * 64] = -Sd
    c1 = np.arange(N1)
    p1 = 2 * np.pi * np.outer(c1, c1) / N1
    f1 = np.stack([np.cos(p1), np.sin(p1), -np.sin(p1)]) / np.sqrt(float(n))
    pp = np.arange(128)
    p2 = 2 * np.pi * np.outer(pp, pp) / 128
    f2 = np.stack([np.cos(p2), np.sin(p2)])
    pt = 2 * np.pi * np.outer(pp, c1) / n
    tw = np.stack([np.cos(pt), -np.sin(pt)]).astype(np.float32)
    return cs.astype(bf16_np), f1.astype(bf16_np), f2.astype(bf16_np), tw


def _route_consts():
    p = np.arange(128)
    same = (p[:, None] // 8) == (p[None, :] // 8)
    blk = same.astype(np.float32)
    low = (same & (p[:, None] < p[None, :])).astype(np.float32)
    iota = np.broadcast_to(np.arange(32, dtype=np.float32), (128, NE, 32)).copy()
    return blk, low, iota


A_END, B_END, C_END, Q_END, K_END, V_END = 512, 768, 1280, 1536, 1792, 2048
IN_COLS = 6144
GELU_C = 1.5957691216057308


class Stream:
    pass


def build_program(n, L=2, dbg=(), dbg_phases=None):
    nc = bass.Bass("TRN2", target_bir_lowering=False)
    N1 = n // 128
    cap = 2 * n // NE
    capc = 2 * NCTX // NE
    st = ExitStack()
    k = K(nc, st)
    ins = {}

    def ext(name, shape, dtype=F32):
        h = nc.dram_tensor(name, list(shape), dtype, kind="ExternalInput")
        ins[name] = T(h, name)
        return ins[name]

    def scr(name, shape, dtype):
        kind = "ExternalOutput" if name in dbg else "Internal"
        h = nc.dram_tensor(name, list(shape), dtype, kind=kind)
        return T(h, name)

    x_in = ext("x", [n, D]); ctx_in = ext("ctx", [NCTX, D])
    c_in = ext("c_pk", [128, 8]); cc_in = ext("cctx_pk", [128, 8])
    w_mod = ext("w_mod", [L, D, 6 * D]); b_mod = ext("b_mod", [L, 6 * D])
    n1g = ext("norm1_g", [L, D]); n2g = ext("norm2_g", [L, D]); fng = ext("final_norm_g", [1, D])
    w_in = ext("w_in", [L, D, IN_COLS]); w_qkp = ext("w_qkp", [L, D, 512])
    sgu_g = ext("sgu_ln_g", [L, W]); sgu_b = ext("sgu_ln_b", [L, W])
    wsT = ext("wsT", [L, 128, 4, 128]); bsp = ext("b_spatial", [L, 4, 128])
    w_br = [ext(nm, [L, W, D]) for nm in ("w_a_out", "w_b_out", "w_c_out", "w_d_out")]
    conv_wT = ext("conv_wT", [L, W, 31]); conv_b = ext("conv_b_c", [L, W, 1])
    cln_g = ext("conv_ln_g_c", [L, W, 1]); cln_b = ext("conv_ln_b_c", [L, W, 1])
    w_out = ext("w_out", [L, D, D]); w_router = ext("w_router", [L, D, NE])
    w_gate = ext("w_gate_e", [L, NE, D, D]); w_up = ext("w_up_e", [L, NE, D, D]); w_down = ext("w_down_e", [L, NE, D, D])
    id_bf = ext("id_bf", [128, 128], BF16); id_f = ext("id_f", [128, 128])
    rope = ext("rope", [4, 128, n])
    ulist, qtiles = _attn_geometry(n)
    U = len(ulist)
    biasT = ext("biasT", [L, 128, 4 * U, 128], BF16)
    cs_c = ext("dft_cs", [128, 256], BF16)
    f1x = ext("dft_f1x", [3, N1, N1], BF16); f1c = ext("dft_f1c", [3, 2, 2], BF16)
    f2 = ext("dft_f2", [2, 128, 128], BF16)
    twx = ext("dft_twx", [2, 128, N1]); twc = ext("dft_twc", [2, 128, 2])
    r_blk = ext("r_blk", [128, 128]); r_low = ext("r_low", [128, 128]); r_iota = ext("r_iota", [128, NE, 32])
    tidx = ext("tid_x", [128, N1]); tidc = ext("tid_c", [128, 2])
    out_h = nc.dram_tensor("out", [n, D], F32, kind="ExternalOutput")
    OUT = T(out_h, "out")

    def mk_stream(si, nn, name, xin):
        S = Stream()
        S.si, S.n, S.name, S.N1 = si, nn, name, nn // 128
        S.cap = 2 * nn // NE
        S.Xin = xin
        S.XA = scr(name + "_XA", [nn, D], F32); S.XB = scr(name + "_XB", [nn, D], F32)
        S.hT = scr(name + "_hT", [D, nn], BF16)
        for nm in ("uT", "yT", "qrT", "qpT", "krT", "aT", "bT", "cT", "dT"):
            setattr(S, nm, scr(name + "_" + nm, [W, nn], BF16))
        for nm in ("vln", "Zr", "Zi", "v"):
            setattr(S, nm, scr(name + "_" + nm, [nn, W], BF16))
        S.gT = scr(name + "_gT", [4 * D, nn], BF16)
        S.Y1r = scr(name + "_Y1r", [S.N1, 128 * W], BF16); S.Y1i = scr(name + "_Y1i", [S.N1, 128 * W], BF16)
        S.h2D = scr(name + "_h2D", [nn, D], BF16)
        S.affD = scr(name + "_affD", [NE, nn], F32)
        S.slotD = scr(name + "_slotD", [NE, nn], F32)
        S.idxD = scr(name + "_idxD", [NE, S.cap], I32)
        S.gateD = scr(name + "_gateD", [NE, S.cap], F32)
        S.f1 = f1x if si == 0 else f1c
        S.tw = twx if si == 0 else twc
        S.tid = tidx if si == 0 else tidc
        return S

    SX = mk_stream(0, n, "sx", x_in)
    SC = mk_stream(1, NCTX, "sc", ctx_in)
    modD = [scr("modD%d" % l, [2, 6 * D], F32) for l in range(L)]

    def mmul(out, lhsT, rhs, start, stop, reads, writes):
        k.op("pe", lambda e: e.matmul(out, lhsT=lhsT, rhs=rhs, start=start, stop=stop), reads=reads, writes=writes, signal=bool(stop))

    def load_ident(bf=True, f=False):
        r = []
        if bf:
            t = k.sbuf("identb", [128, 128], BF16); k.dma("sp", t[:], id_bf.ap.ap(), reads=[id_bf], writes=[t]); r.append(t)
        if f:
            t = k.sbuf("identf", [128, 128], F32); k.dma("sp", t[:], id_f.ap.ap(), reads=[id_f], writes=[t]); r.append(t)
        return r

    def mod_phase(l):
        with k.phase():
            cs = k.sbuf("mod_cs", [128, 2, 8], F32)
            k.dma("sp", cs[:, 0, :], c_in.ap.ap(), reads=[c_in], writes=[cs])
            k.dma("sp", cs[:, 1, :], cc_in.ap.ap(), reads=[cc_in], writes=[cs])
            scs = k.sbuf("mod_scs", [128, 2, 8], F32)
            k.op("act", lambda e: e.activation(out=scs[:], in_=cs[:], func=AF.Silu), reads=[cs], writes=[scs])
            wp = Pool(k, "mod_w", 4, [128, 8, 512], F32)
            pp = Pool(k, "mod_ps", 2, [2, 512], F32, "psum")
            bp = Pool(k, "mod_b", 4, [2, 512], F32)
            rp = Pool(k, "mod_r", 4, [2, 512], F32)
            ldq = {}
            def mod_loads(blk):
                wm = wp.next(); bm = bp.next()
                cols = slice(blk * 512, (blk + 1) * 512)
                k.dma("sp", wm[:], w_mod.ap[l, :, cols].rearrange("(c p) n -> p c n", p=128), reads=[w_mod], writes=[wm])
                k.dma("sp", bm[:], b_mod.ap[l:l + 1, cols].to_broadcast([2, 512]), reads=[b_mod], writes=[bm])
                ldq[blk] = (wm, bm)
            for b0 in range(3):
                mod_loads(b0)
            for blk in range(12):
                if blk + 3 < 12:
                    mod_loads(blk + 3)
                wm, bm = ldq.pop(blk)
                ps = pp.next(); rs = rp.next()
                cols = slice(blk * 512, (blk + 1) * 512)
                for c in range(8):
                    mmul(ps[:], scs[:, :, c], wm[:, c, :], c == 0, c == 7, [scs, wm], [ps])
                k.op("dve", lambda e, rs=rs, ps=ps, bm=bm: e.tensor_tensor(out=rs[:], in0=ps[:], in1=bm[:], op=ALU.add), reads=[ps, bm], writes=[rs])
                k.dma("sp", modD[l].ap[:, cols], rs[:], reads=[rs], writes=[modD[l]], acc_w=True)

    def bc_load(dst, src_t, row_ap, F):
        k.dma("sp", dst[:], row_ap.to_broadcast([128, F]), reads=[src_t], writes=[dst])

    def norm_phase(S, l, kind):
        nn, si = S.n, S.si
        with k.phase():
            scale_b = k.sbuf("nm_scale", [128, D], F32)
            if kind == 3:
                bc_load(scale_b, fng, fng.ap[0:1, :], D)
            else:
                g = n1g if kind == 1 else n2g
                o = 0 if kind == 1 else 3
                gb = k.sbuf("nm_g", [128, D], F32); scb = k.sbuf("nm_sc", [128, D], F32)
                shift_b = k.sbuf("nm_shift", [128, D], F32)
                bc_load(gb, g, g.ap[l:l + 1, :], D)
                bc_load(scb, modD[l], modD[l].ap[si:si + 1, (o + 1) * D:(o + 2) * D], D)
                bc_load(shift_b, modD[l], modD[l].ap[si:si + 1, o * D:(o + 1) * D], D)
                k.op("dve", lambda e: e.scalar_tensor_tensor(out=scale_b[:], in0=scb[:], scalar=1.0, in1=gb[:], op0=ALU.add, op1=ALU.mult), reads=[scb, gb], writes=[scale_b])
            if kind == 1:
                identb, = load_ident(True, False)
            if kind == 2:
                identf, = load_ident(False, True)
                rt = k.sbuf("nm_router", [128, 8, NE], F32)
                k.dma("sp", rt[:], w_router.ap[l].rearrange("(c p) e -> p c e", p=128), reads=[w_router], writes=[rt])
                ones16 = k.sbuf("nm_ones16", [NE, NE], F32)
                k.op("dve", lambda e: e.memset(ones16[:], 1.0), writes=[ones16])
                aff_all = k.sbuf("nm_aff", [NE, nn], F32)
            xp = Pool(k, "nm_x", 5, [128, D], F32)
            jp = Pool(k, "nm_junk", 2, [128, D], BF16)
            sp_ = Pool(k, "nm_st", 3, [128, 4], F32)
            tp = Pool(k, "nm_tmp", 3, [128, D], F32)
            hp = Pool(k, "nm_h", 3, [128, D], BF16 if kind == 1 else F32)
            if kind == 1:
                psT = Pool(k, "nm_psT", 2, [128, 8, 128], BF16, "psum")
                hTp = Pool(k, "nm_hT", 2, [128, 8, 128], BF16)
            if kind == 2:
                hbp = Pool(k, "nm_hb", 2, [128, D], BF16)
                psT = Pool(k, "nm_psT", 2, [128, 4, 128], F32, "psum")
                hTp = Pool(k, "nm_hT", 2, [128, 8, 128], F32)
                psl = Pool(k, "nm_psl", 2, [NE, 512], F32, "psum")
                rstate = {}
                e_p = Pool(k, "nm_e", 2, [NE, 512], F32)
                r_p = Pool(k, "nm_r", 2, [NE, 512], F32)
            xq = {}
            def stage0(i):
                    xt = xp.next()
                    k.dma("sp", xt[:], S.Xin.ap[i * 128:(i + 1) * 128, :], reads=[S.Xin], writes=[xt])
                    xq[i] = xt
            def stage1(i):
                    rows = slice(i * 128, (i + 1) * 128)
                    xt = xq.pop(i); jk = jp.next(); s4 = sp_.next(); tmp = tp.next(); h = hp.next()
                    k.op("act", lambda e, xt=xt, jk=jk, s4=s4: e.activation(out=jk[:], in_=xt[:], func=AF.Square, accum_out=s4[:, 0:1]), reads=[xt], writes=[jk, s4])
                    xs = tmp
                    if kind != 3:
                        k.op("pool", lambda e, xt=xt, xs=xs: e.tensor_tensor(out=xs[:], in0=xt[:], in1=scale_b[:], op=ALU.mult), reads=[xt, scale_b], writes=[xs])
                    k.op("act", lambda e, s4=s4: e.activation(out=s4[:, 1:2], in_=s4[:, 0:1], func=AF.Sqrt, bias=1e-6, scale=1.0 / D), reads=[s4], writes=[s4])
                    k.op("dve", lambda e, s4=s4: e.reciprocal(out=s4[:, 2:3], in_=s4[:, 1:2]), reads=[s4], writes=[s4])
                    if kind == 3:
                        k.op("dve", lambda e, xt=xt, s4=s4, h=h: e.scalar_tensor_tensor(out=h[:], in0=xt[:], scalar=s4[:, 2:3], in1=scale_b[:], op0=ALU.mult, op1=ALU.mult), reads=[xt, s4, scale_b], writes=[h])
                        k.dma("sp", OUT.ap[rows, :], h[:], reads=[h], writes=[OUT], acc_w=True)
                        return None
                    k.op("dve", lambda e, tmp=tmp, xs=xs, s4=s4, h=h: e.scalar_tensor_tensor(out=h[:], in0=xs[:], scalar=s4[:, 2:3], in1=shift_b[:], op0=ALU.mult, op1=ALU.add), reads=[xs, s4, shift_b], writes=[h])
                    return (rows, h)
            def stage2(ctx_):
                    rows, h = ctx_
                    if kind == 1:
                        pt = psT.next(); hT = hTp.next()
                        for c in range(8):
                            k.op("pe", lambda e, c=c, pt=pt, h=h: e.transpose(out=pt[:, c, :], in_=h[:, c * 128:(c + 1) * 128], identity=identb[:]), reads=[h, identb], writes=[pt], signal=(c == 7))
                        k.op("act", lambda e, pt=pt, hT=hT: e.activation(out=hT[:], in_=pt[:], func=AF.Copy), reads=[pt], writes=[hT])
                        k.dma("sp", S.hT.ap.ap().rearrange("(c p) t -> p c t", p=128)[:, :, rows], hT[:], reads=[hT], writes=[S.hT], acc_w=True)
                    else:
                        hb = hbp.next(); hT = hTp.next()
                        k.op("act", lambda e, hb=hb, h=h: e.activation(out=hb[:], in_=h[:], func=AF.Copy), reads=[h], writes=[hb])
                        k.dma("sp", S.h2D.ap[rows, :], hb[:], reads=[hb], writes=[S.h2D], acc_w=True)
                        for half in range(2):
                            pt = psT.next()
                            for c in range(4):
                                cc = half * 4 + c
                                k.op("pe", lambda e, c=c, cc=cc, pt=pt, h=h: e.transpose(out=pt[:, c, :], in_=h[:, cc * 128:(cc + 1) * 128], identity=identf[:]), reads=[h, identf], writes=[pt], signal=(c == 3))
                            k.op("dve", lambda e, pt=pt, hT=hT, half=half: e.tensor_copy(out=hT[:, half * 4:(half + 1) * 4, :], in_=pt[:]), reads=[pt], writes=[hT])
                        i_ = rows.start // 128
                        if i_ % 4 == 0:
                            rstate["pl"] = psl.next()
                        pl = rstate["pl"]
                        for c in range(8):
                            mmul(pl[:, (i_ % 4) * 128:(i_ % 4 + 1) * 128], rt[:, c, :], hT[:, c, :], c == 0, c == 7, [rt, hT], [pl])
                        if i_ % 4 == 3 or i_ == nn // 128 - 1:
                            nb_ = (i_ % 4 + 1) * 128
                            c0 = (i_ // 4) * 512
                            ee = e_p.next(); rr = r_p.next()
                            k.op("act", lambda e, pl=pl, ee=ee, nb_=nb_: e.activation(out=ee[:, :nb_], in_=pl[:, :nb_], func=AF.Exp), reads=[pl], writes=[ee])
                            pl2 = psl.next()
                            mmul(pl2[:, :nb_], ones16[:], ee[:, :nb_], True, True, [ones16, ee], [pl2])
                            k.op("dve", lambda e, pl2=pl2, rr=rr, nb_=nb_: e.reciprocal(out=rr[:, :nb_], in_=pl2[:, :nb_]), reads=[pl2], writes=[rr])
                            k.op("dve", lambda e, ee=ee, rr=rr, nb_=nb_, c0=c0: e.tensor_tensor(out=aff_all[:, c0:c0 + nb_], in0=ee[:, :nb_], in1=rr[:, :nb_], op=ALU.mult), reads=[ee, rr], writes=[aff_all])

            pend = None
            NTL = nn // 128
            stage0(0)
            if NTL > 1:
                stage0(1)
            for i in range(NTL):
                if i + 2 < NTL:
                    stage0(i + 2)
                cur = stage1(i)
                if pend is not None:
                    stage2(pend)
                pend = cur
            if pend is not None:
                stage2(pend)
            if kind == 2:
                k.dma("sp", S.affD.ap.ap(), aff_all[:], reads=[aff_all], writes=[S.affD])

    def inproj_phase(specs, l):
        with k.phase():
            wb = k.sbuf("ip_w", [128, 8, IN_COLS], BF16)
            for c in range(8):
                k.dma("pool", wb[:, c, :], w_in.ap[l, c * 128:(c + 1) * 128, :], reads=[w_in], writes=[wb], acc_w=True)
            wp_ = k.sbuf("ip_wp", [128, 8, 512], BF16)
            k.dma("pool", wp_[:], w_qkp.ap[l].rearrange("(c p) n -> p c n", p=128), reads=[w_qkp], writes=[wp_])
            csb = k.sbuf("ip_cs", [128, 256], BF16)
            k.dma("sp", csb[:], cs_c.ap.ap(), reads=[cs_c], writes=[csb])
            lg = k.sbuf("ip_lg", [128, W], F32); lb = k.sbuf("ip_lb", [128, W], F32)
            bc_load(lg, sgu_g, sgu_g.ap[l:l + 1, :], W); bc_load(lb, sgu_b, sgu_b.ap[l:l + 1, :], W)
            for S, kv_only in specs:
              nn, si = S.n, S.si
              TT = 512 if nn >= 512 else nn
              with k.phase():
                hp = Pool(k, "ip_h", 2, [128, 8, TT], BF16)
                ps = Pool(k, "ip_ps", 8, [128, 512], F32, "psum")
                ev = Pool(k, "ip_ev", 4, [128, TT], BF16)
                f32p = Pool(k, "ip_f32", 4, [128, TT], F32)
                rp = Pool(k, "ip_rope", 2, [128, 4, TT], F32)
                zbp = Pool(k, "ip_zb", 2, [128, 2, TT], BF16)
                tmv = Pool(k, "ip_tmv", 3, [128, 512], F32)
                tmb = Pool(k, "ip_tmb", 3, [128, 512], BF16)
                stp = Pool(k, "ip_st", 3, [128, 8], F32)

                def fm(wt, col0, h, reads_w):
                    p = ps.next()
                    for c in range(8):
                        mmul(p[:, :TT], wt[:, c, col0:col0 + 128], h[:, c, :], c == 0, c == 7, [reads_w, h], [p])
                    return p

                def gelu_to(dst_ap, p, width, reads_extra, writes):
                    sq = f32p.next(); t2 = f32p.next()
                    k.op("act", lambda e: e.activation(out=sq[:, :width], in_=p[:, :width], func=AF.Square), reads=[p], writes=[sq])
                    k.op("dve", lambda e: e.tensor_scalar(out=sq[:, :width], in0=sq[:, :width], scalar1=0.044715, scalar2=1.0, op0=ALU.mult, op1=ALU.add), reads=[sq], writes=[sq])
                    k.op("dve", lambda e: e.tensor_tensor(out=t2[:, :width], in0=p[:, :width], in1=sq[:, :width], op=ALU.mult), reads=[p, sq], writes=[t2])
                    k.op("act", lambda e: e.activation(out=t2[:, :width], in_=t2[:, :width], func=AF.Sigmoid, scale=GELU_C), reads=[t2], writes=[t2])
                    k.op("dve", lambda e: e.tensor_tensor(out=dst_ap, in0=p[:, :width], in1=t2[:, :width], op=ALU.mult), reads=[p, t2], writes=writes)

                def ip_loads(j):
                    tk = slice(j * TT, (j + 1) * TT)
                    h = hp.next()
                    k.dma("sp", h[:], S.hT.ap.ap().rearrange("(c p) t -> p c t", p=128)[:, :, tk], reads=[S.hT], writes=[h])
                    rt = None
                    if si == 0:
                        rt = rp.next()
                        k.dma("sp", rt[:], rope.ap.ap().rearrange("f p t -> p f t")[:, :, tk], reads=[rope], writes=[rt])
                    return h, rt
                nxt = ip_loads(0)
                for j in range(nn // TT):
                    tk = slice(j * TT, (j + 1) * TT)
                    h, rt = nxt
                    if j + 1 < nn // TT:
                        nxt = ip_loads(j + 1)
                    fmv = lambda T_: T_.ap.ap().rearrange("(c p) t -> p c t", p=128)
                    if not kv_only:
                        zb = zbp.next()
                        for ch in range(2):
                            p = fm(wb, A_END + ch * 128, h, wb)
                            k.op("act", lambda e, p=p, zb=zb, ch=ch, TT=TT: e.activation(out=zb[:, ch, :], in_=p[:, :TT], func=AF.Copy), reads=[p], writes=[zb])
                    for s_ in range(TT // 128):
                        trow = slice(j * TT + s_ * 128, j * TT + (s_ + 1) * 128)
                        hs = lambda c: h[:, c, s_ * 128:(s_ + 1) * 128]
                        p = ps.next()
                        for c in range(8):
                            mmul(p[:, 0:256], hs(c), wb[:, c, K_END:V_END], c == 0, c == 7, [h, wb], [p])
                        if not kv_only:
                            for c in range(8):
                                mmul(p[:, 256:512], hs(c), wb[:, c, 256:512], c == 0, c == 7, [h, wb], [p])
                        ov = tmb.next()
                        k.op("act", lambda e, ov=ov, p=p: e.activation(out=ov[:, 0:256], in_=p[:, 0:256], func=AF.Copy), reads=[p], writes=[ov])
                        k.dma("sp", S.v.ap[trow, :], ov[:, 0:256], reads=[ov], writes=[S.v], acc_w=True)
                        if kv_only:
                            continue
                        gv = tmv.next(); s8 = stp.next(); ol = tmb.next()
                        sq = f32p.next(); t2 = f32p.next()
                        pv = p
                        k.op("act", lambda e, sq=sq, pv=pv: e.activation(out=sq[:, :256], in_=pv[:, 256:512], func=AF.Square), reads=[pv], writes=[sq])
                        k.op("dve", lambda e, sq=sq: e.tensor_scalar(out=sq[:, :256], in0=sq[:, :256], scalar1=0.044715, scalar2=1.0, op0=ALU.mult, op1=ALU.add), reads=[sq], writes=[sq])
                        k.op("dve", lambda e, sq=sq, t2=t2, pv=pv: e.tensor_tensor(out=t2[:, :256], in0=pv[:, 256:512], in1=sq[:, :256], op=ALU.mult), reads=[pv, sq], writes=[t2])
                        k.op("act", lambda e, t2=t2: e.activation(out=t2[:, :256], in_=t2[:, :256], func=AF.Sigmoid, scale=GELU_C), reads=[t2], writes=[t2])
                        k.op("dve", lambda e, gv=gv, t2=t2, pv=pv: e.tensor_tensor(out=gv[:, :256], in0=pv[:, 256:512], in1=t2[:, :256], op=ALU.mult), reads=[pv, t2], writes=[gv])
                        k.op("dve", lambda e, gv=gv, s8=s8: e.bn_stats(out=s8[:, 0:6], in_=gv[:, :256]), reads=[gv], writes=[s8])
                        k.op("dve", lambda e, s8=s8: e.bn_aggr(out=s8[:, 6:8], in_=s8[:, 0:6]), reads=[s8], writes=[s8])
                        k.op("act", lambda e, s8=s8: e.activation(out=s8[:, 0:1], in_=s8[:, 7:8], func=AF.Sqrt, bias=1e-6, scale=1.0), reads=[s8], writes=[s8])
                        k.op("dve", lambda e, s8=s8: e.reciprocal(out=s8[:, 1:2], in_=s8[:, 0:1]), reads=[s8], writes=[s8])
                        k.op("dve", lambda e, gv=gv, s8=s8: e.tensor_scalar(out=gv[:, :256], in0=gv[:, :256], scalar1=s8[:, 6:7], scalar2=s8[:, 1:2], op0=ALU.subtract, op1=ALU.mult), reads=[gv, s8], writes=[gv])
                        k.op("pool", lambda e, gv=gv: e.tensor_tensor(out=gv[:, :256], in0=gv[:, :256], in1=lg[:], op=ALU.mult), reads=[gv, lg], writes=[gv])
                        k.op("pool", lambda e, gv=gv, ol=ol: e.tensor_tensor(out=ol[:, :256], in0=gv[:, :256], in1=lb[:], op=ALU.add), reads=[gv, lb], writes=[ol])
                        k.dma("sp", S.vln.ap[trow, :], ol[:, :256], reads=[ol], writes=[S.vln], acc_w=True)
                        pz = ps.next()
                        for ch in range(2):
                            mmul(pz[:, ch * 256:(ch + 1) * 256], zb[:, ch, s_ * 128:(s_ + 1) * 128], csb[:], True, True, [zb, csb], [pz])
                        oz = tmb.next()
                        k.op("act", lambda e, oz=oz, pz=pz: e.activation(out=oz[:], in_=pz[:], func=AF.Copy), reads=[pz], writes=[oz])
                        ozv = oz[:].rearrange("p (c r f) -> p c r f", c=2, r=2)
                        k.dma("sp", S.Zr.ap[trow, :].rearrange("t (c f) -> t c f", c=2), ozv[:, :, 0, :], reads=[oz], writes=[S.Zr], acc_w=True)
                        k.dma("sp", S.Zi.ap[trow, :].rearrange("t (c f) -> t c f", c=2), ozv[:, :, 1, :], reads=[oz], writes=[S.Zi], acc_w=True)
                    if not kv_only:
                        for ch in range(2):
                            p = fm(wb, ch * 128, h, wb); o = ev.next()
                            gelu_to(o[:], p, TT, [], [o])
                            k.dma("sp", fmv(S.uT)[:, ch, tk], o[:], reads=[o], writes=[S.uT], acc_w=True)
                        for ch in range(2):
                            pa = fm(wb, B_END + ch * 128, h, wb); pg = fm(wb, B_END + 256 + ch * 128, h, wb)
                            sg = f32p.next(); o = ev.next()
                            k.op("act", lambda e, sg=sg, pg=pg, TT=TT: e.activation(out=sg[:], in_=pg[:, :TT], func=AF.Sigmoid), reads=[pg], writes=[sg])
                            k.op("dve", lambda e, o=o, pa=pa, sg=sg, TT=TT: e.tensor_tensor(out=o[:], in0=pa[:, :TT], in1=sg[:], op=ALU.mult), reads=[pa, sg], writes=[o])
                            k.dma("sp", fmv(S.yT)[:, ch, tk], o[:], reads=[o], writes=[S.yT], acc_w=True)
                    for which, col0, pc0, dstr, dstp in ((0, C_END, 0, S.qrT, S.qpT), (1, Q_END, 256, S.krT, None)):
                        if kv_only and which == 0:
                            continue
                        for ch in range(2):
                            p = fm(wb, col0 + ch * 128, h, wb)
                            if si == 0:
                                pp_ = fm(wp_, pc0 + ch * 128, h, wp_)
                                t1 = f32p.next(); t2 = f32p.next(); o = ev.next()
                                k.op("dve", lambda e, t1=t1, p=p, rt=rt, which=which, TT=TT: e.tensor_tensor(out=t1[:], in0=p[:, :TT], in1=rt[:, 2 * which, :], op=ALU.mult), reads=[p, rt], writes=[t1])
                                k.op("dve", lambda e, t2=t2, pp_=pp_, rt=rt, which=which, TT=TT: e.tensor_tensor(out=t2[:], in0=pp_[:, :TT], in1=rt[:, 2 * which + 1, :], op=ALU.mult), reads=[pp_, rt], writes=[t2])
                                k.op("pool", lambda e, o=o, t1=t1, t2=t2: e.tensor_tensor(out=o[:], in0=t1[:], in1=t2[:], op=ALU.add), reads=[t1, t2], writes=[o])
                                k.dma("sp", fmv(dstr)[:, ch, tk], o[:], reads=[o], writes=[dstr], acc_w=True)
                                if which == 0:
                                    o2 = ev.next()
                                    k.op("act", lambda e, o2=o2, p=p, TT=TT: e.activation(out=o2[:], in_=p[:, :TT], func=AF.Copy, scale=0.125), reads=[p], writes=[o2])
                                    k.dma("sp", fmv(dstp)[:, ch, tk], o2[:], reads=[o2], writes=[dstp], acc_w=True)
                            else:
                                o = ev.next()
                                if which == 0:
                                    k.op("act", lambda e, o=o, p=p, TT=TT: e.activation(out=o[:], in_=p[:, :TT], func=AF.Copy, scale=0.125), reads=[p], writes=[o])
                                    k.dma("sp", fmv(S.qpT)[:, ch, tk], o[:], reads=[o], writes=[S.qpT], acc_w=True)
                                else:
                                    k.op("act", lambda e, o=o, p=p, TT=TT: e.activation(out=o[:], in_=p[:, :TT], func=AF.Copy), reads=[p], writes=[o])
                                    k.dma("sp", fmv(S.krT)[:, ch, tk], o[:], reads=[o], writes=[S.krT], acc_w=True)
                    if not kv_only:
                        for ch in range(32):
                            p = fm(wb, V_END + ch * 128, h, wb); o = ev.next()
                            k.op("act", lambda e, p=p, o=o, TT=TT: e.activation(out=o[:], in_=p[:, :TT], func=AF.Sigmoid), reads=[p], writes=[o])
                            k.dma("sp", S.gT.ap.ap().rearrange("(c p) t -> p c t", p=128)[:, ch, tk], o[:], reads=[o], writes=[S.gT], acc_w=True)

    def gmlp_phase(S, l):
        nn = S.n
        TT = 512 if nn >= 512 else nn
        NCH = TT // 128
        fmv = lambda T_: T_.ap.ap().rearrange("(c p) t -> p c t", p=128)
        with k.phase():
            ws = k.sbuf("gm_ws", [128, 4, 128], BF16)
            k.dma("pool", ws[:], wsT.ap[l], reads=[wsT], writes=[ws])
            bs = k.sbuf("gm_bs", [1, 4, 128], BF16)
            k.dma("pool", bs[:], bsp.ap[l:l + 1], reads=[bsp], writes=[bs])
            ones = k.sbuf("gm_ones", [1, 128], BF16)
            k.op("dve", lambda e: e.memset(ones[:], 1.0), writes=[ones])
            vp = Pool(k, "gm_v", 2, [128, NCH, W], BF16)
            up = Pool(k, "gm_u", 2, [128, 2, TT], BF16)
            ap_ = Pool(k, "gm_a", 2, [128, 2, TT], BF16)
            pp = Pool(k, "gm_ps", 8, [128, NCH, 128], F32, "psum")
            def gm_loads(j):
                tk = slice(j * TT, (j + 1) * TT)
                vt = vp.next(); ut = up.next()
                k.dma("sp", vt[:], S.vln.ap[tk, :].rearrange("(c p) f -> p c f", p=128), reads=[S.vln], writes=[vt])
                k.dma("sp", ut[:], fmv(S.uT)[:, :, tk], reads=[S.uT], writes=[ut])
                return vt, ut
            nxt = gm_loads(0)
            for j in range(nn // TT):
                tk = slice(j * TT, (j + 1) * TT)
                vt, ut = nxt
                if j + 1 < nn // TT:
                    nxt = gm_loads(j + 1)
                at = ap_.next()
                for hf in range(2):
                    for gi in range(2):
                        g = 2 * hf + gi
                        p = pp.next()
                        for cc in range(NCH):
                            mmul(p[:, cc, :], vt[:, cc, hf * 128:(hf + 1) * 128], ws[:, g, :], True, False, [vt, ws], [p])
                            mmul(p[:, cc, :], ones[:], bs[:, g, :], False, True, [ones, bs], [p])
                        pr = slice(gi * 64, (gi + 1) * 64)
                        k.op("dve", lambda e, at=at, p=p, ut=ut, pr=pr, hf=hf: e.tensor_tensor(out=at[pr, hf, :].rearrange("p (c q) -> p c q", q=128), in0=p[pr, :, :], in1=ut[pr, hf, :].rearrange("p (c q) -> p c q", q=128), op=ALU.mult), reads=[p, ut], writes=[at])
                k.dma("sp", fmv(S.aT)[:, :, tk], at[:], reads=[at], writes=[S.aT], acc_w=True)

    def conv_phase(S, l):
        nn = S.n
        TT = 512 if nn >= 512 else nn
        fmv = lambda T_: T_.ap.ap().rearrange("(c p) t -> p c t", p=128)
        with k.phase():
            identf, = load_ident(False, True)
            cw = k.sbuf("cv_w", [128, 2, 31], F32)
            k.dma("sp", cw[:], conv_wT.ap[l].rearrange("(h p) j -> p h j", p=128), reads=[conv_wT], writes=[cw])
            prm = k.sbuf("cv_prm", [128, 3, 2], F32)
            for i_, src in enumerate((conv_b, cln_g, cln_b)):
                for hf in range(2):
                    k.dma("sp", prm[:, i_, hf:hf + 1], src.ap[l, hf * 128:(hf + 1) * 128, :], reads=[src], writes=[prm], acc_w=True)
            dg = k.sbuf("cv_diag", [128, 2, 31, 128], BF16)
            for hf in range(2):
                for j in range(31):
                    k.op("dve", lambda e, hf=hf, j=j: e.tensor_scalar(out=dg[:, hf, j, :], in0=identf[:], scalar1=cw[:, hf, j:j + 1], scalar2=None, op0=ALU.mult), reads=[identf, cw], writes=[dg])
            onesf = k.sbuf("cv_ones", [128, 128], F32)
            k.op("dve", lambda e: e.memset(onesf[:], 1.0 / W), writes=[onesf])
            yp = Pool(k, "cv_y", 2, [128, 2, TT + 30], BF16)
            pc = Pool(k, "cv_pc", 4, [128, TT], F32, "psum")
            pst = Pool(k, "cv_pst", 2, [128, 2, TT], F32, "psum")
            y2p = Pool(k, "cv_y2", 3, [128, 2, TT], F32)
            sqp = Pool(k, "cv_sq", 3, [128, 2, TT], F32)
            stp = Pool(k, "cv_st", 2, [128, 2, TT], F32)
            op_ = Pool(k, "cv_o", 2, [128, 2, TT], BF16)
            def cv_a(j):
                    t0 = j * TT
                    yt = yp.next()
                    lo, hi = max(0, t0 - 15), min(nn, t0 + TT + 15)
                    if lo > t0 - 15 or hi < t0 + TT + 15:
                        k.op("pool", lambda e, yt=yt: e.memset(yt[:], 0.0), writes=[yt])
                    k.dma("sp", yt[:, :, lo - (t0 - 15):hi - (t0 - 15)], fmv(S.yT)[:, :, lo:hi], reads=[S.yT], writes=[yt])
                    y2 = y2p.next(); sq = sqp.next()
                    for hf in range(2):
                        p = pc.next()
                        for jj in range(31):
                            mmul(p[:], dg[:, hf, jj, :], yt[:, hf, jj:jj + TT], jj == 0, jj == 30, [dg, yt], [p])
                        k.op("act", lambda e, y2=y2, p=p, hf=hf: e.activation(out=y2[:, hf, :], in_=p[:], func=AF.Identity, bias=prm[:, 0, hf:hf + 1], scale=1.0), reads=[p, prm], writes=[y2])
                        k.op("act", lambda e, y2=y2, sq=sq, hf=hf: e.activation(out=sq[:, hf, :], in_=y2[:, hf, :], func=AF.Square), reads=[y2], writes=[sq])
                    return (t0, y2, sq)
            def cv_b(ctx_):
                    t0, y2, sq = ctx_
                    ps_ = pst.next()
                    for hf in range(2):
                        mmul(ps_[:, 0, :], onesf[:], y2[:, hf, :], hf == 0, hf == 1, [onesf, y2], [ps_])
                    for hf in range(2):
                        mmul(ps_[:, 1, :], onesf[:], sq[:, hf, :], hf == 0, hf == 1, [onesf, sq], [ps_])
                    stt = stp.next(); ot = op_.next()
                    k.op("act", lambda e, stt=stt, ps_=ps_: e.activation(out=stt[:, 0, :], in_=ps_[:, 0, :], func=AF.Copy), reads=[ps_], writes=[stt])
                    k.op("dve", lambda e, stt=stt: e.tensor_tensor(out=stt[:, 1, :], in0=stt[:, 0, :], in1=stt[:, 0, :], op=ALU.mult), reads=[stt], writes=[stt])
                    k.op("dve", lambda e, stt=stt, ps_=ps_: e.tensor_tensor(out=stt[:, 1, :], in0=ps_[:, 1, :], in1=stt[:, 1, :], op=ALU.subtract), reads=[ps_, stt], writes=[stt])
                    k.op("act", lambda e, stt=stt: e.activation(out=stt[:, 1, :], in_=stt[:, 1, :], func=AF.Sqrt, bias=1e-6, scale=1.0), reads=[stt], writes=[stt])
                    k.op("dve", lambda e, stt=stt: e.reciprocal(out=stt[:, 1, :], in_=stt[:, 1, :]), reads=[stt], writes=[stt])
                    for hf in range(2):
                        k.op("dve", lambda e, y2=y2, stt=stt, hf=hf: e.tensor_tensor(out=y2[:, hf, :], in0=y2[:, hf, :], in1=stt[:, 0, :], op=ALU.subtract), reads=[y2, stt], writes=[y2])
                        k.op("pool", lambda e, y2=y2, stt=stt, hf=hf: e.tensor_tensor(out=y2[:, hf, :], in0=y2[:, hf, :], in1=stt[:, 1, :], op=ALU.mult), reads=[y2, stt], writes=[y2])
                        k.op("act", lambda e, y2=y2, ot=ot, hf=hf: e.activation(out=ot[:, hf, :], in_=y2[:, hf, :], func=AF.Silu, bias=prm[:, 2, hf:hf + 1], scale=prm[:, 1, hf:hf + 1]), reads=[y2, prm], writes=[ot])
                    k.dma("sp", fmv(S.cT)[:, :, t0:t0 + TT], ot[:], reads=[ot], writes=[S.cT], acc_w=True)
            pend = None
            for j in range(nn // TT):
                cur = cv_a(j)
                if pend is not None:
                    cv_b(pend)
                pend = cur
            cv_b(pend)

    def fourier_phase(S, l):
        nn, NN1 = S.n, S.N1
        fmv = lambda T_: T_.ap.ap().rearrange("(c p) t -> p c t", p=128)
        CB = 4096
        with k.phase():
            f1sb = k.sbuf("ff_f1", [NN1, 3, NN1], BF16)
            k.dma("sp", f1sb[:], S.f1.ap.ap().rearrange("m c q -> c m q"), reads=[S.f1], writes=[f1sb])
            zp = Pool(k, "ff_z", 4, [NN1, CB], BF16)
            yp = Pool(k, "ff_y", 4, [NN1, CB], BF16)
            pp = Pool(k, "ff_ps", 4, [NN1, 512], F32, "psum")
            Zrv = S.Zr.ap.ap().rearrange("(c p) f -> c (p f)", p=128)
            Ziv = S.Zi.ap.ap().rearrange("(c p) f -> c (p f)", p=128)
            def ff_loads(blk):
                cb = slice(blk * CB, (blk + 1) * CB)
                zr = zp.next(); zi = zp.next()
                k.dma("sp", zr[:], Zrv[:, cb], reads=[S.Zr], writes=[zr])
                k.dma("sp", zi[:], Ziv[:, cb], reads=[S.Zi], writes=[zi])
                return zr, zi
            nxt = ff_loads(0)
            for blk in range(128 * W // CB):
                cb = slice(blk * CB, (blk + 1) * CB)
                zr, zi = nxt
                if blk + 1 < 128 * W // CB:
                    nxt = ff_loads(blk + 1)
                yr = yp.next(); yi = yp.next()
                for sub in range(CB // 512):
                    cs_ = slice(sub * 512, (sub + 1) * 512)
                    pr = pp.next(); pi = pp.next()
                    mmul(pr[:], f1sb[:, 0, :], zr[:, cs_], True, False, [f1sb, zr], [pr])
                    mmul(pr[:], f1sb[:, 1, :], zi[:, cs_], False, True, [f1sb, zi], [pr])
                    mmul(pi[:], f1sb[:, 0, :], zi[:, cs_], True, False, [f1sb, zi], [pi])
                    mmul(pi[:], f1sb[:, 2, :], zr[:, cs_], False, True, [f1sb, zr], [pi])
                    k.op("act", lambda e, yr=yr, pr=pr, cs_=cs_: e.activation(out=yr[:, cs_], in_=pr[:], func=AF.Copy), reads=[pr], writes=[yr])
                    k.op("dve", lambda e, yi=yi, pi=pi, cs_=cs_: e.tensor_copy(out=yi[:, cs_], in_=pi[:]), reads=[pi], writes=[yi])
                k.dma("sp", S.Y1r.ap[:, cb], yr[:], reads=[yr], writes=[S.Y1r], acc_w=True)
                k.dma("sp", S.Y1i.ap[:, cb], yi[:], reads=[yi], writes=[S.Y1i], acc_w=True)
        with k.phase():
            KB = min(8, NN1)
            G = min(4, KB)
            f2sb = k.sbuf("ff_f2", [128, 2, 128], BF16)
            k.dma("sp", f2sb[:], f2.ap.ap().rearrange("m p q -> p m q"), reads=[f2], writes=[f2sb])
            tw = k.sbuf("ff_tw", [128, 2, NN1], F32)
            k.dma("sp", tw[:], S.tw.ap.ap().rearrange("m p q -> p m q"), reads=[S.tw], writes=[tw])
            outT = k.sbuf("ff_out", [128, 2, nn], BF16)
            yrp = Pool(k, "ff_yr", 2, [128, KB, W], BF16)
            yip = Pool(k, "ff_yi", 2, [128, KB, W], BF16)
            ypr = Pool(k, "ff_ypr", 2, [128, KB, W], BF16)
            ypi = Pool(k, "ff_ypi", 2, [128, KB, W], BF16)
            tp = Pool(k, "ff_t", 6, [128, W], F32)
            po = Pool(k, "ff_po", 4, [128, G, 128], F32, "psum")
            Y1rv = S.Y1r.ap.ap().rearrange("q (p f) -> p q f", p=128)
            Y1iv = S.Y1i.ap.ap().rearrange("q (p f) -> p q f", p=128)
            def f2_loads(kb):
                ks = slice(kb * KB, (kb + 1) * KB)
                yr = yrp.next(); yi = yip.next()
                k.dma("sp", yr[:], Y1rv[:, ks, :], reads=[S.Y1r], writes=[yr])
                k.dma("sp", yi[:], Y1iv[:, ks, :], reads=[S.Y1i], writes=[yi])
                return yr, yi
            nxt = f2_loads(0)
            for kb in range(NN1 // KB):
                ks = slice(kb * KB, (kb + 1) * KB)
                yr, yi = nxt
                if kb + 1 < NN1 // KB:
                    nxt = f2_loads(kb + 1)
                qr = ypr.next(); qi = ypi.next()
                for kk in range(KB):
                    k1 = kb * KB + kk
                    t1 = tp.next(); t2 = tp.next()
                    k.op("act", lambda e, t1=t1, yi=yi, kk=kk, k1=k1: e.activation(out=t1[:], in_=yi[:, kk, :], func=AF.Copy, scale=tw[:, 1, k1:k1 + 1]), reads=[yi, tw], writes=[t1])
                    k.op("dve", lambda e, t1=t1, yr=yr, qr=qr, kk=kk, k1=k1: e.scalar_tensor_tensor(out=qr[:, kk, :], in0=yr[:, kk, :], scalar=tw[:, 0, k1:k1 + 1], in1=t1[:], op0=ALU.mult, op1=ALU.subtract), reads=[yr, tw, t1], writes=[qr])
                    k.op("act", lambda e, t2=t2, yi=yi, kk=kk, k1=k1: e.activation(out=t2[:], in_=yi[:, kk, :], func=AF.Copy, scale=tw[:, 0, k1:k1 + 1]), reads=[yi, tw], writes=[t2])
                    k.op("dve", lambda e, t2=t2, yr=yr, qi=qi, kk=kk, k1=k1: e.scalar_tensor_tensor(out=qi[:, kk, :], in0=yr[:, kk, :], scalar=tw[:, 1, k1:k1 + 1], in1=t2[:], op0=ALU.mult, op1=ALU.add), reads=[yr, tw, t2], writes=[qi])
                for fh in range(2):
                    fs = slice(fh * 128, (fh + 1) * 128)
                    for g0 in range(0, KB, G):
                        p = po.next()
                        for gg in range(G):
                            kk = g0 + gg
                            mmul(p[:, gg, :], qr[:, kk, fs], f2sb[:, 0, :], True, False, [qr, f2sb], [p])
                            mmul(p[:, gg, :], qi[:, kk, fs], f2sb[:, 1, :], False, True, [qi, f2sb], [p])
                        k1_0 = kb * KB + g0
                        dst = outT[:, fh, :].rearrange("p (a b) -> p b a", b=NN1)[:, k1_0:k1_0 + G, :]
                        k.op("act", lambda e, dst=dst, p=p: e.activation(out=dst, in_=p[:], func=AF.Copy), reads=[p], writes=[outT])
            k.dma("sp", fmv(S.bT), outT[:], reads=[outT], writes=[S.bT])

    def attention_phase(S, SCx, l):
        nn, si = S.n, S.si
        NT = nn // 128
        fmv = lambda T_: T_.ap.ap().rearrange("(c p) t -> p c t", p=128)
        with k.phase():
            identb, = load_ident(True, False)
            kc = k.sbuf("at_kc", [128, 2, NCTX], BF16)
            k.dma("sp", kc[:], fmv(SCx.krT), reads=[SCx.krT], writes=[kc])
            vc = k.sbuf("at_vc", [128, 2, 4, 65], BF16)
            k.op("dve", lambda e: e.memset(vc[:], 1.0), writes=[vc])
            for t_ in range(2):
                k.dma("sp", vc[:, t_, :, 0:64], SCx.v.ap[t_ * 128:(t_ + 1) * 128, :].rearrange("p (h d) -> p h d", h=4), reads=[SCx.v], writes=[vc], acc_w=True)
            if si == 0:
                kr = k.sbuf("at_kr", [128, 2, nn], BF16)
                k.dma("sp", kr[:], fmv(S.krT), reads=[S.krT], writes=[kr])
                vs = k.sbuf("at_v", [128, NT, 4, 65], BF16)
                k.op("dve", lambda e: e.memset(vs[:], 1.0), writes=[vs])
                for t_ in range(NT):
                    k.dma("sp", vs[:, t_, :, 0:64], S.v.ap[t_ * 128:(t_ + 1) * 128, :].rearrange("p (h d) -> p h d", h=4), reads=[S.v], writes=[vs], acc_w=True)
                bias = k.sbuf("at_bias", [128, 4 * U, 128], BF16)
                k.dma("sp", bias[:], biasT.ap[l], reads=[biasT], writes=[bias])
            qrp = Pool(k, "at_qr", 3, [128, 2, 128], BF16)
            qpp = Pool(k, "at_qp", 3, [128, 2, 128], BF16)
            psS = Pool(k, "at_S", 2, [128, 8, 128], F32, "psum")
            psO = Pool(k, "at_O", 2, [128, 128], F32, "psum")
            psT = Pool(k, "at_T", 1, [128, 2, 128], BF16, "psum")
            Ep = Pool(k, "at_E", 3, [128, 8, 128], BF16)
            rcp = Pool(k, "at_rc", 4, [128, 1], F32)
            otp = Pool(k, "at_ot", 3, [128, W], BF16)
            dtp = Pool(k, "at_dt", 2, [128, 2, 128], BF16)
            items = [(qt, h) for qt in range(NT) for h in range(4)]
            qst = {}

            def stage_s(qt, h):
                qs = slice(qt * 128, (qt + 1) * 128)
                if h == 0:
                    qp_t = qpp.next()
                    k.dma("sp", qp_t[:], fmv(S.qpT)[:, :, qs], reads=[S.qpT], writes=[qp_t])
                    qr_t = None
                    lst = []
                    if si == 0:
                        qr_t = qrp.next()
                        k.dma("sp", qr_t[:], fmv(S.qrT)[:, :, qs], reads=[S.qrT], writes=[qr_t])
                        lst = qtiles[qt]
                    qst[qt] = (qp_t, qr_t, lst)
                qp_t, qr_t, lst = qst[qt]
                nw = len(lst); nt = nw + 2
                ch, pr = h // 2, slice((h % 2) * 64, (h % 2) * 64 + 64)
                Sp = psS.next(); E = Ep.next()
                for j_, (kt, u) in enumerate(lst):
                    mmul(Sp[:, j_, :], identb[:], bias[:, h * U + u, :], True, False, [identb, bias], [Sp])
                    mmul(Sp[:, j_, :], kr[pr, ch, kt * 128:(kt + 1) * 128], qr_t[pr, ch, :], False, True, [kr, qr_t], [Sp])
                for cj in range(2):
                    mmul(Sp[:, nw + cj, :], kc[pr, ch, cj * 128:(cj + 1) * 128], qp_t[pr, ch, :], True, True, [kc, qp_t], [Sp])
                k.op("act", lambda e, E=E, Sp=Sp, nt=nt: e.activation(out=E[:, 0:nt, :], in_=Sp[:, 0:nt, :], func=AF.Exp), reads=[Sp], writes=[E])
                return E

            cur_ot = {}

            def stage_pv(qt, h, E):
                qs = slice(qt * 128, (qt + 1) * 128)
                qp_t, qr_t, lst = qst[qt]
                nw = len(lst); nt = nw + 2
                if h == 0:
                    cur_ot[qt] = otp.next()
                ot = cur_ot[qt]
                O = psO.next(); rc = rcp.next()
                for j_ in range(nt):
                    rhs = vs[:, lst[j_][0], h, :] if j_ < nw else vc[:, j_ - nw, h, :]
                    mmul(O[:, 0:65], E[:, j_, :], rhs, j_ == 0, j_ == nt - 1, [E, vc] + ([vs] if si == 0 else []), [O])
                k.op("dve", lambda e, rc=rc, O=O: e.reciprocal(out=rc[:], in_=O[:, 64:65]), reads=[O], writes=[rc])
                k.op("dve", lambda e, ot=ot, O=O, rc=rc, h=h: e.tensor_scalar(out=ot[:, h * 64:(h + 1) * 64], in0=O[:, 0:64], scalar1=rc[:, 0:1], scalar2=None, op0=ALU.mult), reads=[O, rc], writes=[ot])
                if h == 3:
                    pt = psT.next(); dt = dtp.next()
                    for c in range(2):
                        k.op("pe", lambda e, c=c, pt=pt, ot=ot: e.transpose(out=pt[:, c, :], in_=ot[:, c * 128:(c + 1) * 128], identity=identb[:]), reads=[ot, identb], writes=[pt], signal=(c == 1))
                    k.op("act", lambda e, pt=pt, dt=dt: e.activation(out=dt[:], in_=pt[:], func=AF.Copy), reads=[pt], writes=[dt])
                    k.dma("sp", fmv(S.dT)[:, :, qs], dt[:], reads=[dt], writes=[S.dT], acc_w=True)

            Es = {0: stage_s(*items[0])}
            for i_, (qt, h) in enumerate(items):
                if i_ + 1 < len(items):
                    Es[i_ + 1] = stage_s(*items[i_ + 1])
                stage_pv(qt, h, Es.pop(i_))

    def merge_phase(S, l, Xsrc, Xdst):
        nn, si = S.n, S.si
        TT = 512 if nn >= 512 else nn
        fmv = lambda T_: T_.ap.ap().rearrange("(c p) t -> p c t", p=128)
        with k.phase():
            wbr = []
            for br in range(4):
                t = k.sbuf("mg_wbr", [128, 2, D], BF16)
                k.dma("pool", t[:], w_br[br].ap[l].rearrange("(c p) n -> p c n", p=128), reads=[w_br[br]], writes=[t])
                wbr.append(t)
            wo = k.sbuf("mg_wo", [128, 8, D], BF16)
            k.dma("pool", wo[:], w_out.ap[l].rearrange("(c p) n -> p c n", p=128), reads=[w_out], writes=[wo])
            identb, = load_ident(True, False)
            gtb = k.sbuf("mg_gt", [128, D], F32)
            bc_load(gtb, modD[l], modD[l].ap[si:si + 1, 2 * D:3 * D], D)
            brp = [Pool(k, "mg_br%d" % i, 2, [128, 2, TT], BF16) for i in range(4)]
            gp = Pool(k, "mg_g", 2, [128, 32, TT], BF16)
            ps = Pool(k, "mg_ps", 8, [128, 512], F32, "psum")
            tp = Pool(k, "mg_t", 8, [128, TT], BF16)
            mp = Pool(k, "mg_m", 2, [128, 8, TT], BF16)
            xp = Pool(k, "mg_x", 2, [128, D], F32)
            tmp = Pool(k, "mg_tmp", 2, [128, D], F32)
            xop = Pool(k, "mg_xo", 2, [128, D], F32)
            srcs = [S.aT, S.bT, S.cT, S.dT]
            def out_group(j, mT, g_, st_):
                s_, half = g_ // 2, g_ % 2
                rows = slice(j * TT + s_ * 128, j * TT + (s_ + 1) * 128)
                if half == 0:
                    st_["xt"] = xp.next(); st_["tm"] = tmp.next(); st_["xo"] = xop.next()
                    k.dma("sp", st_["xt"][:], Xsrc.ap[rows, :], reads=[Xsrc], writes=[st_["xt"]])
                xt, tm, xo = st_["xt"], st_["tm"], st_["xo"]
                hs = slice(half * 512, (half + 1) * 512)
                p = ps.next()
                for kc in range(8):
                    mmul(p[:], mT[:, kc, s_ * 128:(s_ + 1) * 128], wo[:, kc, hs], kc == 0, kc == 7, [mT, wo], [p])
                k.op("dve", lambda e, tm=tm, p=p, hs=hs: e.tensor_tensor(out=tm[:, hs], in0=p[:], in1=gtb[:, hs], op=ALU.mult), reads=[p, gtb], writes=[tm])
                if half == 1:
                    k.op("pool", lambda e, xo=xo, tm=tm, xt=xt: e.tensor_tensor(out=xo[:], in0=tm[:], in1=xt[:], op=ALU.add), reads=[tm, xt], writes=[xo])
                    k.dma("sp", Xdst.ap[rows, :], xo[:], reads=[xo], writes=[Xdst], acc_w=True)

            NG = (TT // 128) * 2
            prev = None
            def mg_loads(j):
                tk = slice(j * TT, (j + 1) * TT)
                bts = []
                for br in range(4):
                    t = brp[br].next()
                    k.dma("sp", t[:], fmv(srcs[br])[:, :, tk], reads=[srcs[br]], writes=[t])
                    bts.append(t)
                g = gp.next()
                for q4 in range(4):
                    k.dma("sp", g[:, q4 * 8:(q4 + 1) * 8, :], S.gT.ap.ap().rearrange("(c p) t -> p c t", p=128)[:, q4 * 8:(q4 + 1) * 8, tk], reads=[S.gT], writes=[g], acc_w=True)
                return bts, g
            nxt = mg_loads(0)
            for j in range(nn // TT):
                tk = slice(j * TT, (j + 1) * TT)
                bts, g = nxt
                if j + 1 < nn // TT:
                    nxt = mg_loads(j + 1)
                mT = mp.next()
                st_ = {}
                for oc in range(8):
                    ts = []
                    for br in range(4):
                        p = ps.next()
                        for kc in range(2):
                            mmul(p[:, :TT], wbr[br][:, kc, oc * 128:(oc + 1) * 128], bts[br][:, kc, :], kc == 0, kc == 1, [wbr[br], bts[br]], [p])
                        t = tp.next()
                        k.op("dve", lambda e, t=t, p=p, g=g, br=br, oc=oc: e.tensor_tensor(out=t[:], in0=p[:, :TT], in1=g[:, br * 8 + oc, :], op=ALU.mult), reads=[p, g], writes=[t])
                        ts.append(t)
                    if prev is not None and oc < NG:
                        out_group(prev[0], prev[1], oc, st_)
                    pm = ps.next()
                    for br in range(4):
                        mmul(pm[:, :TT], identb[:], ts[br][:], br == 0, br == 3, [identb, ts[br]], [pm])
                    k.op("act", lambda e, pm=pm, mT=mT, oc=oc: e.activation(out=mT[:, oc, :], in_=pm[:, :TT], func=AF.Copy), reads=[pm], writes=[mT])
                prev = (j, mT)
            st_ = {}
            for g_ in range(NG):
                out_group(prev[0], prev[1], g_, st_)

    def route_phase(S, l):
        nn, cap_ = S.n, S.cap
        Tseg = nn // 8
        NCH = nn // 128
        if not hasattr(S, "slot2D"):
            S.slot2D = scr(S.name + "_slot2D", [NE, nn], F32)
        seg = lambda T_: T_.ap.ap().rearrange("e (s t) -> (e s) t", s=8)
        with k.phase():
            blk = k.sbuf("rt_blk", [128, 128], F32); low = k.sbuf("rt_low", [128, 128], F32)
            k.dma("sp", blk[:], r_blk.ap.ap(), reads=[r_blk], writes=[blk])
            k.dma("sp", low[:], r_low.ap.ap(), reads=[r_low], writes=[low])
            A = k.sbuf("rt_A", [128, Tseg], F32)
            k.dma("sp", A[:], seg(S.affD), reads=[S.affD], writes=[A])
            junk = k.sbuf("rt_junk", [128, Tseg], F32)
            sv = k.sbuf("rt_sv", [128, 8], F32)
            k.op("dve", lambda e: e.memset(sv[:], 0.0), writes=[sv])
            k.op("dve", lambda e: e.memset(sv[:, 1:2], 1.0), reads=[sv], writes=[sv])
            pc = Pool(k, "rt_pc", 2, [128, 1], F32, "psum")
            for it in range(30):
                step = 2.0 ** -(it + 1)
                k.op("dve", lambda e, step=step: e.tensor_scalar(out=sv[:, 2:3], in0=sv[:, 0:1], scalar1=step, scalar2=None, op0=ALU.add), reads=[sv], writes=[sv])
                k.op("dve", lambda e: e.tensor_scalar(out=junk[:], in0=A[:], scalar1=sv[:, 2:3], scalar2=0.0, op0=ALU.is_ge, op1=ALU.add, accum_out=sv[:, 3:4]), reads=[A, sv], writes=[junk, sv])
                p = pc.next()
                mmul(p[:], blk[:], sv[:, 3:4], True, True, [blk, sv], [p])
                k.op("dve", lambda e, p=p, step=step: e.tensor_scalar(out=sv[:, 4:5], in0=p[:], scalar1=float(cap_), scalar2=step, op0=ALU.is_ge, op1=ALU.mult), reads=[p, sv], writes=[sv])
                k.op("dve", lambda e: e.tensor_tensor(out=sv[:, 0:1], in0=sv[:, 0:1], in1=sv[:, 4:5], op=ALU.add), reads=[sv], writes=[sv])
            mask = k.sbuf("rt_mask", [128, Tseg], F32)
            k.op("dve", lambda e: e.tensor_scalar(out=mask[:], in0=A[:], scalar1=sv[:, 0:1], scalar2=None, op0=ALU.is_ge), reads=[A, sv], writes=[mask])
            k.op("pool", lambda e: e.memset(junk[:], 0.0), writes=[junk])
            cs = k.sbuf("rt_cs", [128, Tseg], F32)
            k.op("dve", lambda e: e.tensor_tensor_scan(out=cs[:], data0=mask[:], data1=junk[:], initial=0.0, op0=ALU.add, op1=ALU.add), reads=[mask, junk], writes=[cs])
            k.op("dve", lambda e: e.tensor_copy(out=sv[:, 6:7], in_=cs[:, Tseg - 1:Tseg]), reads=[cs, sv], writes=[sv])
            p = pc.next()
            mmul(p[:], low[:], sv[:, 6:7], True, True, [low, sv], [p])
            k.op("dve", lambda e, p=p: e.tensor_copy(out=sv[:, 7:8], in_=p[:]), reads=[p, sv], writes=[sv])
            INV = 2016.0
            k.op("dve", lambda e: e.tensor_scalar(out=cs[:], in0=cs[:], scalar1=sv[:, 7:8], scalar2=-(INV + 1.0), op0=ALU.add, op1=ALU.add), reads=[cs, sv], writes=[cs])
            k.op("dve", lambda e: e.tensor_tensor(out=cs[:], in0=cs[:], in1=mask[:], op=ALU.mult), reads=[cs, mask], writes=[cs])
            k.op("dve", lambda e: e.tensor_scalar(out=cs[:], in0=cs[:], scalar1=INV, scalar2=None, op0=ALU.add), reads=[cs], writes=[cs])
            si_ = k.sbuf("rt_si", [128, Tseg], I32); s2 = k.sbuf("rt_s2", [128, Tseg], I32)
            k.op("dve", lambda e: e.tensor_copy(out=si_[:], in_=cs[:]), reads=[cs], writes=[si_])
            k.op("dve", lambda e: e.tensor_scalar(out=s2[:], in0=si_[:], scalar1=5, scalar2=None, op0=ALU.arith_shift_right), reads=[si_], writes=[s2])
            k.op("dve", lambda e: e.tensor_copy(out=mask[:], in_=s2[:]), reads=[s2], writes=[mask])
            k.dma("sp", seg(S.slotD), mask[:], reads=[mask], writes=[S.slotD])
            s3 = k.sbuf("rt_s3", [128, Tseg], I32)
            k.op("dve", lambda e: e.tensor_scalar(out=s3[:], in0=si_[:], scalar1=31, scalar2=None, op0=ALU.bitwise_and), reads=[si_], writes=[s3])
            k.op("dve", lambda e: e.tensor_copy(out=junk[:], in_=s3[:]), reads=[s3], writes=[junk])
            k.dma("sp", seg(S.slot2D), junk[:], reads=[junk], writes=[S.slot2D])
        with k.phase():
            identf, = load_ident(False, True)
            iota = k.sbuf("rt_iota", [128, NE, 32], F32)
            k.dma("sp", iota[:], r_iota.ap.ap(), reads=[r_iota], writes=[iota])
            tid = k.sbuf("rt_tid", [128, NCH], F32)
            k.dma("sp", tid[:], S.tid.ap.ap(), reads=[S.tid], writes=[tid])
            rows3 = []
            for nm, src in (("hi", S.slotD), ("lo", S.slot2D), ("af", S.affD)):
                t = k.sbuf("rt_" + nm, [NE, nn], F32)
                k.dma("sp", t[:], src.ap.ap(), reads=[src], writes=[t])
                rows3.append(t)
            pT = Pool(k, "rt_pT", 3, [128, 3, NE], F32, "psum")
            tmp_ = Pool(k, "rt_tm", 3, [128, 3, NE], F32)
            Ap = Pool(k, "rt_Ap", 3, [128, NE, 32], F32)
            Bp = Pool(k, "rt_Bp", 3, [128, NE, 32], F32)
            AGp = Pool(k, "rt_AG", 3, [128, NE, 2, 32], F32)
            accp = Pool(k, "rt_acc", 2, [64, NE, 32], F32, "psum")
            res = k.sbuf("rt_res", [64, NE, 32], F32)
            k.op("dve", lambda e: e.memset(res[:], 0.0), writes=[res])
            for c in range(NCH):
                cs_ = slice(c * 128, (c + 1) * 128)
                pt = pT.next(); tm = tmp_.next(); Aa = Ap.next(); Bb = Bp.next(); AG = AGp.next()
                for i_ in range(3):
                    k.op("pe", lambda e, i_=i_, pt=pt, cs_=cs_: e.transpose(out=pt[:, i_, :], in_=rows3[i_][:, cs_], identity=identf[0:NE, 0:NE]), reads=[rows3[i_], identf], writes=[pt], signal=(i_ == 2))
                k.op("act", lambda e, pt=pt, tm=tm: e.activation(out=tm[:], in_=pt[:], func=AF.Copy), reads=[pt], writes=[tm])
                b0, b1, b2 = [tm[:, i_, :].unsqueeze(2).to_broadcast([128, NE, 32]) for i_ in range(3)]
                k.op("dve", lambda e, Aa=Aa, b0=b0: e.tensor_tensor(out=Aa[:], in0=iota[:], in1=b0, op=ALU.is_equal), reads=[iota, tm], writes=[Aa])
                k.op("dve", lambda e, Bb=Bb, b1=b1: e.tensor_tensor(out=Bb[:], in0=iota[:], in1=b1, op=ALU.is_equal), reads=[iota, tm], writes=[Bb])
                k.op("dve", lambda e, Aa=Aa, AG=AG, b2=b2: e.tensor_tensor(out=AG[:, :, 1, :], in0=Aa[:], in1=b2, op=ALU.mult), reads=[Aa, tm], writes=[AG])
                k.op("act", lambda e, Aa=Aa, AG=AG, c=c: e.activation(out=AG[:, :, 0, :], in_=Aa[:], func=AF.Copy, scale=tid[:, c:c + 1]), reads=[Aa, tid], writes=[AG])
                acc = accp.next()
                for e_ in range(NE):
                    mmul(acc[:, e_, :], AG[:, e_, :, :], Bb[:, e_, :], True, True, [AG, Bb], [acc])
                k.op("dve", lambda e, acc=acc: e.tensor_tensor(out=res[:], in0=acc[:], in1=res[:], op=ALU.add), reads=[acc, res], writes=[res])
            resi = k.sbuf("rt_resi", [32, NE, 32], I32)
            k.op("dve", lambda e: e.tensor_copy(out=resi[:], in_=res[0:32]), reads=[res], writes=[resi])
            na = max(1, cap_ // 32)
            k.dma("sp", S.idxD.ap.ap().rearrange("e (a b) -> a e b", b=32), resi[0:na], reads=[resi], writes=[S.idxD])
            k.dma("sp", S.gateD.ap.ap().rearrange("e (a b) -> a e b", b=32), res[32:32 + na], reads=[res], writes=[S.gateD])

    def experts_phase(l, streams):
        with k.phase():
            identb, = load_ident(True, False)
            gtb = {}
            for S in streams:
                t = k.sbuf("ex_gt", [128, D], F32)
                bc_load(t, modD[l], modD[l].ap[S.si:S.si + 1, 5 * D:6 * D], D)
                gtb[S.si] = t
            wgp = Pool(k, "ex_wg", 2, [128, 8, D], BF16)
            wup = Pool(k, "ex_wu", 2, [128, 8, D], BF16)
            wdp = Pool(k, "ex_wd", 2, [128, 8, D], BF16)
            CAPM = max(S.cap for S in streams)
            xeTp = Pool(k, "ex_xeT", 2, [128, 8, CAPM], BF16)
            hT = k.sbuf("ex_hT", [128, 8, CAPM], BF16)
            xep = Pool(k, "ex_xe", 3, [128, D], BF16)
            yp = Pool(k, "ex_y", 3, [128, D], F32)
            sap = Pool(k, "ex_sa", 2, [128, 512], F32)
            ixp = Pool(k, "ex_ix", 3, [128, 8], I32)
            gap = Pool(k, "ex_ga", 3, [128, 8], F32)
            psT = Pool(k, "ex_psT", 1, [128, 8, 128], BF16, "psum")
            psA = Pool(k, "ex_psA", 2, [128, 512], F32, "psum")
            psU = Pool(k, "ex_psU", 2, [128, 512], F32, "psum")
            psY = Pool(k, "ex_psY", 3, [128, 512], F32, "psum")
            items = [(e_, S) for e_ in range(NE) for S in streams]
            wts = {}

            def load_w(e_):
                wg = wgp.next(); wu = wup.next(); wd = wdp.next()
                for wt, src in ((wg, w_gate), (wu, w_up), (wd, w_down)):
                    k.dma("pool", wt[:], src.ap[l, e_].rearrange("(c p) f -> p c f", p=128), reads=[src], writes=[wt])
                wts[e_] = (wg, wu, wd)

            def stage_g(e_, S):
                cap_ = S.cap
                P_ = min(128, cap_)
                ntl = max(1, cap_ // 128)
                ix = ixp.next(); ga = gap.next(); xeT = xeTp.next()
                k.dma("sp", ix[:P_, :ntl], S.idxD.ap[e_, :].rearrange("(p t) -> p t", t=ntl), reads=[S.idxD], writes=[ix])
                k.dma("sp", ga[:P_, :ntl], S.gateD.ap[e_, :].rearrange("(p t) -> p t", t=ntl), reads=[S.gateD], writes=[ga])
                k.op("dve", lambda e, ix=ix, P_=P_, ntl=ntl, hi_=S.n - 1: e.tensor_scalar(out=ix[:P_, :ntl], in0=ix[:P_, :ntl], scalar1=hi_, scalar2=0, op0=ALU.min, op1=ALU.max), reads=[ix], writes=[ix])
                for t_ in range(ntl):
                    xe = xep.next()
                    k.dma_raw("pool", lambda e, xe=xe, ix=ix, t_=t_, S=S, P_=P_: e.indirect_dma_start(out=xe[:P_, :], out_offset=None, in_=S.h2D.ap.ap(), in_offset=bass.IndirectOffsetOnAxis(ap=ix[:P_, t_:t_ + 1], axis=0)), reads=[S.h2D, ix], writes=[xe])
                    pt = psT.next()
                    for c in range(8):
                        k.op("pe", lambda e, c=c, pt=pt, xe=xe, P_=P_: e.transpose(out=pt[:, c, :P_], in_=xe[:P_, c * 128:(c + 1) * 128], identity=identb[:P_, :P_]), reads=[xe, identb], writes=[pt], signal=(c == 7))
                    k.op("act", lambda e, pt=pt, t_=t_, P_=P_, xeT=xeT: e.activation(out=xeT[:, :, t_ * 128:t_ * 128 + P_], in_=pt[:, :, :P_], func=AF.Copy), reads=[pt], writes=[xeT])
                return (ix, ga, xeT)

            def stage_u(e_, S, ctx_):
                ix, ga, xeT = ctx_
                wg, wu, wd = wts[e_]
                cap_ = S.cap
                NB = min(512, cap_)
                for fc in range(8):
                    fs = slice(fc * 128, (fc + 1) * 128)
                    for nb in range(cap_ // NB):
                        ns = slice(nb * NB, (nb + 1) * NB)
                        pa = psA.next(); pu = psU.next(); sa = sap.next()
                        for c in range(8):
                            mmul(pa[:, :NB], wg[:, c, fs], xeT[:, c, ns], c == 0, c == 7, [wg, xeT], [pa])
                        for c in range(8):
                            mmul(pu[:, :NB], wu[:, c, fs], xeT[:, c, ns], c == 0, c == 7, [wu, xeT], [pu])
                        k.op("act", lambda e, sa=sa, pa=pa, NB=NB: e.activation(out=sa[:, :NB], in_=pa[:, :NB], func=AF.Silu), reads=[pa], writes=[sa])
                        k.op("dve", lambda e, sa=sa, pu=pu, fc=fc, ns=ns, NB=NB: e.tensor_tensor(out=hT[:, fc, ns], in0=pu[:, :NB], in1=sa[:, :NB], op=ALU.mult), reads=[pu, sa], writes=[hT])

            def stage_d(e_, S, ctx_):
                ix, ga, xeT = ctx_
                wg, wu, wd = wts[e_]
                cap_ = S.cap
                P_ = min(128, cap_)
                ntl = max(1, cap_ // 128)
                Xn = S.Xnext
                for t_ in range(ntl):
                    y = yp.next()
                    for half in range(2):
                        hs = slice(half * 512, (half + 1) * 512)
                        py = psY.next()
                        for fc in range(8):
                            mmul(py[:P_, :], hT[:, fc, t_ * 128:t_ * 128 + P_], wd[:, fc, hs], fc == 0, fc == 7, [hT, wd], [py])
                        k.op("dve", lambda e, y=y, py=py, ga=ga, t_=t_, hs=hs, S=S, P_=P_: e.scalar_tensor_tensor(out=y[:P_, hs], in0=py[:P_, :], scalar=ga[:P_, t_:t_ + 1], in1=gtb[S.si][:P_, hs], op0=ALU.mult, op1=ALU.mult), reads=[py, ga, gtb[S.si]], writes=[y])
                    k.dma_raw("pool", lambda e, y=y, ix=ix, t_=t_, Xn=Xn, P_=P_: e.indirect_dma_start(out=Xn.ap.ap(), out_offset=bass.IndirectOffsetOnAxis(ap=ix[:P_, t_:t_ + 1], axis=0), in_=y[:P_, :], in_offset=None, compute_op=ALU.add), reads=[y, ix], writes=[Xn], acc_w=(t_ > 0))
                    if t_ == 0:
                        Xn.wb = []

            load_w(0)
            ctxs = {0: stage_g(*items[0])}
            for i_, (e_, S) in enumerate(items):
                if S is streams[0] and e_ + 1 < NE:
                    load_w(e_ + 1)
                stage_u(e_, S, ctxs[i_])
                if i_ + 1 < len(items):
                    ctxs[i_ + 1] = stage_g(*items[i_ + 1])
                stage_d(e_, S, ctxs[i_])

    PH = set(dbg_phases) if dbg_phases is not None else None
    def on(name):
        return PH is None or name in PH
    SX.Xcur, SC.Xcur = x_in, ctx_in
    for l in range(L):
        last = l == L - 1
        for S_ in (SX, SC):
            S_.Xin = S_.Xcur
            S_.Xnext = S_.XA if S_.Xcur is not S_.XA else S_.XB
        if on("mod"):
            mod_phase(l)
        if on("norm1"):
            norm_phase(SX, l, 1); norm_phase(SC, l, 1)
        if on("inproj"):
            inproj_phase([(SX, False), (SC, last)], l)
        if on("gmlp"):
            gmlp_phase(SX, l)
            if not last:
                gmlp_phase(SC, l)
        if on("conv"):
            conv_phase(SX, l)
            if not last:
                conv_phase(SC, l)
        if on("fourier"):
            fourier_phase(SX, l)
            if not last:
                fourier_phase(SC, l)
        if on("attn"):
            attention_phase(SX, SC, l)
            if not last:
                attention_phase(SC, SC, l)
        if on("merge"):
            merge_phase(SX, l, SX.Xcur, SX.Xnext)
            if not last:
                merge_phase(SC, l, SC.Xcur, SC.Xnext)
        if on("ffn"):
            strs = [SX] if last else [SX, SC]
            for S_ in strs:
                S_.Xin = S_.Xnext
                norm_phase(S_, l, 2)
                route_phase(S_, l)
            if on("experts"):
                experts_phase(l, strs)
        for S_ in (SX, SC):
            S_.Xcur = S_.Xnext
        if PH is not None:
            break
    SX.Xin = SX.Xcur if PH is None else x_in
    norm_phase(SX, 0, 3)
    k.finish([OUT])
    k.emit()
    st.close()
    return nc, list(ins.keys())


def _prep_inputs(inp, n, L=2):
    f = lambda a: np.ascontiguousarray(np.asarray(a, dtype=np.float32))
    ulist, qtiles = _attn_geometry(n)
    cs, f1x, f2, twx = _dft_consts(n)
    _, f1c, _, twc = _dft_consts(NCTX)
    blk, low, iota = _route_consts()
    w_in = f(inp["w_in"])
    i = np.arange(256)
    perm = np.where((i % 32) < 16, i + 16, i - 16)
    w_qkp = np.concatenate([w_in[:, :, C_END:Q_END][:, :, perm], w_in[:, :, Q_END:K_END][:, :, perm]], axis=2)
    pp = np.arange(128)
    common = {
        "w_mod": f(inp["w_mod"]), "b_mod": f(inp["b_mod"]), "norm1_g": f(inp["norm1_g"]), "norm2_g": f(inp["norm2_g"]),
        "final_norm_g": f(inp["final_norm_g"]).reshape(1, D), "w_in": w_in, "w_qkp": np.ascontiguousarray(w_qkp),
        "sgu_ln_g": f(inp["sgu_ln_g"]), "sgu_ln_b": f(inp["sgu_ln_b"]),
        "wsT": np.ascontiguousarray(f(inp["w_spatial"]).transpose(0, 3, 1, 2)),
        "b_spatial": f(inp["b_spatial"]),
        "w_a_out": f(inp["w_a_out"]), "w_b_out": f(inp["w_b_out"]), "w_c_out": f(inp["w_c_out"]), "w_d_out": f(inp["w_d_out"]),
        "conv_wT": np.ascontiguousarray(f(inp["conv_w"]).transpose(0, 2, 1)), "conv_b_c": f(inp["conv_b"])[:, :, None].copy(),
        "conv_ln_g_c": f(inp["conv_ln_g"])[:, :, None].copy(), "conv_ln_b_c": f(inp["conv_ln_b"])[:, :, None].copy(),
        "w_out": f(inp["w_out"]), "w_router": f(inp["w_router"]),
        "w_gate_e": f(inp["w_gate_e"]), "w_up_e": f(inp["w_up_e"]), "w_down_e": f(inp["w_down_e"]),
        "id_bf": np.eye(128).astype(bf16_np), "id_f": np.eye(128, dtype=np.float32),
        "rope": _rope_tables(n),
        "biasT": np.stack([_bias_tiles(f(inp["rpb"])[l], ulist) for l in range(L)]),
        "dft_cs": cs, "dft_f1x": f1x, "dft_f1c": f1c, "dft_f2": f2, "dft_twx": twx, "dft_twc": twc,
        "r_blk": blk, "r_low": low, "r_iota": iota,
        "tid_x": (np.arange(n // 128)[None, :] * 128 + pp[:, None]).astype(np.float32),
        "tid_c": (np.arange(2)[None, :] * 128 + pp[:, None]).astype(np.float32),
        "cctx_pk": np.ascontiguousarray(f(inp["c_ctx"]).reshape(8, 128).T),
    }
    maps = []
    B = inp["x"].shape[0]
    for b in range(B):
        m = dict(common)
        m["x"] = f(inp["x"][b]); m["ctx"] = f(inp["ctx"][b])
        m["c_pk"] = np.ascontiguousarray(f(inp["c"][b]).reshape(8, 128).T)
        maps.append(m)
    return maps


_CACHE = {}


def kernel(**inputs):
    n = inputs["x"].shape[1]
    B = inputs["x"].shape[0]
    if n not in _CACHE:
        _CACHE[n] = build_program(n)
    nc, names = _CACHE[n]
    maps = _prep_inputs(inputs, n)
    maps = [{kk: m[kk] for kk in names} for m in maps]
    res = run_bass_kernel_spmd(nc, maps, core_ids=list(range(B)))
    return np.stack([np.asarray(r["out"], dtype=np.float32) for r in res.results], axis=0)
```

```python
import numpy as np
import ml_dtypes
from contextlib import ExitStack
import concourse.bass as bass
import concourse.mybir as mybir
from concourse.bass_utils import run_bass_kernel_spmd

F32 = mybir.dt.float32
BF16 = mybir.dt.bfloat16
I32 = mybir.dt.int32
U32 = mybir.dt.uint32
AF = mybir.ActivationFunctionType
ALU = mybir.AluOpType
AX = mybir.AxisListType

N_DMA_SEMS = 40
N_SW_SEMS = 8


class T:
    def __init__(self, ap, name=""):
        self.ap = ap
        self.name = name
        self.w = []
        self.wb = []
        self.r = []

    def __getitem__(self, idx):
        return self.ap[idx]


class K:
    ENGS = ["pe", "act", "dve", "pool", "sp"]

    def __init__(self, nc, stack):
        self.nc = nc
        self.stack = stack
        self.streams = {e: [] for e in self.ENGS}
        self.sems = {}
        for e in self.ENGS:
            self.sems[e] = stack.enter_context(nc.semaphore("c_" + e))
        self.count = {e: 0 for e in self.ENGS}
        self.dma_sems = [stack.enter_context(nc.semaphore("d%d" % i)) for i in range(N_DMA_SEMS)]
        self.dma_val = [0] * N_DMA_SEMS
        self.dma_rr = 0
        self.dma_rr_sw = 0
        self.known = {e: {} for e in self.ENGS}
        self.same_engine_sync = True
        self.n_inst = 0

    def sbuf(self, name, shape, dtype):
        self.n_alloc = getattr(self, "n_alloc", 0) + 1
        name = "%s_%d" % (name, self.n_alloc)
        t = self.stack.enter_context(self.nc.sbuf_tensor(name, list(shape), dtype))
        return T(t, name)

    def psum(self, name, shape, dtype):
        self.n_alloc = getattr(self, "n_alloc", 0) + 1
        name = "%s_%d" % (name, self.n_alloc)
        t = self.stack.enter_context(self.nc.psum_tensor(name, list(shape), dtype))
        return T(t, name)

    def dram(self, name, shape, dtype, kind="Internal"):
        t = self.nc.dram_tensor(name, list(shape), dtype, kind=kind)
        return T(t, name)

    def _sem_of(self, key):
        return self.sems[key] if isinstance(key, str) else self.dma_sems[key]

    def _wait(self, eng, dep):
        key, val = dep
        if key == eng and (not self.same_engine_sync or eng in ("pe", "sp")):
            return
        kn = self.known[eng]
        if kn.get(key, 0) >= val:
            return
        kn[key] = val
        self.streams[eng].append(("wait", key, val))

    def _deps(self, eng, reads, writes, acc_w=False):
        for t in reads:
            for d in t.w:
                self._wait(eng, d)
        for t in writes:
            for d in (t.wb if acc_w else t.w):
                self._wait(eng, d)
            for d in t.r:
                self._wait(eng, d)

    def _mark(self, dep, reads, writes, acc_w=False):
        for t in reads:
            t.r.append(dep)
            if len(t.r) > 64:
                t.r = self._compress(t.r)
        for t in writes:
            if acc_w:
                t.w.append(dep)
                if len(t.w) > 64:
                    t.w = self._compress(t.w)
            else:
                t.w = [dep]
                t.wb = [dep]
            t.r = []

    @staticmethod
    def _compress(deps):
        m = {}
        for k, v in deps:
            if m.get(k, 0) < v:
                m[k] = v
        return list(m.items())

    def op(self, eng, fn, reads=(), writes=(), signal=True):
        self._deps(eng, reads, writes)
        if signal:
            self.count[eng] += 1
            dep = (eng, self.count[eng])
            self.streams[eng].append(("inst", fn, eng, 1))
        else:
            dep = (eng, self.count[eng] + 1)
            self.streams[eng].append(("inst", fn, None, 0))
        self._mark(dep, reads, writes)
        self.n_inst += 1

    def dma(self, q, out, in_, reads=(), writes=(), acc_w=False, **kw):
        self._deps(q, reads, writes, acc_w=acc_w)
        self._throttle(q)
        s = self._next_sem(q)
        if self.dma_val[s] > 0:
            self._wait(q, (s, self.dma_val[s]))
        self.dma_val[s] += 16
        dep = (s, self.dma_val[s])

        def fn(e, out=out, in_=in_, kw=kw):
            return e.dma_start(out=out, in_=in_, **kw)
        self.streams[q].append(("inst", fn, s, 16))
        self._mark(dep, reads, writes, acc_w=acc_w)
        self.n_inst += 1
        if q == "pool":
            self.__dict__.setdefault("pool_out", []).append(dep)
        return dep

    def _next_sem(self, q):
        if q == "pool":
            s = self.dma_rr_sw
            self.dma_rr_sw = (self.dma_rr_sw + 1) % N_SW_SEMS
            return s
        s = N_SW_SEMS + self.dma_rr
        self.dma_rr = (self.dma_rr + 1) % (N_DMA_SEMS - N_SW_SEMS)
        return s

    def _throttle(self, q):
        if q != "pool":
            return
        po = self.__dict__.setdefault("pool_out", [])
        if len(po) >= 2:
            self._wait(q, po[-2])

    def dma_raw(self, q, fn, reads=(), writes=(), acc_w=False):
        self._deps(q, reads, writes, acc_w=acc_w)
        self._throttle(q)
        s = self._next_sem(q)
        if self.dma_val[s] > 0:
            self._wait(q, (s, self.dma_val[s]))
        self.dma_val[s] += 16
        dep = (s, self.dma_val[s])
        self.streams[q].append(("inst", fn, s, 16))
        self._mark(dep, reads, writes, acc_w=acc_w)
        if q == "pool":
            self.__dict__.setdefault("pool_out", []).append(dep)
        return dep

    def finish(self, out_tiles):
        for t in out_tiles:
            for d in t.w:
                self._wait("sp", d)
        for e in self.ENGS:
            if e != "sp" and self.count[e] > 0:
                self._wait("sp", (e, self.count[e]))
        for s in range(N_DMA_SEMS):
            if self.dma_val[s] > 0:
                self._wait("sp", (s, self.dma_val[s]))

    def emit(self):
        nc = self.nc
        engmap = {"pe": "tensor", "act": "scalar", "dve": "vector", "pool": "gpsimd", "sp": "sync"}
        with nc.Block() as block:
            for e in self.ENGS:
                stream = self.streams[e]
                if not stream:
                    continue

                def body(eng, stream=stream):
                    for item in stream:
                        if item[0] == "wait":
                            eng.wait_ge(self._sem_of(item[1]), item[2])
                        else:
                            _, fn, key, inc = item
                            ins = fn(eng)
                            if key is not None:
                                ins.then_inc(self._sem_of(key), inc)
                getattr(block, engmap[e])(body)


class Pool:
    def __init__(self, k, name, n, shape, dtype, space="sbuf"):
        mk = k.sbuf if space == "sbuf" else k.psum
        self.tiles = [mk("%s%d" % (name, i), shape, dtype) for i in range(n)]
        self.i = 0

    def next(self):
        t = self.tiles[self.i]
        self.i = (self.i + 1) % len(self.tiles)
        return t


def _k_phase(self):
    class _Ph:
        def __init__(s, k):
            s.k = k
        def __enter__(s):
            s.prev = s.k.stack
            s.st = ExitStack()
            s.st.__enter__()
            s.k.stack = s.st
            return s
        def __exit__(s, *a):
            s.k.barrier()
            s.k.stack = s.prev
            return s.st.__exit__(*a)
    return _Ph(self)


def _k_barrier(self):
    for e in self.ENGS:
        for e2 in self.ENGS:
            if e2 != e and self.count[e2] > 0:
                self._wait(e, (e2, self.count[e2]))
        for s in range(N_DMA_SEMS):
            if self.dma_val[s] > 0:
                self._wait(e, (s, self.dma_val[s]))


K.phase = _k_phase
K.barrier = _k_barrier


D = 1024
W = 256
NCTX = 256
NE = 16
GW = 64
bf16_np = ml_dtypes.bfloat16
NEG = -30000.0


def _rope_tables(n):
    t = np.arange(n)
    rows, cols = t // GW, t % GW
    inv = 10000.0 ** (-np.arange(16, dtype=np.float64) / 16)
    d = np.arange(128) % 64
    pos = np.where(d[:, None] < 32, rows[None, :], cols[None, :]).astype(np.float64)
    ang = pos * inv[d % 16][:, None]
    cos, sin = np.cos(ang), np.sin(ang)
    sgn = np.where((d % 32) < 16, -1.0, 1.0)[:, None]
    return np.stack([cos / 8, sin * sgn / 8, cos, sin * sgn]).astype(np.float32)


def _attn_geometry(n):
    R = n // GW
    wr = min(8, R)
    s = lambda r: int(np.clip(r - wr // 2, 0, R - wr))
    cst = np.clip(np.arange(GW) - 8, 0, GW - 16)
    uniq = {}
    ulist = []
    qtiles = []
    kc = np.tile(np.arange(GW), 2)
    ka = np.repeat(np.arange(2), GW)
    for qt in range(R // 2):
        r0 = 2 * qt
        lo, hi = s(r0), s(r0 + 1) + wr - 1
        lst = []
        for kt in range(lo // 2, hi // 2 + 1):
            kr = 2 * kt + ka
            qr = r0 + ka
            qc = kc
            srow = np.array([s(r) for r in qr])
            ok_r = (kr[:, None] >= srow[None, :]) & (kr[:, None] < srow[None, :] + wr)
            ok_c = (kc[:, None] >= cst[qc][None, :]) & (kc[:, None] < cst[qc][None, :] + 16)
            mask = ok_r & ok_c
            if not mask.any():
                continue
            dr = np.clip(kr[:, None] - qr[None, :] + 7, 0, 14)
            dc = np.clip(kc[:, None] - qc[None, :], -15, 15) + 15
            key = (2 * kt - r0, mask.tobytes())
            if key not in uniq:
                uniq[key] = len(ulist)
                ulist.append((dr, dc, mask))
            lst.append((kt, uniq[key]))
        qtiles.append(lst)
    return ulist, qtiles


def _bias_tiles(rpb_l, ulist):
    out = np.empty((4, len(ulist), 128, 128), np.float32)
    for u, (dr, dc, mask) in enumerate(ulist):
        for h in range(4):
            out[h, u] = np.where(mask, rpb_l[h][dr, dc], NEG)
    return np.ascontiguousarray(out.transpose(2, 0, 1, 3).reshape(128, 4 * len(ulist), 128)).astype(bf16_np)


def _dft_consts(n):
    N1 = n // 128
    sc = 1.0 / 8.0
    dd = np.arange(64)
    ph = 2 * np.pi * np.outer(dd, dd) / 64
    Cd, Sd = np.cos(ph) * sc, np.sin(ph) * sc
    cs = np.zeros((128, 256))
    for g in range(2):
        cs[g * 64:(g + 1) * 64, g * 64:(g + 1) * 64] = Cd
        cs[g * 64:(g + 1) * 64, 128 + g * 64:128 + (g + 1) * 64] = -Sd
    c1 = np.arange(N1)
    p1 = 2 * np.pi * np.outer(c1, c1) / N1
    f1 = np.stack([np.cos(p1), np.sin(p1), -np.sin(p1)]) / np.sqrt(float(n))
    pp = np.arange(128)
    p2 = 2 * np.pi * np.outer(pp, pp) / 128
    f2 = np.stack([np.cos(p2), np.sin(p2)])
    pt = 2 * np.pi * np.outer(pp, c1) / n
    tw = np.stack([np.cos(pt), -np.sin(pt)]).astype(np.float32)
    return cs.astype(bf16_np), f1.astype(bf16_np), f2.astype(bf16_np), tw


def _route_consts():
    p = np.arange(128)
    same = (p[:, None] // 8) == (p[None, :] // 8)
    blk = same.astype(np.float32)
    low = (same & (p[:, None] < p[None, :])).astype(np.float32)
    iota = np.broadcast_to(np.arange(32, dtype=np.float32), (128, NE, 32)).copy()
    return blk, low, iota


A_END, B_END, C_END, Q_END, K_END, V_END = 512, 768, 1280, 1536, 1792, 2048
IN_COLS = 6144
GELU_C = 1.5957691216057308


class Stream:
    pass


def build_program(n, L=2, dbg=(), dbg_phases=None):
    nc = bass.Bass("TRN2", target_bir_lowering=False)
    N1 = n // 128
    cap = 2 * n // NE
    capc = 2 * NCTX // NE
    st = ExitStack()
    k = K(nc, st)
    ins = {}

    def ext(name, shape, dtype=F32):
        h = nc.dram_tensor(name, list(shape), dtype, kind="ExternalInput")
        ins[name] = T(h, name)
        return ins[name]

    def scr(name, shape, dtype):
        kind = "ExternalOutput" if name in dbg else "Internal"
        h = nc.dram_tensor(name, list(shape), dtype, kind=kind)
        return T(h, name)

    x_in = ext("x", [n, D]); ctx_in = ext("ctx", [NCTX, D])
    c_in = ext("c_pk", [128, 8]); cc_in = ext("cctx_pk", [128, 8])
    w_mod = ext("w_mod", [L, D, 6 * D]); b_mod = ext("b_mod", [L, 6 * D])
    n1g = ext("norm1_g", [L, D]); n2g = ext("norm2_g", [L, D]); fng = ext("final_norm_g", [1, D])
    w_in = ext("w_in", [L, D, IN_COLS]); w_qkp = ext("w_qkp", [L, D, 512])
    sgu_g = ext("sgu_ln_g", [L, W]); sgu_b = ext("sgu_ln_b", [L, W])
    wsT = ext("wsT", [L, 128, 4, 128]); bsp = ext("b_spatial", [L, 4, 128])
    w_br = [ext(nm, [L, W, D]) for nm in ("w_a_out", "w_b_out", "w_c_out", "w_d_out")]
    conv_wT = ext("conv_wT", [L, W, 31]); conv_b = ext("conv_b_c", [L, W, 1])
    cln_g = ext("conv_ln_g_c", [L, W, 1]); cln_b = ext("conv_ln_b_c", [L, W, 1])
    w_out = ext("w_out", [L, D, D]); w_router = ext("w_router", [L, D, NE])
    w_gate = ext("w_gate_e", [L, NE, D, D]); w_up = ext("w_up_e", [L, NE, D, D]); w_down = ext("w_down_e", [L, NE, D, D])
    id_bf = ext("id_bf", [128, 128], BF16); id_f = ext("id_f", [128, 128])
    rope = ext("rope", [4, 128, n])
    ulist, qtiles = _attn_geometry(n)
    U = len(ulist)
    biasT = ext("biasT", [L, 128, 4 * U, 128], BF16)
    cs_c = ext("dft_cs", [128, 256], BF16)
    f1x = ext("dft_f1x", [3, N1, N1], BF16); f1c = ext("dft_f1c", [3, 2, 2], BF16)
    f2 = ext("dft_f2", [2, 128, 128], BF16)
    twx = ext("dft_twx", [2, 128, N1]); twc = ext("dft_twc", [2, 128, 2])
    r_blk = ext("r_blk", [128, 128]); r_low = ext("r_low", [128, 128]); r_iota = ext("r_iota", [128, NE, 32])
    tidx = ext("tid_x", [128, N1]); tidc = ext("tid_c", [128, 2])
    out_h = nc.dram_tensor("out", [n, D], F32, kind="ExternalOutput")
    OUT = T(out_h, "out")

    def mk_stream(si, nn, name, xin):
        S = Stream()
        S.si, S.n, S.name, S.N1 = si, nn, name, nn // 128
        S.cap = 2 * nn // NE
        S.Xin = xin
        S.XA = scr(name + "_XA", [nn, D], F32); S.XB = scr(name + "_XB", [nn, D], F32)
        S.hT = scr(name + "_hT", [D, nn], BF16)
        for nm in ("uT", "yT", "qrT", "qpT", "krT", "aT", "bT", "cT", "dT"):
            setattr(S, nm, scr(name + "_" + nm, [W, nn], BF16))
        for nm in ("vln", "Zr", "Zi", "v"):
            setattr(S, nm, scr(name + "_" + nm, [nn, W], BF16))
        S.gT = scr(name + "_gT", [4 * D, nn], BF16)
        S.Y1r = scr(name + "_Y1r", [S.N1, 128 * W], BF16); S.Y1i = scr(name + "_Y1i", [S.N1, 128 * W], BF16)
        S.h2D = scr(name + "_h2D", [nn, D], BF16)
        S.affD = scr(name + "_affD", [NE, nn], F32)
        S.slotD = scr(name + "_slotD", [NE, nn], F32)
        S.idxD = scr(name + "_idxD", [NE, S.cap], I32)
        S.gateD = scr(name + "_gateD", [NE, S.cap], F32)
        S.f1 = f1x if si == 0 else f1c
        S.tw = twx if si == 0 else twc
        S.tid = tidx if si == 0 else tidc
        return S

    SX = mk_stream(0, n, "sx", x_in)
    SC = mk_stream(1, NCTX, "sc", ctx_in)
    modD = [scr("modD%d" % l, [2, 6 * D], F32) for l in range(L)]

    def mmul(out, lhsT, rhs, start, stop, reads, writes):
        k.op("pe", lambda e: e.matmul(out, lhsT=lhsT, rhs=rhs, start=start, stop=stop), reads=reads, writes=writes, signal=bool(stop))

    def load_ident(bf=True, f=False):
        r = []
        if bf:
            t = k.sbuf("identb", [128, 128], BF16); k.dma("sp", t[:], id_bf.ap.ap(), reads=[id_bf], writes=[t]); r.append(t)
        if f:
            t = k.sbuf("identf", [128, 128], F32); k.dma("sp", t[:], id_f.ap.ap(), reads=[id_f], writes=[t]); r.append(t)
        return r

    def mod_phase(l):
        with k.phase():
            cs = k.sbuf("mod_cs", [128, 2, 8], F32)
            k.dma("sp", cs[:, 0, :], c_in.ap.ap(), reads=[c_in], writes=[cs])
            k.dma("sp", cs[:, 1, :], cc_in.ap.ap(), reads=[cc_in], writes=[cs])
            scs = k.sbuf("mod_scs", [128, 2, 8], F32)
            k.op("act", lambda e: e.activation(out=scs[:], in_=cs[:], func=AF.Silu), reads=[cs], writes=[scs])
            wp = Pool(k, "mod_w", 4, [128, 8, 512], F32)
            pp = Pool(k, "mod_ps", 2, [2, 512], F32, "psum")
            bp = Pool(k, "mod_b", 4, [2, 512], F32)
            rp = Pool(k, "mod_r", 4, [2, 512], F32)
            ldq = {}
            def mod_loads(blk):
                wm = wp.next(); bm = bp.next()
                cols = slice(blk * 512, (blk + 1) * 512)
                k.dma("sp", wm[:], w_mod.ap[l, :, cols].rearrange("(c p) n -> p c n", p=128), reads=[w_mod], writes=[wm])
                k.dma("sp", bm[:], b_mod.ap[l:l + 1, cols].to_broadcast([2, 512]), reads=[b_mod], writes=[bm])
                ldq[blk] = (wm, bm)
            for b0 in range(3):
                mod_loads(b0)
            for blk in range(12):
                if blk + 3 < 12:
                    mod_loads(blk + 3)
                wm, bm = ldq.pop(blk)
                ps = pp.next(); rs = rp.next()
                cols = slice(blk * 512, (blk + 1) * 512)
                for c in range(8):
                    mmul(ps[:], scs[:, :, c], wm[:, c, :], c == 0, c == 7, [scs, wm], [ps])
                k.op("dve", lambda e, rs=rs, ps=ps, bm=bm: e.tensor_tensor(out=rs[:], in0=ps[:], in1=bm[:], op=ALU.add), reads=[ps, bm], writes=[rs])
                k.dma("sp", modD[l].ap[:, cols], rs[:], reads=[rs], writes=[modD[l]], acc_w=True)

    def bc_load(dst, src_t, row_ap, F):
        k.dma("sp", dst[:], row_ap.to_broadcast([128, F]), reads=[src_t], writes=[dst])

    def norm_phase(S, l, kind):
        nn, si = S.n, S.si
        with k.phase():
            scale_b = k.sbuf("nm_scale", [128, D], F32)
            if kind == 3:
                bc_load(scale_b, fng, fng.ap[0:1, :], D)
            else:
                g = n1g if kind == 1 else n2g
                o = 0 if kind == 1 else 3
                gb = k.sbuf("nm_g", [128, D], F32); scb = k.sbuf("nm_sc", [128, D], F32)
                shift_b = k.sbuf("nm_shift", [128, D], F32)
                bc_load(gb, g, g.ap[l:l + 1, :], D)
                bc_load(scb, modD[l], modD[l].ap[si:si + 1, (o + 1) * D:(o + 2) * D], D)
                bc_load(shift_b, modD[l], modD[l].ap[si:si + 1, o * D:(o + 1) * D], D)
                k.op("dve", lambda e: e.scalar_tensor_tensor(out=scale_b[:], in0=scb[:], scalar=1.0, in1=gb[:], op0=ALU.add, op1=ALU.mult), reads=[scb, gb], writes=[scale_b])
            if kind == 1:
                identb, = load_ident(True, False)
            if kind == 2:
                identf, = load_ident(False, True)
                rt = k.sbuf("nm_router", [128, 8, NE], F32)
                k.dma("sp", rt[:], w_router.ap[l].rearrange("(c p) e -> p c e", p=128), reads=[w_router], writes=[rt])
                ones16 = k.sbuf("nm_ones16", [NE, NE], F32)
                k.op("dve", lambda e: e.memset(ones16[:], 1.0), writes=[ones16])
                aff_all = k.sbuf("nm_aff", [NE, nn], F32)
            xp = Pool(k, "nm_x", 5, [128, D], F32)
            jp = Pool(k, "nm_junk", 2, [128, D], BF16)
            sp_ = Pool(k, "nm_st", 3, [128, 4], F32)
            tp = Pool(k, "nm_tmp", 3, [128, D], F32)
            hp = Pool(k, "nm_h", 3, [128, D], BF16 if kind == 1 else F32)
            if kind == 1:
                psT = Pool(k, "nm_psT", 2, [128, 8, 128], BF16, "psum")
                hTp = Pool(k, "nm_hT", 2, [128, 8, 128], BF16)
            if kind == 2:
                hbp = Pool(k, "nm_hb", 2, [128, D], BF16)
                psT = Pool(k, "nm_psT", 2, [128, 4, 128], F32, "psum")
                hTp = Pool(k, "nm_hT", 2, [128, 8, 128], F32)
                psl = Pool(k, "nm_psl", 2, [NE, 512], F32, "psum")
                rstate = {}
                e_p = Pool(k, "nm_e", 2, [NE, 512], F32)
                r_p = Pool(k, "nm_r", 2, [NE, 512], F32)
            xq = {}
            def stage0(i):
                    xt = xp.next()
                    k.dma("sp", xt[:], S.Xin.ap[i * 128:(i + 1) * 128, :], reads=[S.Xin], writes=[xt])
                    xq[i] = xt
            def stage1(i):
                    rows = slice(i * 128, (i + 1) * 128)
                    xt = xq.pop(i); jk = jp.next(); s4 = sp_.next(); tmp = tp.next(); h = hp.next()
                    k.op("act", lambda e, xt=xt, jk=jk, s4=s4: e.activation(out=jk[:], in_=xt[:], func=AF.Square, accum_out=s4[:, 0:1]), reads=[xt], writes=[jk, s4])
                    xs = tmp
                    if kind != 3:
                        k.op("pool", lambda e, xt=xt, xs=xs: e.tensor_tensor(out=xs[:], in0=xt[:], in1=scale_b[:], op=ALU.mult), reads=[xt, scale_b], writes=[xs])
                    k.op("act", lambda e, s4=s4: e.activation(out=s4[:, 1:2], in_=s4[:, 0:1], func=AF.Sqrt, bias=1e-6, scale=1.0 / D), reads=[s4], writes=[s4])
                    k.op("dve", lambda e, s4=s4: e.reciprocal(out=s4[:, 2:3], in_=s4[:, 1:2]), reads=[s4], writes=[s4])
                    if kind == 3:
                        k.op("dve", lambda e, xt=xt, s4=s4, h=h: e.scalar_tensor_tensor(out=h[:], in0=xt[:], scalar=s4[:, 2:3], in1=scale_b[:], op0=ALU.mult, op1=ALU.mult), reads=[xt, s4, scale_b], writes=[h])
                        k.dma("sp", OUT.ap[rows, :], h[:], reads=[h], writes=[OUT], acc_w=True)
                        return None
                    k.op("dve", lambda e, tmp=tmp, xs=xs, s4=s4, h=h: e.scalar_tensor_tensor(out=h[:], in0=xs[:], scalar=s4[:, 2:3], in1=shift_b[:], op0=ALU.mult, op1=ALU.add), reads=[xs, s4, shift_b], writes=[h])
                    return (rows, h)
            def stage2(ctx_):
                    rows, h = ctx_
                    if kind == 1:
                        pt = psT.next(); hT = hTp.next()
                        for c in range(8):
                            k.op("pe", lambda e, c=c, pt=pt, h=h: e.transpose(out=pt[:, c, :], in_=h[:, c * 128:(c + 1) * 128], identity=identb[:]), reads=[h, identb], writes=[pt], signal=(c == 7))
                        k.op("act", lambda e, pt=pt, hT=hT: e.activation(out=hT[:], in_=pt[:], func=AF.Copy), reads=[pt], writes=[hT])
                        k.dma("sp", S.hT.ap.ap().rearrange("(c p) t -> p c t", p=128)[:, :, rows], hT[:], reads=[hT], writes=[S.hT], acc_w=True)
                    else:
                        hb = hbp.next(); hT = hTp.next()
                        k.op("act", lambda e, hb=hb, h=h: e.activation(out=hb[:], in_=h[:], func=AF.Copy), reads=[h], writes=[hb])
                        k.dma("sp", S.h2D.ap[rows, :], hb[:], reads=[hb], writes=[S.h2D], acc_w=True)
                        for half in range(2):
                            pt = psT.next()
                            for c in range(4):
                                cc = half * 4 + c
                                k.op("pe", lambda e, c=c, cc=cc, pt=pt, h=h: e.transpose(out=pt[:, c, :], in_=h[:, cc * 128:(cc + 1) * 128], identity=identf[:]), reads=[h, identf], writes=[pt], signal=(c == 3))
                            k.op("dve", lambda e, pt=pt, hT=hT, half=half: e.tensor_copy(out=hT[:, half * 4:(half + 1) * 4, :], in_=pt[:]), reads=[pt], writes=[hT])
                        i_ = rows.start // 128
                        if i_ % 4 == 0:
                            rstate["pl"] = psl.next()
                        pl = rstate["pl"]
                        for c in range(8):
                            mmul(pl[:, (i_ % 4) * 128:(i_ % 4 + 1) * 128], rt[:, c, :], hT[:, c, :], c == 0, c == 7, [rt, hT], [pl])
                        if i_ % 4 == 3 or i_ == nn // 128 - 1:
                            nb_ = (i_ % 4 + 1) * 128
                            c0 = (i_ // 4) * 512
                            ee = e_p.next(); rr = r_p.next()
                            k.op("act", lambda e, pl=pl, ee=ee, nb_=nb_: e.activation(out=ee[:, :nb_], in_=pl[:, :nb_], func=AF.Exp), reads=[pl], writes=[ee])
                            pl2 = psl.next()
                            mmul(pl2[:, :nb_], ones16[:], ee[:, :nb_], True, True, [ones16, ee], [pl2])
                            k.op("dve", lambda e, pl2=pl2, rr=rr, nb_=nb_: e.reciprocal(out=rr[:, :nb_], in_=pl2[:, :nb_]), reads=[pl2], writes=[rr])
                            k.op("dve", lambda e, ee=ee, rr=rr, nb_=nb_, c0=c0: e.tensor_tensor(out=aff_all[:, c0:c0 + nb_], in0=ee[:, :nb_], in1=rr[:, :nb_], op=ALU.mult), reads=[ee, rr], writes=[aff_all])

            pend = None
            NTL = nn // 128
            stage0(0)
            if NTL > 1:
                stage0(1)
            for i in range(NTL):
                if i + 2 < NTL:
                    stage0(i + 2)
                cur = stage1(i)
                if pend is not None:
                    stage2(pend)
                pend = cur
            if pend is not None:
                stage2(pend)
            if kind == 2:
                k.dma("sp", S.affD.ap.ap(), aff_all[:], reads=[aff_all], writes=[S.affD])

    def inproj_phase(specs, l):
        with k.phase():
            wb = k.sbuf("ip_w", [128, 8, IN_COLS], BF16)
            for c in range(8):
                k.dma("pool", wb[:, c, :], w_in.ap[l, c * 128:(c + 1) * 128, :], reads=[w_in], writes=[wb], acc_w=True)
            wp_ = k.sbuf("ip_wp", [128, 8, 512], BF16)
            k.dma("pool", wp_[:], w_qkp.ap[l].rearrange("(c p) n -> p c n", p=128), reads=[w_qkp], writes=[wp_])
            csb = k.sbuf("ip_cs", [128, 256], BF16)
            k.dma("sp", csb[:], cs_c.ap.ap(), reads=[cs_c], writes=[csb])
            lg = k.sbuf("ip_lg", [128, W], F32); lb = k.sbuf("ip_lb", [128, W], F32)
            bc_load(lg, sgu_g, sgu_g.ap[l:l + 1, :], W); bc_load(lb, sgu_b, sgu_b.ap[l:l + 1, :], W)
            for S, kv_only in specs:
              nn, si = S.n, S.si
              TT = 512 if nn >= 512 else nn
              with k.phase():
                hp = Pool(k, "ip_h", 2, [128, 8, TT], BF16)
                ps = Pool(k, "ip_ps", 8, [128, 512], F32, "psum")
                ev = Pool(k, "ip_ev", 4, [128, TT], BF16)
                f32p = Pool(k, "ip_f32", 4, [128, TT], F32)
                rp = Pool(k, "ip_rope", 2, [128, 4, TT], F32)
                zbp = Pool(k, "ip_zb", 2, [128, 2, TT], BF16)
                tmv = Pool(k, "ip_tmv", 3, [128, 512], F32)
                tmb = Pool(k, "ip_tmb", 3, [128, 512], BF16)
                stp = Pool(k, "ip_st", 3, [128, 8], F32)

                def fm(wt, col0, h, reads_w):
                    p = ps.next()
                    for c in range(8):
                        mmul(p[:, :TT], wt[:, c, col0:col0 + 128], h[:, c, :], c == 0, c == 7, [reads_w, h], [p])
                    return p

                def gelu_to(dst_ap, p, width, reads_extra, writes):
                    sq = f32p.next(); t2 = f32p.next()
                    k.op("act", lambda e: e.activation(out=sq[:, :width], in_=p[:, :width], func=AF.Square), reads=[p], writes=[sq])
                    k.op("dve", lambda e: e.tensor_scalar(out=sq[:, :width], in0=sq[:, :width], scalar1=0.044715, scalar2=1.0, op0=ALU.mult, op1=ALU.add), reads=[sq], writes=[sq])
                    k.op("dve", lambda e: e.tensor_tensor(out=t2[:, :width], in0=p[:, :width], in1=sq[:, :width], op=ALU.mult), reads=[p, sq], writes=[t2])
                    k.op("act", lambda e: e.activation(out=t2[:, :width], in_=t2[:, :width], func=AF.Sigmoid, scale=GELU_C), reads=[t2], writes=[t2])
                    k.op("dve", lambda e: e.tensor_tensor(out=dst_ap, in0=p[:, :width], in1=t2[:, :width], op=ALU.mult), reads=[p, t2], writes=writes)

                def ip_loads(j):
                    tk = slice(j * TT, (j + 1) * TT)
                    h = hp.next()
                    k.dma("sp", h[:], S.hT.ap.ap().rearrange("(c p) t -> p c t", p=128)[:, :, tk], reads=[S.hT], writes=[h])
                    rt = None
                    if si == 0:
                        rt = rp.next()
                        k.dma("sp", rt[:], rope.ap.ap().rearrange("f p t -> p f t")[:, :, tk], reads=[rope], writes=[rt])
                    return h, rt
                nxt = ip_loads(0)
                for j in range(nn // TT):
                    tk = slice(j * TT, (j + 1) * TT)
                    h, rt = nxt
                    if j + 1 < nn // TT:
                        nxt = ip_loads(j + 1)
                    fmv = lambda T_: T_.ap.ap().rearrange("(c p) t -> p c t", p=128)
                    if not kv_only:
                        zb = zbp.next()
                        for ch in range(2):
                            p = fm(wb, A_END + ch * 128, h, wb)
                            k.op("act", lambda e, p=p, zb=zb, ch=ch, TT=TT: e.activation(out=zb[:, ch, :], in_=p[:, :TT], func=AF.Copy), reads=[p], writes=[zb])
                    for s_ in range(TT // 128):
                        trow = slice(j * TT + s_ * 128, j * TT + (s_ + 1) * 128)
                        hs = lambda c: h[:, c, s_ * 128:(s_ + 1) * 128]
                        p = ps.next()
                        for c in range(8):
                            mmul(p[:, 0:256], hs(c), wb[:, c, K_END:V_END], c == 0, c == 7, [h, wb], [p])
                        if not kv_only:
                            for c in range(8):
                                mmul(p[:, 256:512], hs(c), wb[:, c, 256:512], c == 0, c == 7, [h, wb], [p])
                        ov = tmb.next()
                        k.op("act", lambda e, ov=ov, p=p: e.activation(out=ov[:, 0:256], in_=p[:, 0:256], func=AF.Copy), reads=[p], writes=[ov])
                        k.dma("sp", S.v.ap[trow, :], ov[:, 0:256], reads=[ov], writes=[S.v], acc_w=True)
                        if kv_only:
                            continue
                        gv = tmv.next(); s8 = stp.next(); ol = tmb.next()
                        sq = f32p.next(); t2 = f32p.next()
                        pv = p
                        k.op("act", lambda e, sq=sq, pv=pv: e.activation(out=sq[:, :256], in_=pv[:, 256:512], func=AF.Square), reads=[pv], writes=[sq])
                        k.op("dve", lambda e, sq=sq: e.tensor_scalar(out=sq[:, :256], in0=sq[:, :256], scalar1=0.044715, scalar2=1.0, op0=ALU.mult, op1=ALU.add), reads=[sq], writes=[sq])
                        k.op("dve", lambda e, sq=sq, t2=t2, pv=pv: e.tensor_tensor(out=t2[:, :256], in0=pv[:, 256:512], in1=sq[:, :256], op=ALU.mult), reads=[pv, sq], writes=[t2])
                        k.op("act", lambda e, t2=t2: e.activation(out=t2[:, :256], in_=t2[:, :256], func=AF.Sigmoid, scale=GELU_C), reads=[t2], writes=[t2])
                        k.op("dve", lambda e, gv=gv, t2=t2, pv=pv: e.tensor_tensor(out=gv[:, :256], in0=pv[:, 256:512], in1=t2[:, :256], op=ALU.mult), reads=[pv, t2], writes=[gv])
                        k.op("dve", lambda e, gv=gv, s8=s8: e.bn_stats(out=s8[:, 0:6], in_=gv[:, :256]), reads=[gv], writes=[s8])
                        k.op("dve", lambda e, s8=s8: e.bn_aggr(out=s8[:, 6:8], in_=s8[:, 0:6]), reads=[s8], writes=[s8])
                        k.op("act", lambda e, s8=s8: e.activation(out=s8[:, 0:1], in_=s8[:, 7:8], func=AF.Sqrt, bias=1e-6, scale=1.0), reads=[s8], writes=[s8])
                        k.op("dve", lambda e, s8=s8: e.reciprocal(out=s8[:, 1:2], in_=s8[:, 0:1]), reads=[s8], writes=[s8])
                        k.op("dve", lambda e, gv=gv, s8=s8: e.tensor_scalar(out=gv[:, :256], in0=gv[:, :256], scalar1=s8[:, 6:7], scalar2=s8[:, 1:2], op0=ALU.subtract, op1=ALU.mult), reads=[gv, s8], writes=[gv])
                        k.op("pool", lambda e, gv=gv: e.tensor_tensor(out=gv[:, :256], in0=gv[:, :256], in1=lg[:], op=ALU.mult), reads=[gv, lg], writes=[gv])
                        k.op("pool", lambda e, gv=gv, ol=ol: e.tensor_tensor(out=ol[:, :256], in0=gv[:, :256], in1=lb[:], op=ALU.add), reads=[gv, lb], writes=[ol])
                        k.dma("sp", S.vln.ap[trow, :], ol[:, :256], reads=[ol], writes=[S.vln], acc_w=True)
                        pz = ps.next()
                        for ch in range(2):
                            mmul(pz[:, ch * 256:(ch + 1) * 256], zb[:, ch, s_ * 128:(s_ + 1) * 128], csb[:], True, True, [zb, csb], [pz])
                        oz = tmb.next()
                        k.op("act", lambda e, oz=oz, pz=pz: e.activation(out=oz[:], in_=pz[:], func=AF.Copy), reads=[pz], writes=[oz])
                        ozv = oz[:].rearrange("p (c r f) -> p c r f", c=2, r=2)
                        k.dma("sp", S.Zr.ap[trow, :].rearrange("t (c f) -> t c f", c=2), ozv[:, :, 0, :], reads=[oz], writes=[S.Zr], acc_w=True)
                        k.dma("sp", S.Zi.ap[trow, :].rearrange("t (c f) -> t c f", c=2), ozv[:, :, 1, :], reads=[oz], writes=[S.Zi], acc_w=True)
                    if not kv_only:
                        for ch in range(2):
                            p = fm(wb, ch * 128, h, wb); o = ev.next()
                            gelu_to(o[:], p, TT, [], [o])
                            k.dma("sp", fmv(S.uT)[:, ch, tk], o[:], reads=[o], writes=[S.uT], acc_w=True)
                        for ch in range(2):
                            pa = fm(wb, B_END + ch * 128, h, wb); pg = fm(wb, B_END + 256 + ch * 128, h, wb)
                            sg = f32p.next(); o = ev.next()
                            k.op("act", lambda e, sg=sg, pg=pg, TT=TT: e.activation(out=sg[:], in_=pg[:, :TT], func=AF.Sigmoid), reads=[pg], writes=[sg])
                            k.op("dve", lambda e, o=o, pa=pa, sg=sg, TT=TT: e.tensor_tensor(out=o[:], in0=pa[:, :TT], in1=sg[:], op=ALU.mult), reads=[pa, sg], writes=[o])
                            k.dma("sp", fmv(S.yT)[:, ch, tk], o[:], reads=[o], writes=[S.yT], acc_w=True)
                    for which, col0, pc0, dstr, dstp in ((0, C_END, 0, S.qrT, S.qpT), (1, Q_END, 256, S.krT, None)):
                        if kv_only and which == 0:
                            continue
                        for ch in range(2):
                            p = fm(wb, col0 + ch * 128, h, wb)
                            if si == 0:
                                pp_ = fm(wp_, pc0 + ch * 128, h, wp_)
                                t1 = f32p.next(); t2 = f32p.next(); o = ev.next()
                                k.op("dve", lambda e, t1=t1, p=p, rt=rt, which=which, TT=TT: e.tensor_tensor(out=t1[:], in0=p[:, :TT], in1=rt[:, 2 * which, :], op=ALU.mult), reads=[p, rt], writes=[t1])
                                k.op("dve", lambda e, t2=t2, pp_=pp_, rt=rt, which=which, TT=TT: e.tensor_tensor(out=t2[:], in0=pp_[:, :TT], in1=rt[:, 2 * which + 1, :], op=ALU.mult), reads=[pp_, rt], writes=[t2])
                                k.op("pool", lambda e, o=o, t1=t1, t2=t2: e.tensor_tensor(out=o[:], in0=t1[:], in1=t2[:], op=ALU.add), reads=[t1, t2], writes=[o])
                                k.dma("sp", fmv(dstr)[:, ch, tk], o[:], reads=[o], writes=[dstr], acc_w=True)
                                if which == 0:
                                    o2 = ev.next()
                                    k.op("act", lambda e, o2=o2, p=p, TT=TT: e.activation(out=o2[:], in_=p[:, :TT], func=AF.Copy, scale=0.125), reads=[p], writes=[o2])
                                    k.dma("sp", fmv(dstp)[:, ch, tk], o2[:], reads=[o2], writes=[dstp], acc_w=True)
                            else:
                                o = ev.next()
                                if which == 0:
                                    k.op("act", lambda e, o=o, p=p, TT=TT: e.activation(out=o[:], in_=p[:, :TT], func=AF.Copy, scale=0.125), reads=[p], writes=[o])
                                    k.dma("sp", fmv(S.qpT)[:, ch, tk], o[:], reads=[o], writes=[S.qpT], acc_w=True)
                                else:
                                    k.op("act", lambda e, o=o, p=p, TT=TT: e.activation(out=o[:], in_=p[:, :TT], func=AF.Copy), reads=[p], writes=[o])
                                    k.dma("sp", fmv(S.krT)[:, ch, tk], o[:], reads=[o], writes=[S.krT], acc_w=True)
                    if not kv_only:
                        for ch in range(32):
                            p = fm(wb, V_END + ch * 128, h, wb); o = ev.next()
                            k.op("act", lambda e, p=p, o=o, TT=TT: e.activation(out=o[:], in_=p[:, :TT], func=AF.Sigmoid), reads=[p], writes=[o])
                            k.dma("sp", S.gT.ap.ap().rearrange("(c p) t -> p c t", p=128)[:, ch, tk], o[:], reads=[o], writes=[S.gT], acc_w=True)

    def gmlp_phase(S, l):
        nn = S.n
        TT = 512 if nn >= 512 else nn
        NCH = TT // 128
        fmv = lambda T_: T_.ap.ap().rearrange("(c p) t -> p c t", p=128)
        with k.phase():
            ws = k.sbuf("gm_ws", [128, 4, 128], BF16)
            k.dma("pool", ws[:], wsT.ap[l], reads=[wsT], writes=[ws])
            bs = k.sbuf("gm_bs", [1, 4, 128], BF16)
            k.dma("pool", bs[:], bsp.ap[l:l + 1], reads=[bsp], writes=[bs])
            ones = k.sbuf("gm_ones", [1, 128], BF16)
            k.op("dve", lambda e: e.memset(ones[:], 1.0), writes=[ones])
            vp = Pool(k, "gm_v", 2, [128, NCH, W], BF16)
            up = Pool(k, "gm_u", 2, [128, 2, TT], BF16)
            ap_ = Pool(k, "gm_a", 2, [128, 2, TT], BF16)
            pp = Pool(k, "gm_ps", 8, [128, NCH, 128], F32, "psum")
            def gm_loads(j):
                tk = slice(j * TT, (j + 1) * TT)
                vt = vp.next(); ut = up.next()
                k.dma("sp", vt[:], S.vln.ap[tk, :].rearrange("(c p) f -> p c f", p=128), reads=[S.vln], writes=[vt])
                k.dma("sp", ut[:], fmv(S.uT)[:, :, tk], reads=[S.uT], writes=[ut])
                return vt, ut
            nxt = gm_loads(0)
            for j in range(nn // TT):
                tk = slice(j * TT, (j + 1) * TT)
                vt, ut = nxt
                if j + 1 < nn // TT:
                    nxt = gm_loads(j + 1)
                at = ap_.next()
                for hf in range(2):
                    for gi in range(2):
                        g = 2 * hf + gi
                        p = pp.next()
                        for cc in range(NCH):
                            mmul(p[:, cc, :], vt[:, cc, hf * 128:(hf + 1) * 128], ws[:, g, :], True, False, [vt, ws], [p])
                            mmul(p[:, cc, :], ones[:], bs[:, g, :], False, True, [ones, bs], [p])
                        pr = slice(gi * 64, (gi + 1) * 64)
                        k.op("dve", lambda e, at=at, p=p, ut=ut, pr=pr, hf=hf: e.tensor_tensor(out=at[pr, hf, :].rearrange("p (c q) -> p c q", q=128), in0=p[pr, :, :], in1=ut[pr, hf, :].rearrange("p (c q) -> p c q", q=128), op=ALU.mult), reads=[p, ut], writes=[at])
                k.dma("sp", fmv(S.aT)[:, :, tk], at[:], reads=[at], writes=[S.aT], acc_w=True)

    def conv_phase(S, l):
        nn = S.n
        TT = 512 if nn >= 512 else nn
        fmv = lambda T_: T_.ap.ap().rearrange("(c p) t -> p c t", p=128)
        with k.phase():
            identf, = load_ident(False, True)
            cw = k.sbuf("cv_w", [128, 2, 31], F32)
            k.dma("sp", cw[:], conv_wT.ap[l].rearrange("(h p) j -> p h j", p=128), reads=[conv_wT], writes=[cw])
            prm = k.sbuf("cv_prm", [128, 3, 2], F32)
            for i_, src in enumerate((conv_b, cln_g, cln_b)):
                for hf in range(2):
                    k.dma("sp", prm[:, i_, hf:hf + 1], src.ap[l, hf * 128:(hf + 1) * 128, :], reads=[src], writes=[prm], acc_w=True)
            dg = k.sbuf("cv_diag", [128, 2, 31, 128], BF16)
            for hf in range(2):
                for j in range(31):
                    k.op("dve", lambda e, hf=hf, j=j: e.tensor_scalar(out=dg[:, hf, j, :], in0=identf[:], scalar1=cw[:, hf, j:j + 1], scalar2=None, op0=ALU.mult), reads=[identf, cw], writes=[dg])
            onesf = k.sbuf("cv_ones", [128, 128], F32)
            k.op("dve", lambda e: e.memset(onesf[:], 1.0 / W), writes=[onesf])
            yp = Pool(k, "cv_y", 2, [128, 2, TT + 30], BF16)
            pc = Pool(k, "cv_pc", 4, [128, TT], F32, "psum")
            pst = Pool(k, "cv_pst", 2, [128, 2, TT], F32, "psum")
            y2p = Pool(k, "cv_y2", 3, [128, 2, TT], F32)
            sqp = Pool(k, "cv_sq", 3, [128, 2, TT], F32)
            stp = Pool(k, "cv_st", 2, [128, 2, TT], F32)
            op_ = Pool(k, "cv_o", 2, [128, 2, TT], BF16)
            def cv_a(j):
                    t0 = j * TT
                    yt = yp.next()
                    lo, hi = max(0, t0 - 15), min(nn, t0 + TT + 15)
                    if lo > t0 - 15 or hi < t0 + TT + 15:
                        k.op("pool", lambda e, yt=yt: e.memset(yt[:], 0.0), writes=[yt])
                    k.dma("sp", yt[:, :, lo - (t0 - 15):hi - (t0 - 15)], fmv(S.yT)[:, :, lo:hi], reads=[S.yT], writes=[yt])
                    y2 = y2p.next(); sq = sqp.next()
                    for hf in range(2):
                        p = pc.next()
                        for jj in range(31):
                            mmul(p[:], dg[:, hf, jj, :], yt[:, hf, jj:jj + TT], jj == 0, jj == 30, [dg, yt], [p])
                        k.op("act", lambda e, y2=y2, p=p, hf=hf: e.activation(out=y2[:, hf, :], in_=p[:], func=AF.Identity, bias=prm[:, 0, hf:hf + 1], scale=1.0), reads=[p, prm], writes=[y2])
                        k.op("act", lambda e, y2=y2, sq=sq, hf=hf: e.activation(out=sq[:, hf, :], in_=y2[:, hf, :], func=AF.Square), reads=[y2], writes=[sq])
                    return (t0, y2, sq)
            def cv_b(ctx_):
                    t0, y2, sq = ctx_
                    ps_ = pst.next()
                    for hf in range(2):
                        mmul(ps_[:, 0, :], onesf[:], y2[:, hf, :], hf == 0, hf == 1, [onesf, y2], [ps_])
                    for hf in range(2):
                        mmul(ps_[:, 1, :], onesf[:], sq[:, hf, :], hf == 0, hf == 1, [onesf, sq], [ps_])
                    stt = stp.next(); ot = op_.next()
                    k.op("act", lambda e, stt=stt, ps_=ps_: e.activation(out=stt[:, 0, :], in_=ps_[:, 0, :], func=AF.Copy), reads=[ps_], writes=[stt])
                    k.op("dve", lambda e, stt=stt: e.tensor_tensor(out=stt[:, 1, :], in0=stt[:, 0, :], in1=stt[:, 0, :], op=ALU.mult), reads=[stt], writes=[stt])
                    k.op("dve", lambda e, stt=stt, ps_=ps_: e.tensor_tensor(out=stt[:, 1, :], in0=ps_[:, 1, :], in1=stt[:, 1, :], op=ALU.subtract), reads=[ps_, stt], writes=[stt])
                    k.op("act", lambda e, stt=stt: e.activation(out=stt[:, 1, :], in_=stt[:, 1, :], func=AF.Sqrt, bias=1e-6, scale=1.0), reads=[stt], writes=[stt])
                    k.op("dve", lambda e, stt=stt: e.reciprocal(out=stt[:, 1, :], in_=stt[:, 1, :]), reads=[stt], writes=[stt])
                    for hf in range(2):
                        k.op("dve", lambda e, y2=y2, stt=stt, hf=hf: e.tensor_tensor(out=y2[:, hf, :], in0=y2[:, hf, :], in1=stt[:, 0, :], op=ALU.subtract), reads=[y2, stt], writes=[y2])
                        k.op("pool", lambda e, y2=y2, stt=stt, hf=hf: e.tensor_tensor(out=y2[:, hf, :], in0=y2[:, hf, :], in1=stt[:, 1, :], op=ALU.mult), reads=[y2, stt], writes=[y2])
                        k.op("act", lambda e, y2=y2, ot=ot, hf=hf: e.activation(out=ot[:, hf, :], in_=y2[:, hf, :], func=AF.Silu, bias=prm[:, 2, hf:hf + 1], scale=prm[:, 1, hf:hf + 1]), reads=[y2, prm], writes=[ot])
                    k.dma("sp", fmv(S.cT)[:, :, t0:t0 + TT], ot[:], reads=[ot], writes=[S.cT], acc_w=True)
            pend = None
            for j in range(nn // TT):
                cur = cv_a(j)
                if pend is not None:
                    cv_b(pend)
                pend = cur
            cv_b(pend)

    def fourier_phase(S, l):
        nn, NN1 = S.n, S.N1
        fmv = lambda T_: T_.ap.ap().rearrange("(c p) t -> p c t", p=128)
        CB = 4096
        with k.phase():
            f1sb = k.sbuf("ff_f1", [NN1, 3, NN1], BF16)
            k.dma("sp", f1sb[:], S.f1.ap.ap().rearrange("m c q -> c m q"), reads=[S.f1], writes=[f1sb])
            zp = Pool(k, "ff_z", 4, [NN1, CB], BF16)
            yp = Pool(k, "ff_y", 4, [NN1, CB], BF16)
            pp = Pool(k, "ff_ps", 4, [NN1, 512], F32, "psum")
            Zrv = S.Zr.ap.ap().rearrange("(c p) f -> c (p f)", p=128)
            Ziv = S.Zi.ap.ap().rearrange("(c p) f -> c (p f)", p=128)
            def ff_loads(blk):
                cb = slice(blk * CB, (blk + 1) * CB)
                zr = zp.next(); zi = zp.next()
                k.dma("sp", zr[:], Zrv[:, cb], reads=[S.Zr], writes=[zr])
                k.dma("sp", zi[:], Ziv[:, cb], reads=[S.Zi], writes=[zi])
                return zr, zi
            nxt = ff_loads(0)
            for blk in range(128 * W // CB):
                cb = slice(blk * CB, (blk + 1) * CB)
                zr, zi = nxt
                if blk + 1 < 128 * W // CB:
                    nxt = ff_loads(blk + 1)
                yr = yp.next(); yi = yp.next()
                for sub in range(CB // 512):
                    cs_ = slice(sub * 512, (sub + 1) * 512)
                    pr = pp.next(); pi = pp.next()
                    mmul(pr[:], f1sb[:, 0, :], zr[:, cs_], True, False, [f1sb, zr], [pr])
                    mmul(pr[:], f1sb[:, 1, :], zi[:, cs_], False, True, [f1sb, zi], [pr])
                    mmul(pi[:], f1sb[:, 0, :], zi[:, cs_], True, False, [f1sb, zi], [pi])
                    mmul(pi[:], f1sb[:, 2, :], zr[:, cs_], False, True, [f1sb, zr], [pi])
                    k.op("act", lambda e, yr=yr, pr=pr, cs_=cs_: e.activation(out=yr[:, cs_], in_=pr[:], func=AF.Copy), reads=[pr], writes=[yr])
                    k.op("dve", lambda e, yi=yi, pi=pi, cs_=cs_: e.tensor_copy(out=yi[:, cs_], in_=pi[:]), reads=[pi], writes=[yi])
                k.dma("sp", S.Y1r.ap[:, cb], yr[:], reads=[yr], writes=[S.Y1r], acc_w=True)
                k.dma("sp", S.Y1i.ap[:, cb], yi[:], reads=[yi], writes=[S.Y1i], acc_w=True)
        with k.phase():
            KB = min(8, NN1)
            G = min(4, KB)
            f2sb = k.sbuf("ff_f2", [128, 2, 128], BF16)
            k.dma("sp", f2sb[:], f2.ap.ap().rearrange("m p q -> p m q"), reads=[f2], writes=[f2sb])
            tw = k.sbuf("ff_tw", [128, 2, NN1], F32)
            k.dma("sp", tw[:], S.tw.ap.ap().rearrange("m p q -> p m q"), reads=[S.tw], writes=[tw])
            outT = k.sbuf("ff_out", [128, 2, nn], BF16)
            yrp = Pool(k, "ff_yr", 2, [128, KB, W], BF16)
            yip = Pool(k, "ff_yi", 2, [128, KB, W], BF16)
            ypr = Pool(k, "ff_ypr", 2, [128, KB, W], BF16)
            ypi = Pool(k, "ff_ypi", 2, [128, KB, W], BF16)
            tp = Pool(k, "ff_t", 6, [128, W], F32)
            po = Pool(k, "ff_po", 4, [128, G, 128], F32, "psum")
            Y1rv = S.Y1r.ap.ap().rearrange("q (p f) -> p q f", p=128)
            Y1iv = S.Y1i.ap.ap().rearrange("q (p f) -> p q f", p=128)
            def f2_loads(kb):
                ks = slice(kb * KB, (kb + 1) * KB)
                yr = yrp.next(); yi = yip.next()
                k.dma("sp", yr[:], Y1rv[:, ks, :], reads=[S.Y1r], writes=[yr])
                k.dma("sp", yi[:], Y1iv[:, ks, :], reads=[S.Y1i], writes=[yi])
                return yr, yi
            nxt = f2_loads(0)
            for kb in range(NN1 // KB):
                ks = slice(kb * KB, (kb + 1) * KB)
                yr, yi = nxt
                if kb + 1 < NN1 // KB:
                    nxt = f2_loads(kb + 1)
                qr = ypr.next(); qi = ypi.next()
                for kk in range(KB):
                    k1 = kb * KB + kk
                    t1 = tp.next(); t2 = tp.next()
                    k.op("act", lambda e, t1=t1, yi=yi, kk=kk, k1=k1: e.activation(out=t1[:], in_=yi[:, kk, :], func=AF.Copy, scale=tw[:, 1, k1:k1 + 1]), reads=[yi, tw], writes=[t1])
                    k.op("dve", lambda e, t1=t1, yr=yr, qr=qr, kk=kk, k1=k1: e.scalar_tensor_tensor(out=qr[:, kk, :], in0=yr[:, kk, :], scalar=tw[:, 0, k1:k1 + 1], in1=t1[:], op0=ALU.mult, op1=ALU.subtract), reads=[yr, tw, t1], writes=[qr])
                    k.op("act", lambda e, t2=t2, yi=yi, kk=kk, k1=k1: e.activation(out=t2[:], in_=yi[:, kk, :], func=AF.Copy, scale=tw[:, 0, k1:k1 + 1]), reads=[yi, tw], writes=[t2])
                    k.op("dve", lambda e, t2=t2, yr=yr, qi=qi, kk=kk, k1=k1: e.scalar_tensor_tensor(out=qi[:, kk, :], in0=yr[:, kk, :], scalar=tw[:, 1, k1:k1 + 1], in1=t2[:], op0=ALU.mult, op1=ALU.add), reads=[yr, tw, t2], writes=[qi])
                for fh in range(2):
                    fs = slice(fh * 128, (fh + 1) * 128)
                    for g0 in range(0, KB, G):
                        p = po.next()
                        for gg in range(G):
                            kk = g0 + gg
                            mmul(p[:, gg, :], qr[:, kk, fs], f2sb[:, 0, :], True, False, [qr, f2sb], [p])
                            mmul(p[:, gg, :], qi[:, kk, fs], f2sb[:, 1, :], False, True, [qi, f2sb], [p])
                        k1_0 = kb * KB + g0
                        dst = outT[:, fh, :].rearrange("p (a b) -> p b a", b=NN1)[:, k1_0:k1_0 + G, :]
                        k.op("dve", lambda e, dst=dst, p=p: e.tensor_copy(out=dst, in_=p[:]), reads=[p], writes=[outT])
            k.dma("sp", fmv(S.bT), outT[:], reads=[outT], writes=[S.bT])

    def attention_phase(S, SCx, l):
        nn, si = S.n, S.si
        NT = nn // 128
        fmv = lambda T_: T_.ap.ap().rearrange("(c p) t -> p c t", p=128)
        with k.phase():
            identb, = load_ident(True, False)
            kc = k.sbuf("at_kc", [128, 2, NCTX], BF16)
            k.dma("sp", kc[:], fmv(SCx.krT), reads=[SCx.krT], writes=[kc])
            vc = k.sbuf("at_vc", [128, 2, 4, 65], BF16)
            k.op("dve", lambda e: e.memset(vc[:], 1.0), writes=[vc])
            for t_ in range(2):
                k.dma("sp", vc[:, t_, :, 0:64], SCx.v.ap[t_ * 128:(t_ + 1) * 128, :].rearrange("p (h d) -> p h d", h=4), reads=[SCx.v], writes=[vc], acc_w=True)
            if si == 0:
                kr = k.sbuf("at_kr", [128, 2, nn], BF16)
                k.dma("sp", kr[:], fmv(S.krT), reads=[S.krT], writes=[kr])
                vs = k.sbuf("at_v", [128, NT, 4, 65], BF16)
                k.op("dve", lambda e: e.memset(vs[:], 1.0), writes=[vs])
                for t_ in range(NT):
                    k.dma("sp", vs[:, t_, :, 0:64], S.v.ap[t_ * 128:(t_ + 1) * 128, :].rearrange("p (h d) -> p h d", h=4), reads=[S.v], writes=[vs], acc_w=True)
                bias = k.sbuf("at_bias", [128, 4 * U, 128], BF16)
                k.dma("sp", bias[:], biasT.ap[l], reads=[biasT], writes=[bias])
            qrp = Pool(k, "at_qr", 3, [128, 2, 128], BF16)
            qpp = Pool(k, "at_qp", 3, [128, 2, 128], BF16)
            psS = Pool(k, "at_S", 2, [128, 8, 128], F32, "psum")
            psO = Pool(k, "at_O", 2, [128, 128], F32, "psum")
            psT = Pool(k, "at_T", 1, [128, 2, 128], BF16, "psum")
            Ep = Pool(k, "at_E", 3, [128, 8, 128], BF16)
            rcp = Pool(k, "at_rc", 4, [128, 1], F32)
            otp = Pool(k, "at_ot", 3, [128, W], BF16)
            dtp = Pool(k, "at_dt", 2, [128, 2, 128], BF16)
            items = [(qt, h) for qt in range(NT) for h in range(4)]
            qst = {}

            def stage_s(qt, h):
                qs = slice(qt * 128, (qt + 1) * 128)
                if h == 0:
                    qp_t = qpp.next()
                    k.dma("sp", qp_t[:], fmv(S.qpT)[:, :, qs], reads=[S.qpT], writes=[qp_t])
                    qr_t = None
                    lst = []
                    if si == 0:
                        qr_t = qrp.next()
                        k.dma("sp", qr_t[:], fmv(S.qrT)[:, :, qs], reads=[S.qrT], writes=[qr_t])
                        lst = qtiles[qt]
                    qst[qt] = (qp_t, qr_t, lst)
                qp_t, qr_t, lst = qst[qt]
                nw = len(lst); nt = nw + 2
                ch, pr = h // 2, slice((h % 2) * 64, (h % 2) * 64 + 64)
                Sp = psS.next(); E = Ep.next()
                for j_, (kt, u) in enumerate(lst):
                    mmul(Sp[:, j_, :], identb[:], bias[:, h * U + u, :], True, False, [identb, bias], [Sp])
                    mmul(Sp[:, j_, :], kr[pr, ch, kt * 128:(kt + 1) * 128], qr_t[pr, ch, :], False, True, [kr, qr_t], [Sp])
                for cj in range(2):
                    mmul(Sp[:, nw + cj, :], kc[pr, ch, cj * 128:(cj + 1) * 128], qp_t[pr, ch, :], True, True, [kc, qp_t], [Sp])
                k.op("act", lambda e, E=E, Sp=Sp, nt=nt: e.activation(out=E[:, 0:nt, :], in_=Sp[:, 0:nt, :], func=AF.Exp), reads=[Sp], writes=[E])
                return E

            cur_ot = {}

            def stage_pv(qt, h, E):
                qs = slice(qt * 128, (qt + 1) * 128)
                qp_t, qr_t, lst = qst[qt]
                nw = len(lst); nt = nw + 2
                if h == 0:
                    cur_ot[qt] = otp.next()
                ot = cur_ot[qt]
                O = psO.next(); rc = rcp.next()
                for j_ in range(nt):
                    rhs = vs[:, lst[j_][0], h, :] if j_ < nw else vc[:, j_ - nw, h, :]
                    mmul(O[:, 0:65], E[:, j_, :], rhs, j_ == 0, j_ == nt - 1, [E, vc] + ([vs] if si == 0 else []), [O])
                k.op("dve", lambda e, rc=rc, O=O: e.reciprocal(out=rc[:], in_=O[:, 64:65]), reads=[O], writes=[rc])
                k.op("dve", lambda e, ot=ot, O=O, rc=rc, h=h: e.tensor_scalar(out=ot[:, h * 64:(h + 1) * 64], in0=O[:, 0:64], scalar1=rc[:, 0:1], scalar2=None, op0=ALU.mult), reads=[O, rc], writes=[ot])
                if h == 3:
                    pt = psT.next(); dt = dtp.next()
                    for c in range(2):
                        k.op("pe", lambda e, c=c, pt=pt, ot=ot: e.transpose(out=pt[:, c, :], in_=ot[:, c * 128:(c + 1) * 128], identity=identb[:]), reads=[ot, identb], writes=[pt], signal=(c == 1))
                    k.op("act", lambda e, pt=pt, dt=dt: e.activation(out=dt[:], in_=pt[:], func=AF.Copy), reads=[pt], writes=[dt])
                    k.dma("sp", fmv(S.dT)[:, :, qs], dt[:], reads=[dt], writes=[S.dT], acc_w=True)

            Es = {0: stage_s(*items[0])}
            for i_, (qt, h) in enumerate(items):
                if i_ + 1 < len(items):
                    Es[i_ + 1] = stage_s(*items[i_ + 1])
                stage_pv(qt, h, Es.pop(i_))

    def merge_phase(S, l, Xsrc, Xdst):
        nn, si = S.n, S.si
        TT = 512 if nn >= 512 else nn
        fmv = lambda T_: T_.ap.ap().rearrange("(c p) t -> p c t", p=128)
        with k.phase():
            wbr = []
            for br in range(4):
                t = k.sbuf("mg_wbr", [128, 2, D], BF16)
                k.dma("pool", t[:], w_br[br].ap[l].rearrange("(c p) n -> p c n", p=128), reads=[w_br[br]], writes=[t])
                wbr.append(t)
            wo = k.sbuf("mg_wo", [128, 8, D], BF16)
            k.dma("pool", wo[:], w_out.ap[l].rearrange("(c p) n -> p c n", p=128), reads=[w_out], writes=[wo])
            identb, = load_ident(True, False)
            gtb = k.sbuf("mg_gt", [128, D], F32)
            bc_load(gtb, modD[l], modD[l].ap[si:si + 1, 2 * D:3 * D], D)
            brp = [Pool(k, "mg_br%d" % i, 2, [128, 2, TT], BF16) for i in range(4)]
            gp = Pool(k, "mg_g", 2, [128, 32, TT], BF16)
            ps = Pool(k, "mg_ps", 8, [128, 512], F32, "psum")
            tp = Pool(k, "mg_t", 8, [128, TT], BF16)
            mp = Pool(k, "mg_m", 2, [128, 8, TT], BF16)
            xp = Pool(k, "mg_x", 2, [128, D], F32)
            tmp = Pool(k, "mg_tmp", 2, [128, D], F32)
            xop = Pool(k, "mg_xo", 2, [128, D], F32)
            srcs = [S.aT, S.bT, S.cT, S.dT]
            def out_group(j, mT, g_, st_):
                s_, half = g_ // 2, g_ % 2
                rows = slice(j * TT + s_ * 128, j * TT + (s_ + 1) * 128)
                if half == 0:
                    st_["xt"] = xp.next(); st_["tm"] = tmp.next(); st_["xo"] = xop.next()
                    k.dma("sp", st_["xt"][:], Xsrc.ap[rows, :], reads=[Xsrc], writes=[st_["xt"]])
                xt, tm, xo = st_["xt"], st_["tm"], st_["xo"]
                hs = slice(half * 512, (half + 1) * 512)
                p = ps.next()
                for kc in range(8):
                    mmul(p[:], mT[:, kc, s_ * 128:(s_ + 1) * 128], wo[:, kc, hs], kc == 0, kc == 7, [mT, wo], [p])
                k.op("dve", lambda e, tm=tm, p=p, hs=hs: e.tensor_tensor(out=tm[:, hs], in0=p[:], in1=gtb[:, hs], op=ALU.mult), reads=[p, gtb], writes=[tm])
                if half == 1:
                    k.op("pool", lambda e, xo=xo, tm=tm, xt=xt: e.tensor_tensor(out=xo[:], in0=tm[:], in1=xt[:], op=ALU.add), reads=[tm, xt], writes=[xo])
                    k.dma("sp", Xdst.ap[rows, :], xo[:], reads=[xo], writes=[Xdst], acc_w=True)

            NG = (TT // 128) * 2
            prev = None
            def mg_loads(j):
                tk = slice(j * TT, (j + 1) * TT)
                bts = []
                for br in range(4):
                    t = brp[br].next()
                    k.dma("sp", t[:], fmv(srcs[br])[:, :, tk], reads=[srcs[br]], writes=[t])
                    bts.append(t)
                g = gp.next()
                for q4 in range(4):
                    k.dma("sp", g[:, q4 * 8:(q4 + 1) * 8, :], S.gT.ap.ap().rearrange("(c p) t -> p c t", p=128)[:, q4 * 8:(q4 + 1) * 8, tk], reads=[S.gT], writes=[g], acc_w=True)
                return bts, g
            nxt = mg_loads(0)
            for j in range(nn // TT):
                tk = slice(j * TT, (j + 1) * TT)
                bts, g = nxt
                if j + 1 < nn // TT:
                    nxt = mg_loads(j + 1)
                mT = mp.next()
                st_ = {}
                for oc in range(8):
                    ts = []
                    for br in range(4):
                        p = ps.next()
                        for kc in range(2):
                            mmul(p[:, :TT], wbr[br][:, kc, oc * 128:(oc + 1) * 128], bts[br][:, kc, :], kc == 0, kc == 1, [wbr[br], bts[br]], [p])
                        t = tp.next()
                        k.op("dve", lambda e, t=t, p=p, g=g, br=br, oc=oc: e.tensor_tensor(out=t[:], in0=p[:, :TT], in1=g[:, br * 8 + oc, :], op=ALU.mult), reads=[p, g], writes=[t])
                        ts.append(t)
                    if prev is not None and oc < NG:
                        out_group(prev[0], prev[1], oc, st_)
                    pm = ps.next()
                    for br in range(4):
                        mmul(pm[:, :TT], identb[:], ts[br][:], br == 0, br == 3, [identb, ts[br]], [pm])
                    k.op("act", lambda e, pm=pm, mT=mT, oc=oc: e.activation(out=mT[:, oc, :], in_=pm[:, :TT], func=AF.Copy), reads=[pm], writes=[mT])
                prev = (j, mT)
            st_ = {}
            for g_ in range(NG):
                out_group(prev[0], prev[1], g_, st_)

    def route_phase(S, l):
        nn, cap_ = S.n, S.cap
        Tseg = nn // 8
        NCH = nn // 128
        if not hasattr(S, "slot2D"):
            S.slot2D = scr(S.name + "_slot2D", [NE, nn], F32)
        seg = lambda T_: T_.ap.ap().rearrange("e (s t) -> (e s) t", s=8)
        with k.phase():
            blk = k.sbuf("rt_blk", [128, 128], F32); low = k.sbuf("rt_low", [128, 128], F32)
            k.dma("sp", blk[:], r_blk.ap.ap(), reads=[r_blk], writes=[blk])
            k.dma("sp", low[:], r_low.ap.ap(), reads=[r_low], writes=[low])
            A = k.sbuf("rt_A", [128, Tseg], F32)
            k.dma("sp", A[:], seg(S.affD), reads=[S.affD], writes=[A])
            junk = k.sbuf("rt_junk", [128, Tseg], F32)
            sv = k.sbuf("rt_sv", [128, 8], F32)
            k.op("dve", lambda e: e.memset(sv[:], 0.0), writes=[sv])
            k.op("dve", lambda e: e.memset(sv[:, 1:2], 1.0), reads=[sv], writes=[sv])
            pc = Pool(k, "rt_pc", 2, [128, 1], F32, "psum")
            for it in range(30):
                step = 2.0 ** -(it + 1)
                k.op("dve", lambda e, step=step: e.tensor_scalar(out=sv[:, 2:3], in0=sv[:, 0:1], scalar1=step, scalar2=None, op0=ALU.add), reads=[sv], writes=[sv])
                k.op("dve", lambda e: e.tensor_scalar(out=junk[:], in0=A[:], scalar1=sv[:, 2:3], scalar2=0.0, op0=ALU.is_ge, op1=ALU.add, accum_out=sv[:, 3:4]), reads=[A, sv], writes=[junk, sv])
                p = pc.next()
                mmul(p[:], blk[:], sv[:, 3:4], True, True, [blk, sv], [p])
                k.op("dve", lambda e, p=p, step=step: e.tensor_scalar(out=sv[:, 4:5], in0=p[:], scalar1=float(cap_), scalar2=step, op0=ALU.is_ge, op1=ALU.mult), reads=[p, sv], writes=[sv])
                k.op("dve", lambda e: e.tensor_tensor(out=sv[:, 0:1], in0=sv[:, 0:1], in1=sv[:, 4:5], op=ALU.add), reads=[sv], writes=[sv])
            mask = k.sbuf("rt_mask", [128, Tseg], F32)
            k.op("dve", lambda e: e.tensor_scalar(out=mask[:], in0=A[:], scalar1=sv[:, 0:1], scalar2=None, op0=ALU.is_ge), reads=[A, sv], writes=[mask])
            k.op("pool", lambda e: e.memset(junk[:], 0.0), writes=[junk])
            cs = k.sbuf("rt_cs", [128, Tseg], F32)
            k.op("dve", lambda e: e.tensor_tensor_scan(out=cs[:], data0=mask[:], data1=junk[:], initial=0.0, op0=ALU.add, op1=ALU.add), reads=[mask, junk], writes=[cs])
            k.op("dve", lambda e: e.tensor_copy(out=sv[:, 6:7], in_=cs[:, Tseg - 1:Tseg]), reads=[cs, sv], writes=[sv])
            p = pc.next()
            mmul(p[:], low[:], sv[:, 6:7], True, True, [low, sv], [p])
            k.op("dve", lambda e, p=p: e.tensor_copy(out=sv[:, 7:8], in_=p[:]), reads=[p, sv], writes=[sv])
            INV = 2016.0
            k.op("dve", lambda e: e.tensor_scalar(out=cs[:], in0=cs[:], scalar1=sv[:, 7:8], scalar2=-(INV + 1.0), op0=ALU.add, op1=ALU.add), reads=[cs, sv], writes=[cs])
            k.op("dve", lambda e: e.tensor_tensor(out=cs[:], in0=cs[:], in1=mask[:], op=ALU.mult), reads=[cs, mask], writes=[cs])
            k.op("dve", lambda e: e.tensor_scalar(out=cs[:], in0=cs[:], scalar1=INV, scalar2=None, op0=ALU.add), reads=[cs], writes=[cs])
            si_ = k.sbuf("rt_si", [128, Tseg], I32); s2 = k.sbuf("rt_s2", [128, Tseg], I32)
            k.op("dve", lambda e: e.tensor_copy(out=si_[:], in_=cs[:]), reads=[cs], writes=[si_])
            k.op("dve", lambda e: e.tensor_scalar(out=s2[:], in0=si_[:], scalar1=5, scalar2=None, op0=ALU.arith_shift_right), reads=[si_], writes=[s2])
            k.op("dve", lambda e: e.tensor_copy(out=mask[:], in_=s2[:]), reads=[s2], writes=[mask])
            k.dma("sp", seg(S.slotD), mask[:], reads=[mask], writes=[S.slotD])
            s3 = k.sbuf("rt_s3", [128, Tseg], I32)
            k.op("dve", lambda e: e.tensor_scalar(out=s3[:], in0=si_[:], scalar1=31, scalar2=None, op0=ALU.bitwise_and), reads=[si_], writes=[s3])
            k.op("dve", lambda e: e.tensor_copy(out=junk[:], in_=s3[:]), reads=[s3], writes=[junk])
            k.dma("sp", seg(S.slot2D), junk[:], reads=[junk], writes=[S.slot2D])
        with k.phase():
            identf, = load_ident(False, True)
            iota = k.sbuf("rt_iota", [128, NE, 32], F32)
            k.dma("sp", iota[:], r_iota.ap.ap(), reads=[r_iota], writes=[iota])
            tid = k.sbuf("rt_tid", [128, NCH], F32)
            k.dma("sp", tid[:], S.tid.ap.ap(), reads=[S.tid], writes=[tid])
            rows3 = []
            for nm, src in (("hi", S.slotD), ("lo", S.slot2D), ("af", S.affD)):
                t = k.sbuf("rt_" + nm, [NE, nn], F32)
                k.dma("sp", t[:], src.ap.ap(), reads=[src], writes=[t])
                rows3.append(t)
            pT = Pool(k, "rt_pT", 3, [128, 3, NE], F32, "psum")
            tmp_ = Pool(k, "rt_tm", 3, [128, 3, NE], F32)
            Ap = Pool(k, "rt_Ap", 3, [128, NE, 32], F32)
            Bp = Pool(k, "rt_Bp", 3, [128, NE, 32], F32)
            AGp = Pool(k, "rt_AG", 3, [128, NE, 2, 32], F32)
            accp = Pool(k, "rt_acc", 2, [64, NE, 32], F32, "psum")
            res = k.sbuf("rt_res", [64, NE, 32], F32)
            k.op("dve", lambda e: e.memset(res[:], 0.0), writes=[res])
            for c in range(NCH):
                cs_ = slice(c * 128, (c + 1) * 128)
                pt = pT.next(); tm = tmp_.next(); Aa = Ap.next(); Bb = Bp.next(); AG = AGp.next()
                for i_ in range(3):
                    k.op("pe", lambda e, i_=i_, pt=pt, cs_=cs_: e.transpose(out=pt[:, i_, :], in_=rows3[i_][:, cs_], identity=identf[0:NE, 0:NE]), reads=[rows3[i_], identf], writes=[pt], signal=(i_ == 2))
                k.op("act", lambda e, pt=pt, tm=tm: e.activation(out=tm[:], in_=pt[:], func=AF.Copy), reads=[pt], writes=[tm])
                b0, b1, b2 = [tm[:, i_, :].unsqueeze(2).to_broadcast([128, NE, 32]) for i_ in range(3)]
                k.op("dve", lambda e, Aa=Aa, b0=b0: e.tensor_tensor(out=Aa[:], in0=iota[:], in1=b0, op=ALU.is_equal), reads=[iota, tm], writes=[Aa])
                k.op("dve", lambda e, Bb=Bb, b1=b1: e.tensor_tensor(out=Bb[:], in0=iota[:], in1=b1, op=ALU.is_equal), reads=[iota, tm], writes=[Bb])
                k.op("dve", lambda e, Aa=Aa, AG=AG, b2=b2: e.tensor_tensor(out=AG[:, :, 1, :], in0=Aa[:], in1=b2, op=ALU.mult), reads=[Aa, tm], writes=[AG])
                k.op("act", lambda e, Aa=Aa, AG=AG, c=c: e.activation(out=AG[:, :, 0, :], in_=Aa[:], func=AF.Copy, scale=tid[:, c:c + 1]), reads=[Aa, tid], writes=[AG])
                acc = accp.next()
                for e_ in range(NE):
                    mmul(acc[:, e_, :], AG[:, e_, :, :], Bb[:, e_, :], True, True, [AG, Bb], [acc])
                k.op("dve", lambda e, acc=acc: e.tensor_tensor(out=res[:], in0=acc[:], in1=res[:], op=ALU.add), reads=[acc, res], writes=[res])
            resi = k.sbuf("rt_resi", [32, NE, 32], I32)
            k.op("dve", lambda e: e.tensor_copy(out=resi[:], in_=res[0:32]), reads=[res], writes=[resi])
            na = max(1, cap_ // 32)
            k.dma("sp", S.idxD.ap.ap().rearrange("e (a b) -> a e b", b=32), resi[0:na], reads=[resi], writes=[S.idxD])
            k.dma("sp", S.gateD.ap.ap().rearrange("e (a b) -> a e b", b=32), res[32:32 + na], reads=[res], writes=[S.gateD])

    def experts_phase(l, streams):
        with k.phase():
            identb, = load_ident(True, False)
            gtb = {}
            for S in streams:
                t = k.sbuf("ex_gt", [128, D], F32)
                bc_load(t, modD[l], modD[l].ap[S.si:S.si + 1, 5 * D:6 * D], D)
                gtb[S.si] = t
            wgp = Pool(k, "ex_wg", 2, [128, 8, D], BF16)
            wup = Pool(k, "ex_wu", 2, [128, 8, D], BF16)
            wdp = Pool(k, "ex_wd", 2, [128, 8, D], BF16)
            CAPM = max(S.cap for S in streams)
            xeTp = Pool(k, "ex_xeT", 2, [128, 8, CAPM], BF16)
            hT = k.sbuf("ex_hT", [128, 8, CAPM], BF16)
            xep = Pool(k, "ex_xe", 3, [128, D], BF16)
            yp = Pool(k, "ex_y", 3, [128, D], F32)
            sap = Pool(k, "ex_sa", 2, [128, 512], F32)
            ixp = Pool(k, "ex_ix", 3, [128, 8], I32)
            gap = Pool(k, "ex_ga", 3, [128, 8], F32)
            psT = Pool(k, "ex_psT", 1, [128, 8, 128], BF16, "psum")
            psA = Pool(k, "ex_psA", 2, [128, 512], F32, "psum")
            psU = Pool(k, "ex_psU", 2, [128, 512], F32, "psum")
            psY = Pool(k, "ex_psY", 3, [128, 512], F32, "psum")
            items = [(e_, S) for e_ in range(NE) for S in streams]
            wts = {}

            def load_w(e_):
                wg = wgp.next(); wu = wup.next(); wd = wdp.next()
                for wt, src in ((wg, w_gate), (wu, w_up), (wd, w_down)):
                    k.dma("pool", wt[:], src.ap[l, e_].rearrange("(c p) f -> p c f", p=128), reads=[src], writes=[wt])
                wts[e_] = (wg, wu, wd)

            def stage_g(e_, S):
                cap_ = S.cap
                P_ = min(128, cap_)
                ntl = max(1, cap_ // 128)
                ix = ixp.next(); ga = gap.next(); xeT = xeTp.next()
                k.dma("sp", ix[:P_, :ntl], S.idxD.ap[e_, :].rearrange("(p t) -> p t", t=ntl), reads=[S.idxD], writes=[ix])
                k.dma("sp", ga[:P_, :ntl], S.gateD.ap[e_, :].rearrange("(p t) -> p t", t=ntl), reads=[S.gateD], writes=[ga])
                k.op("dve", lambda e, ix=ix, P_=P_, ntl=ntl, hi_=S.n - 1: e.tensor_scalar(out=ix[:P_, :ntl], in0=ix[:P_, :ntl], scalar1=hi_, scalar2=0, op0=ALU.min, op1=ALU.max), reads=[ix], writes=[ix])
                for t_ in range(ntl):
                    xe = xep.next()
                    k.dma_raw("pool", lambda e, xe=xe, ix=ix, t_=t_, S=S, P_=P_: e.indirect_dma_start(out=xe[:P_, :], out_offset=None, in_=S.h2D.ap.ap(), in_offset=bass.IndirectOffsetOnAxis(ap=ix[:P_, t_:t_ + 1], axis=0)), reads=[S.h2D, ix], writes=[xe])
                    pt = psT.next()
                    for c in range(8):
                        k.op("pe", lambda e, c=c, pt=pt, xe=xe, P_=P_: e.transpose(out=pt[:, c, :P_], in_=xe[:P_, c * 128:(c + 1) * 128], identity=identb[:P_, :P_]), reads=[xe, identb], writes=[pt], signal=(c == 7))
                    k.op("act", lambda e, pt=pt, t_=t_, P_=P_, xeT=xeT: e.activation(out=xeT[:, :, t_ * 128:t_ * 128 + P_], in_=pt[:, :, :P_], func=AF.Copy), reads=[pt], writes=[xeT])
                return (ix, ga, xeT)

            def stage_u(e_, S, ctx_):
                ix, ga, xeT = ctx_
                wg, wu, wd = wts[e_]
                cap_ = S.cap
                NB = min(512, cap_)
                for fc in range(8):
                    fs = slice(fc * 128, (fc + 1) * 128)
                    for nb in range(cap_ // NB):
                        ns = slice(nb * NB, (nb + 1) * NB)
                        pa = psA.next(); pu = psU.next(); sa = sap.next()
                        for c in range(8):
                            mmul(pa[:, :NB], wg[:, c, fs], xeT[:, c, ns], c == 0, c == 7, [wg, xeT], [pa])
                        for c in range(8):
                            mmul(pu[:, :NB], wu[:, c, fs], xeT[:, c, ns], c == 0, c == 7, [wu, xeT], [pu])
                        k.op("act", lambda e, sa=sa, pa=pa, NB=NB: e.activation(out=sa[:, :NB], in_=pa[:, :NB], func=AF.Silu), reads=[pa], writes=[sa])
                        k.op("dve", lambda e, sa=sa, pu=pu, fc=fc, ns=ns, NB=NB: e.tensor_tensor(out=hT[:, fc, ns], in0=pu[:, :NB], in1=sa[:, :NB], op=ALU.mult), reads=[pu, sa], writes=[hT])

            def stage_d(e_, S, ctx_):
                ix, ga, xeT = ctx_
                wg, wu, wd = wts[e_]
                cap_ = S.cap
                P_ = min(128, cap_)
                ntl = max(1, cap_ // 128)
                Xn = S.Xnext
                for t_ in range(ntl):
                    y = yp.next()
                    for half in range(2):
                        hs = slice(half * 512, (half + 1) * 512)
                        py = psY.next()
                        for fc in range(8):
                            mmul(py[:P_, :], hT[:, fc, t_ * 128:t_ * 128 + P_], wd[:, fc, hs], fc == 0, fc == 7, [hT, wd], [py])
                        k.op("dve", lambda e, y=y, py=py, ga=ga, t_=t_, hs=hs, S=S, P_=P_: e.scalar_tensor_tensor(out=y[:P_, hs], in0=py[:P_, :], scalar=ga[:P_, t_:t_ + 1], in1=gtb[S.si][:P_, hs], op0=ALU.mult, op1=ALU.mult), reads=[py, ga, gtb[S.si]], writes=[y])
                    k.dma_raw("pool", lambda e, y=y, ix=ix, t_=t_, Xn=Xn, P_=P_: e.indirect_dma_start(out=Xn.ap.ap(), out_offset=bass.IndirectOffsetOnAxis(ap=ix[:P_, t_:t_ + 1], axis=0), in_=y[:P_, :], in_offset=None, compute_op=ALU.add), reads=[y, ix], writes=[Xn], acc_w=(t_ > 0))
                    if t_ == 0:
                        Xn.wb = []

            load_w(0)
            ctxs = {0: stage_g(*items[0])}
            for i_, (e_, S) in enumerate(items):
                if S is streams[0] and e_ + 1 < NE:
                    load_w(e_ + 1)
                stage_u(e_, S, ctxs[i_])
                if i_ + 1 < len(items):
                    ctxs[i_ + 1] = stage_g(*items[i_ + 1])
                stage_d(e_, S, ctxs[i_])

    PH = set(dbg_phases) if dbg_phases is not None else None
    def on(name):
        return PH is None or name in PH
    SX.Xcur, SC.Xcur = x_in, ctx_in
    for l in range(L):
        last = l == L - 1
        for S_ in (SX, SC):
            S_.Xin = S_.Xcur
            S_.Xnext = S_.XA if S_.Xcur is not S_.XA else S_.XB
        if on("mod"):
            mod_phase(l)
        if on("norm1"):
            norm_phase(SX, l, 1); norm_phase(SC, l, 1)
        if on("inproj"):
            inproj_phase([(SX, False), (SC, last)], l)
        if on("gmlp"):
            gmlp_phase(SX, l)
            if not last:
                gmlp_phase(SC, l)
        if on("conv"):
            conv_phase(SX, l)
            if not last:
                conv_phase(SC, l)
        if on("fourier"):
            fourier_phase(SX, l)
            if not last:
                fourier_phase(SC, l)
        if on("attn"):
            attention_phase(SX, SC, l)
            if not last:
                attention_phase(SC, SC, l)
        if on("merge"):
            merge_phase(SX, l, SX.Xcur, SX.Xnext)
            if not last:
                merge_phase(SC, l, SC.Xcur, SC.Xnext)
        if on("ffn"):
            strs = [SX] if last else [SX, SC]
            for S_ in strs:
                S_.Xin = S_.Xnext
                norm_phase(S_, l, 2)
                route_phase(S_, l)
            if on("experts"):
                experts_phase(l, strs)
        for S_ in (SX, SC):
            S_.Xcur = S_.Xnext
        if PH is not None:
            break
    SX.Xin = SX.Xcur if PH is None else x_in
    norm_phase(SX, 0, 3)
    k.finish([OUT])
    k.emit()
    st.close()
    return nc, list(ins.keys())


def _prep_inputs(inp, n, L=2):
    f = lambda a: np.ascontiguousarray(np.asarray(a, dtype=np.float32))
    ulist, qtiles = _attn_geometry(n)
    cs, f1x, f2, twx = _dft_consts(n)
    _, f1c, _, twc = _dft_consts(NCTX)
    blk, low, iota = _route_consts()
    w_in = f(inp["w_in"])
    i = np.arange(256)
    perm = np.where((i % 32) < 16, i + 16, i - 16)
    w_qkp = np.concatenate([w_in[:, :, C_END:Q_END][:, :, perm], w_in[:, :, Q_END:K_END][:, :, perm]], axis=2)
    pp = np.arange(128)
    common = {
        "w_mod": f(inp["w_mod"]), "b_mod": f(inp["b_mod"]), "norm1_g": f(inp["norm1_g"]), "norm2_g": f(inp["norm2_g"]),
        "final_norm_g": f(inp["final_norm_g"]).reshape(1, D), "w_in": w_in, "w_qkp": np.ascontiguousarray(w_qkp),
        "sgu_ln_g": f(inp["sgu_ln_g"]), "sgu_ln_b": f(inp["sgu_ln_b"]),
        "wsT": np.ascontiguousarray(f(inp["w_spatial"]).transpose(0, 3, 1, 2)),
        "b_spatial": f(inp["b_spatial"]),
        "w_a_out": f(inp["w_a_out"]), "w_b_out": f(inp["w_b_out"]), "w_c_out": f(inp["w_c_out"]), "w_d_out": f(inp["w_d_out"]),
        "conv_wT": np.ascontiguousarray(f(inp["conv_w"]).transpose(0, 2, 1)), "conv_b_c": f(inp["conv_b"])[:, :, None].copy(),
        "conv_ln_g_c": f(inp["conv_ln_g"])[:, :, None].copy(), "conv_ln_b_c": f(inp["conv_ln_b"])[:, :, None].copy(),
        "w_out": f(inp["w_out"]), "w_router": f(inp["w_router"]),
        "w_gate_e": f(inp["w_gate_e"]), "w_up_e": f(inp["w_up_e"]), "w_down_e": f(inp["w_down_e"]),
        "id_bf": np.eye(128).astype(bf16_np), "id_f": np.eye(128, dtype=np.float32),
        "rope": _rope_tables(n),
        "biasT": np.stack([_bias_tiles(f(inp["rpb"])[l], ulist) for l in range(L)]),
        "dft_cs": cs, "dft_f1x": f1x, "dft_f1c": f1c, "dft_f2": f2, "dft_twx": twx, "dft_twc": twc,
        "r_blk": blk, "r_low": low, "r_iota": iota,
        "tid_x": (np.arange(n // 128)[None, :] * 128 + pp[:, None]).astype(np.float32),
        "tid_c": (np.arange(2)[None, :] * 128 + pp[:, None]).astype(np.float32),
        "cctx_pk": np.ascontiguousarray(f(inp["c_ctx"]).reshape(8, 128).T),
    }
    maps = []
    B = inp["x"].shape[0]
    for b in range(B):
        m = dict(common)
        m["x"] = f(inp["x"][b]); m["ctx"] = f(inp["ctx"][b])
        m["c_pk"] = np.ascontiguousarray(f(inp["c"][b]).reshape(8, 128).T)
        maps.append(m)
    return maps


_CACHE = {}


def kernel(**inputs):
    n = inputs["x"].shape[1]
    B = inputs["x"].shape[0]
    if n not in _CACHE:
        _CACHE[n] = build_program(n)
    nc, names = _CACHE[n]
    maps = _prep_inputs(inputs, n)
    maps = [{kk: m[kk] for kk in names} for m in maps]
    res = run_bass_kernel_spmd(nc, maps, core_ids=list(range(B)))
    return np.stack([np.asarray(r["out"], dtype=np.float32) for r in res.results], axis=0)
```

```python
import numpy as np
import ml_dtypes
from contextlib import ExitStack
import concourse.bass as bass
import concourse.mybir as mybir
from concourse.bass_utils import run_bass_kernel_spmd

F32 = mybir.dt.float32
BF16 = mybir.dt.bfloat16
I32 = mybir.dt.int32
U32 = mybir.dt.uint32
AF = mybir.ActivationFunctionType
ALU = mybir.AluOpType
AX = mybir.AxisListType

N_DMA_SEMS = 40
N_SW_SEMS = 8


class T:
    def __init__(self, ap, name=""):
        self.ap = ap
        self.name = name
        self.w = []
        self.wb = []
        self.r = []

    def __getitem__(self, idx):
        return self.ap[idx]


class K:
    ENGS = ["pe", "act", "dve", "pool", "sp"]

    def __init__(self, nc, stack):
        self.nc = nc
        self.stack = stack
        self.streams = {e: [] for e in self.ENGS}
        self.sems = {}
        for e in self.ENGS:
            self.sems[e] = stack.enter_context(nc.semaphore("c_" + e))
        self.count = {e: 0 for e in self.ENGS}
        self.dma_sems = [stack.enter_context(nc.semaphore("d%d" % i)) for i in range(N_DMA_SEMS)]
        self.dma_val = [0] * N_DMA_SEMS
        self.dma_rr = 0
        self.dma_rr_sw = 0
        self.known = {e: {} for e in self.ENGS}
        self.same_engine_sync = True
        self.n_inst = 0

    def sbuf(self, name, shape, dtype):
        self.n_alloc = getattr(self, "n_alloc", 0) + 1
        name = "%s_%d" % (name, self.n_alloc)
        t = self.stack.enter_context(self.nc.sbuf_tensor(name, list(shape), dtype))
        return T(t, name)

    def psum(self, name, shape, dtype):
        self.n_alloc = getattr(self, "n_alloc", 0) + 1
        name = "%s_%d" % (name, self.n_alloc)
        t = self.stack.enter_context(self.nc.psum_tensor(name, list(shape), dtype))
        return T(t, name)

    def dram(self, name, shape, dtype, kind="Internal"):
        t = self.nc.dram_tensor(name, list(shape), dtype, kind=kind)
        return T(t, name)

    def _sem_of(self, key):
        return self.sems[key] if isinstance(key, str) else self.dma_sems[key]

    def _wait(self, eng, dep):
        key, val = dep
        if key == eng and (not self.same_engine_sync or eng in ("pe", "sp")):
            return
        kn = self.known[eng]
        if kn.get(key, 0) >= val:
            return
        kn[key] = val
        self.streams[eng].append(("wait", key, val))

    def _deps(self, eng, reads, writes, acc_w=False):
        for t in reads:
            for d in t.w:
                self._wait(eng, d)
        for t in writes:
            for d in (t.wb if acc_w else t.w):
                self._wait(eng, d)
            for d in t.r:
                self._wait(eng, d)

    def _mark(self, dep, reads, writes, acc_w=False):
        for t in reads:
            t.r.append(dep)
            if len(t.r) > 64:
                t.r = self._compress(t.r)
        for t in writes:
            if acc_w:
                t.w.append(dep)
                if len(t.w) > 64:
                    t.w = self._compress(t.w)
            else:
                t.w = [dep]
                t.wb = [dep]
            t.r = []

    @staticmethod
    def _compress(deps):
        m = {}
        for k, v in deps:
            if m.get(k, 0) < v:
                m[k] = v
        return list(m.items())

    def op(self, eng, fn, reads=(), writes=(), signal=True):
        self._deps(eng, reads, writes)
        if signal:
            self.count[eng] += 1
            dep = (eng, self.count[eng])
            self.streams[eng].append(("inst", fn, eng, 1))
        else:
            dep = (eng, self.count[eng] + 1)
            self.streams[eng].append(("inst", fn, None, 0))
        self._mark(dep, reads, writes)
        self.n_inst += 1

    def dma(self, q, out, in_, reads=(), writes=(), acc_w=False, **kw):
        self._deps(q, reads, writes, acc_w=acc_w)
        self._throttle(q)
        s = self._next_sem(q)
        if self.dma_val[s] > 0:
            self._wait(q, (s, self.dma_val[s]))
        self.dma_val[s] += 16
        dep = (s, self.dma_val[s])

        def fn(e, out=out, in_=in_, kw=kw):
            return e.dma_start(out=out, in_=in_, **kw)
        self.streams[q].append(("inst", fn, s, 16))
        self._mark(dep, reads, writes, acc_w=acc_w)
        self.n_inst += 1
        if q == "pool":
            self.__dict__.setdefault("pool_out", []).append(dep)
        return dep

    def _next_sem(self, q):
        if q == "pool":
            s = self.dma_rr_sw
            self.dma_rr_sw = (self.dma_rr_sw + 1) % N_SW_SEMS
            return s
        s = N_SW_SEMS + self.dma_rr
        self.dma_rr = (self.dma_rr + 1) % (N_DMA_SEMS - N_SW_SEMS)
        return s

    def _throttle(self, q):
        if q != "pool":
            return
        po = self.__dict__.setdefault("pool_out", [])
        if len(po) >= 2:
            self._wait(q, po[-2])

    def dma_raw(self, q, fn, reads=(), writes=(), acc_w=False):
        self._deps(q, reads, writes, acc_w=acc_w)
        self._throttle(q)
        s = self._next_sem(q)
        if self.dma_val[s] > 0:
            self._wait(q, (s, self.dma_val[s]))
        self.dma_val[s] += 16
        dep = (s, self.dma_val[s])
        self.streams[q].append(("inst", fn, s, 16))
        self._mark(dep, reads, writes, acc_w=acc_w)
        if q == "pool":
            self.__dict__.setdefault("pool_out", []).append(dep)
        return dep

    def finish(self, out_tiles):
        for t in out_tiles:
            for d in t.w:
                self._wait("sp", d)
        for e in self.ENGS:
            if e != "sp" and self.count[e] > 0:
                self._wait("sp", (e, self.count[e]))
        for s in range(N_DMA_SEMS):
            if self.dma_val[s] > 0:
                self._wait("sp", (s, self.dma_val[s]))

    def emit(self):
        nc = self.nc
        engmap = {"pe": "tensor", "act": "scalar", "dve": "vector", "pool": "gpsimd", "sp": "sync"}
        with nc.Block() as block:
            for e in self.ENGS:
                stream = self.streams[e]
                if not stream:
                    continue

                def body(eng, stream=stream):
                    for item in stream:
                        if item[0] == "wait":
                            eng.wait_ge(self._sem_of(item[1]), item[2])
                        else:
                            _, fn, key, inc = item
                            ins = fn(eng)
                            if key is not None:
                                ins.then_inc(self._sem_of(key), inc)
                getattr(block, engmap[e])(body)


class Pool:
    def __init__(self, k, name, n, shape, dtype, space="sbuf"):
        mk = k.sbuf if space == "sbuf" else k.psum
        self.tiles = [mk("%s%d" % (name, i), shape, dtype) for i in range(n)]
        self.i = 0

    def next(self):
        t = self.tiles[self.i]
        self.i = (self.i + 1) % len(self.tiles)
        return t


def _k_phase(self):
    class _Ph:
        def __init__(s, k):
            s.k = k
        def __enter__(s):
            s.prev = s.k.stack
            s.st = ExitStack()
            s.st.__enter__()
            s.k.stack = s.st
            return s
        def __exit__(s, *a):
            s.k.barrier()
            s.k.stack = s.prev
            return s.st.__exit__(*a)
    return _Ph(self)


def _k_barrier(self):
    for e in self.ENGS:
        for e2 in self.ENGS:
            if e2 != e and self.count[e2] > 0:
                self._wait(e, (e2, self.count[e2]))
        for s in range(N_DMA_SEMS):
            if self.dma_val[s] > 0:
                self._wait(e, (s, self.dma_val[s]))


K.phase = _k_phase
K.barrier = _k_barrier


D = 1024
W = 256
NCTX = 256
NE = 16
GW = 64
bf16_np = ml_dtypes.bfloat16
NEG = -30000.0


def _rope_tables(n):
    t = np.arange(n)
    rows, cols = t // GW, t % GW
    inv = 10000.0 ** (-np.arange(16, dtype=np.float64) / 16)
    d = np.arange(128) % 64
    pos = np.where(d[:, None] < 32, rows[None, :], cols[None, :]).astype(np.float64)
    ang = pos * inv[d % 16][:, None]
    cos, sin = np.cos(ang), np.sin(ang)
    sgn = np.where((d % 32) < 16, -1.0, 1.0)[:, None]
    return np.stack([cos / 8, sin * sgn / 8, cos, sin * sgn]).astype(np.float32)


def _attn_geometry(n):
    R = n // GW
    wr = min(8, R)
    s = lambda r: int(np.clip(r - wr // 2, 0, R - wr))
    cst = np.clip(np.arange(GW) - 8, 0, GW - 16)
    uniq = {}
    ulist = []
    qtiles = []
    kc = np.tile(np.arange(GW), 2)
    ka = np.repeat(np.arange(2), GW)
    for qt in range(R // 2):
        r0 = 2 * qt
        lo, hi = s(r0), s(r0 + 1) + wr - 1
        lst = []
        for kt in range(lo // 2, hi // 2 + 1):
            kr = 2 * kt + ka
            qr = r0 + ka
            qc = kc
            srow = np.array([s(r) for r in qr])
            ok_r = (kr[:, None] >= srow[None, :]) & (kr[:, None] < srow[None, :] + wr)
            ok_c = (kc[:, None] >= cst[qc][None, :]) & (kc[:, None] < cst[qc][None, :] + 16)
            mask = ok_r & ok_c
            if not mask.any():
                continue
            dr = np.clip(kr[:, None] - qr[None, :] + 7, 0, 14)
            dc = np.clip(kc[:, None] - qc[None, :], -15, 15) + 15
            key = (2 * kt - r0, mask.tobytes())
            if key not in uniq:
                uniq[key] = len(ulist)
                ulist.append((dr, dc, mask))
            lst.append((kt, uniq[key]))
        qtiles.append(lst)
    return ulist, qtiles


def _bias_tiles(rpb_l, ulist):
    out = np.empty((4, len(ulist), 128, 128), np.float32)
    for u, (dr, dc, mask) in enumerate(ulist):
        for h in range(4):
            out[h, u] = np.where(mask, rpb_l[h][dr, dc], NEG)
    return np.ascontiguousarray(out.transpose(2, 0, 1, 3).reshape(128, 4 * len(ulist), 128)).astype(bf16_np)


def _dft_consts(n):
    N1 = n // 128
    sc = 1.0 / 8.0
    dd = np.arange(64)
    ph = 2 * np.pi * np.outer(dd, dd) / 64
    Cd, Sd = np.cos(ph) * sc, np.sin(ph) * sc
    cs = np.zeros((128, 256))
    for g in range(2):
        cs[g * 64:(g + 1) * 64, g * 64:(g + 1) * 64] = Cd
        cs[g * 64:(g + 1) * 64, 128 + g * 64:128 + (g + 1) * 64] = -Sd
    c1 = np.arange(N1)
    p1 = 2 * np.pi * np.outer(c1, c1) / N1
    f1 = np.stack([np.cos(p1), np.sin(p1), -np.sin(p1)]) / np.sqrt(float(n))
    pp = np.arange(128)
    p2 = 2 * np.pi * np.outer(pp, pp) / 128
    f2 = np.stack([np.cos(p2), np.sin(p2)])
    pt = 2 * np.pi * np.outer(pp, c1) / n
    tw = np.stack([np.cos(pt), -np.sin(pt)]).astype(np.float32)
    return cs.astype(bf16_np), f1.astype(bf16_np), f2.astype(bf16_np), tw


def _route_consts():
    p = np.arange(128)
    same = (p[:, None] // 8) == (p[None, :] // 8)
    blk = same.astype(np.float32)
    low = (same & (p[:, None] < p[None, :])).astype(np.float32)
    iota = np.broadcast_to(np.arange(32, dtype=np.float32), (128, NE, 32)).copy()
    return blk, low, iota


A_END, B_END, C_END, Q_END, K_END, V_END = 512, 768, 1280, 1536, 1792, 2048
IN_COLS = 6144
GELU_C = 1.5957691216057308


class Stream:
    pass


def build_program(n, L=2, dbg=(), dbg_phases=None):
    nc = bass.Bass("TRN2", target_bir_lowering=False)
    N1 = n // 128
    cap = 2 * n // NE
    capc = 2 * NCTX // NE
    st = ExitStack()
    k = K(nc, st)
    ins = {}

    def ext(name, shape, dtype=F32):
        h = nc.dram_tensor(name, list(shape), dtype, kind="ExternalInput")
        ins[name] = T(h, name)
        return ins[name]

    def scr(name, shape, dtype):
        kind = "ExternalOutput" if name in dbg else "Internal"
        h = nc.dram_tensor(name, list(shape), dtype, kind=kind)
        return T(h, name)

    x_in = ext("x", [n, D]); ctx_in = ext("ctx", [NCTX, D])
    c_in = ext("c_pk", [128, 8]); cc_in = ext("cctx_pk", [128, 8])
    w_mod = ext("w_mod", [L, D, 6 * D]); b_mod = ext("b_mod", [L, 6 * D])
    n1g = ext("norm1_g", [L, D]); n2g = ext("norm2_g", [L, D]); fng = ext("final_norm_g", [1, D])
    w_in = ext("w_in", [L, D, IN_COLS]); w_qkp = ext("w_qkp", [L, D, 512])
    sgu_g = ext("sgu_ln_g", [L, W]); sgu_b = ext("sgu_ln_b", [L, W])
    wsT = ext("wsT", [L, 128, 4, 128]); bsp = ext("b_spatial", [L, 4, 128])
    w_br = [ext(nm, [L, W, D]) for nm in ("w_a_out", "w_b_out", "w_c_out", "w_d_out")]
    conv_wT = ext("conv_wT", [L, W, 31]); conv_b = ext("conv_b_c", [L, W, 1])
    cln_g = ext("conv_ln_g_c", [L, W, 1]); cln_b = ext("conv_ln_b_c", [L, W, 1])
    w_out = ext("w_out", [L, D, D]); w_router = ext("w_router", [L, D, NE])
    w_gate = ext("w_gate_e", [L, NE, D, D]); w_up = ext("w_up_e", [L, NE, D, D]); w_down = ext("w_down_e", [L, NE, D, D])
    id_bf = ext("id_bf", [128, 128], BF16); id_f = ext("id_f", [128, 128])
    rope = ext("rope", [4, 128, n])
    ulist, qtiles = _attn_geometry(n)
    U = len(ulist)
    biasT = ext("biasT", [L, 128, 4 * U, 128], BF16)
    cs_c = ext("dft_cs", [128, 256], BF16)
    f1x = ext("dft_f1x", [3, N1, N1], BF16); f1c = ext("dft_f1c", [3, 2, 2], BF16)
    f2 = ext("dft_f2", [2, 128, 128], BF16)
    twx = ext("dft_twx", [2, 128, N1]); twc = ext("dft_twc", [2, 128, 2])
    r_blk = ext("r_blk", [128, 128]); r_low = ext("r_low", [128, 128]); r_iota = ext("r_iota", [128, NE, 32])
    tidx = ext("tid_x", [128, N1]); tidc = ext("tid_c", [128, 2])
    out_h = nc.dram_tensor("out", [n, D], F32, kind="ExternalOutput")
    OUT = T(out_h, "out")

    def mk_stream(si, nn, name, xin):
        S = Stream()
        S.si, S.n, S.name, S.N1 = si, nn, name, nn // 128
        S.cap = 2 * nn // NE
        S.Xin = xin
        S.XA = scr(name + "_XA", [nn, D], F32); S.XB = scr(name + "_XB", [nn, D], F32)
        S.hT = scr(name + "_hT", [D, nn], BF16)
        for nm in ("uT", "yT", "qrT", "qpT", "krT", "aT", "bT", "cT", "dT"):
            setattr(S, nm, scr(name + "_" + nm, [W, nn], BF16))
        for nm in ("vln", "Zr", "Zi", "v"):
            setattr(S, nm, scr(name + "_" + nm, [nn, W], BF16))
        S.gT = scr(name + "_gT", [4 * D, nn], BF16)
        S.Y1r = scr(name + "_Y1r", [S.N1, 128 * W], BF16); S.Y1i = scr(name + "_Y1i", [S.N1, 128 * W], BF16)
        S.h2D = scr(name + "_h2D", [nn, D], BF16)
        S.affD = scr(name + "_affD", [NE, nn], F32)
        S.slotD = scr(name + "_slotD", [NE, nn], F32)
        S.idxD = scr(name + "_idxD", [NE, S.cap], I32)
        S.gateD = scr(name + "_gateD", [NE, S.cap], F32)
        S.f1 = f1x if si == 0 else f1c
        S.tw = twx if si == 0 else twc
        S.tid = tidx if si == 0 else tidc
        return S

    SX = mk_stream(0, n, "sx", x_in)
    SC = mk_stream(1, NCTX, "sc", ctx_in)
    modD = [scr("modD%d" % l, [2, 6 * D], F32) for l in range(L)]

    def mmul(out, lhsT, rhs, start, stop, reads, writes):
        k.op("pe", lambda e: e.matmul(out, lhsT=lhsT, rhs=rhs, start=start, stop=stop), reads=reads, writes=writes, signal=bool(stop))

    def load_ident(bf=True, f=False):
        r = []
        if bf:
            t = k.sbuf("identb", [128, 128], BF16); k.dma("sp", t[:], id_bf.ap.ap(), reads=[id_bf], writes=[t]); r.append(t)
        if f:
            t = k.sbuf("identf", [128, 128], F32); k.dma("sp", t[:], id_f.ap.ap(), reads=[id_f], writes=[t]); r.append(t)
        return r

    def mod_phase(l):
        with k.phase():
            cs = k.sbuf("mod_cs", [128, 2, 8], F32)
            k.dma("sp", cs[:, 0, :], c_in.ap.ap(), reads=[c_in], writes=[cs])
            k.dma("sp", cs[:, 1, :], cc_in.ap.ap(), reads=[cc_in], writes=[cs])
            scs = k.sbuf("mod_scs", [128, 2, 8], F32)
            k.op("act", lambda e: e.activation(out=scs[:], in_=cs[:], func=AF.Silu), reads=[cs], writes=[scs])
            wp = Pool(k, "mod_w", 4, [128, 8, 512], F32)
            pp = Pool(k, "mod_ps", 2, [2, 512], F32, "psum")
            bp = Pool(k, "mod_b", 4, [2, 512], F32)
            rp = Pool(k, "mod_r", 4, [2, 512], F32)
            ldq = {}
            def mod_loads(blk):
                wm = wp.next(); bm = bp.next()
                cols = slice(blk * 512, (blk + 1) * 512)
                k.dma("sp", wm[:], w_mod.ap[l, :, cols].rearrange("(c p) n -> p c n", p=128), reads=[w_mod], writes=[wm])
                k.dma("sp", bm[:], b_mod.ap[l:l + 1, cols].to_broadcast([2, 512]), reads=[b_mod], writes=[bm])
                ldq[blk] = (wm, bm)
            for b0 in range(3):
                mod_loads(b0)
            for blk in range(12):
                if blk + 3 < 12:
                    mod_loads(blk + 3)
                wm, bm = ldq.pop(blk)
                ps = pp.next(); rs = rp.next()
                cols = slice(blk * 512, (blk + 1) * 512)
                for c in range(8):
                    mmul(ps[:], scs[:, :, c], wm[:, c, :], c == 0, c == 7, [scs, wm], [ps])
                k.op("dve", lambda e, rs=rs, ps=ps, bm=bm: e.tensor_tensor(out=rs[:], in0=ps[:], in1=bm[:], op=ALU.add), reads=[ps, bm], writes=[rs])
                k.dma("sp", modD[l].ap[:, cols], rs[:], reads=[rs], writes=[modD[l]], acc_w=True)

    def bc_load(dst, src_t, row_ap, F):
        k.dma("sp", dst[:], row_ap.to_broadcast([128, F]), reads=[src_t], writes=[dst])

    def norm_phase(S, l, kind):
        nn, si = S.n, S.si
        with k.phase():
            scale_b = k.sbuf("nm_scale", [128, D], F32)
            if kind == 3:
                bc_load(scale_b, fng, fng.ap[0:1, :], D)
            else:
                g = n1g if kind == 1 else n2g
                o = 0 if kind == 1 else 3
                gb = k.sbuf("nm_g", [128, D], F32); scb = k.sbuf("nm_sc", [128, D], F32)
                shift_b = k.sbuf("nm_shift", [128, D], F32)
                bc_load(gb, g, g.ap[l:l + 1, :], D)
                bc_load(scb, modD[l], modD[l].ap[si:si + 1, (o + 1) * D:(o + 2) * D], D)
                bc_load(shift_b, modD[l], modD[l].ap[si:si + 1, o * D:(o + 1) * D], D)
                k.op("dve", lambda e: e.scalar_tensor_tensor(out=scale_b[:], in0=scb[:], scalar=1.0, in1=gb[:], op0=ALU.add, op1=ALU.mult), reads=[scb, gb], writes=[scale_b])
            if kind == 1:
                identb, = load_ident(True, False)
            if kind == 2:
                identf, = load_ident(False, True)
                rt = k.sbuf("nm_router", [128, 8, NE], F32)
                k.dma("sp", rt[:], w_router.ap[l].rearrange("(c p) e -> p c e", p=128), reads=[w_router], writes=[rt])
                ones16 = k.sbuf("nm_ones16", [NE, NE], F32)
                k.op("dve", lambda e: e.memset(ones16[:], 1.0), writes=[ones16])
                aff_all = k.sbuf("nm_aff", [NE, nn], F32)
            xp = Pool(k, "nm_x", 6, [128, D], F32)
            jp = Pool(k, "nm_junk", 2, [128, D], BF16)
            sp_ = Pool(k, "nm_st", 3, [128, 4], F32)
            tp = Pool(k, "nm_tmp", 3, [128, D], F32)
            hp = Pool(k, "nm_h", 3, [128, D], BF16 if kind == 1 else F32)
            if kind == 1:
                psT = Pool(k, "nm_psT", 2, [128, 8, 128], BF16, "psum")
                hTp = Pool(k, "nm_hT", 2, [128, 8, 128], BF16)
            if kind == 2:
                hbp = Pool(k, "nm_hb", 2, [128, D], BF16)
                psT = Pool(k, "nm_psT", 2, [128, 4, 128], F32, "psum")
                hTp = Pool(k, "nm_hT", 2, [128, 8, 128], F32)
                psl = Pool(k, "nm_psl", 2, [NE, 512], F32, "psum")
                rstate = {}
                e_p = Pool(k, "nm_e", 2, [NE, 512], F32)
                r_p = Pool(k, "nm_r", 2, [NE, 512], F32)
            xq = {}
            def stage0(i):
                    xt = xp.next()
                    k.dma("sp", xt[:], S.Xin.ap[i * 128:(i + 1) * 128, :], reads=[S.Xin], writes=[xt])
                    xq[i] = xt
            def stage1(i):
                    rows = slice(i * 128, (i + 1) * 128)
                    xt = xq.pop(i); jk = jp.next(); s4 = sp_.next(); tmp = tp.next(); h = hp.next()
                    k.op("act", lambda e, xt=xt, jk=jk, s4=s4: e.activation(out=jk[:], in_=xt[:], func=AF.Square, accum_out=s4[:, 0:1]), reads=[xt], writes=[jk, s4])
                    xs = tmp
                    if kind != 3:
                        k.op("pool", lambda e, xt=xt, xs=xs: e.tensor_tensor(out=xs[:], in0=xt[:], in1=scale_b[:], op=ALU.mult), reads=[xt, scale_b], writes=[xs])
                    k.op("act", lambda e, s4=s4: e.activation(out=s4[:, 1:2], in_=s4[:, 0:1], func=AF.Sqrt, bias=1e-6, scale=1.0 / D), reads=[s4], writes=[s4])
                    k.op("dve", lambda e, s4=s4: e.reciprocal(out=s4[:, 2:3], in_=s4[:, 1:2]), reads=[s4], writes=[s4])
                    if kind == 3:
                        k.op("dve", lambda e, xt=xt, s4=s4, h=h: e.scalar_tensor_tensor(out=h[:], in0=xt[:], scalar=s4[:, 2:3], in1=scale_b[:], op0=ALU.mult, op1=ALU.mult), reads=[xt, s4, scale_b], writes=[h])
                        k.dma("sp", OUT.ap[rows, :], h[:], reads=[h], writes=[OUT], acc_w=True)
                        return None
                    k.op("dve", lambda e, tmp=tmp, xs=xs, s4=s4, h=h: e.scalar_tensor_tensor(out=h[:], in0=xs[:], scalar=s4[:, 2:3], in1=shift_b[:], op0=ALU.mult, op1=ALU.add), reads=[xs, s4, shift_b], writes=[h])
                    return (rows, h)
            def stage2(ctx_):
                    rows, h = ctx_
                    if kind == 1:
                        pt = psT.next(); hT = hTp.next()
                        for c in range(8):
                            k.op("pe", lambda e, c=c, pt=pt, h=h: e.transpose(out=pt[:, c, :], in_=h[:, c * 128:(c + 1) * 128], identity=identb[:]), reads=[h, identb], writes=[pt], signal=(c == 7))
                        k.op("act", lambda e, pt=pt, hT=hT: e.activation(out=hT[:], in_=pt[:], func=AF.Copy), reads=[pt], writes=[hT])
                        k.dma("sp", S.hT.ap.ap().rearrange("(c p) t -> p c t", p=128)[:, :, rows], hT[:], reads=[hT], writes=[S.hT], acc_w=True)
                    else:
                        hb = hbp.next(); hT = hTp.next()
                        k.op("act", lambda e, hb=hb, h=h: e.activation(out=hb[:], in_=h[:], func=AF.Copy), reads=[h], writes=[hb])
                        k.dma("sp", S.h2D.ap[rows, :], hb[:], reads=[hb], writes=[S.h2D], acc_w=True)
                        for half in range(2):
                            pt = psT.next()
                            for c in range(4):
                                cc = half * 4 + c
                                k.op("pe", lambda e, c=c, cc=cc, pt=pt, h=h: e.transpose(out=pt[:, c, :], in_=h[:, cc * 128:(cc + 1) * 128], identity=identf[:]), reads=[h, identf], writes=[pt], signal=(c == 3))
                            k.op("dve", lambda e, pt=pt, hT=hT, half=half: e.tensor_copy(out=hT[:, half * 4:(half + 1) * 4, :], in_=pt[:]), reads=[pt], writes=[hT])
                        i_ = rows.start // 128
                        if i_ % 4 == 0:
                            rstate["pl"] = psl.next()
                        pl = rstate["pl"]
                        for c in range(8):
                            mmul(pl[:, (i_ % 4) * 128:(i_ % 4 + 1) * 128], rt[:, c, :], hT[:, c, :], c == 0, c == 7, [rt, hT], [pl])
                        if i_ % 4 == 3 or i_ == nn // 128 - 1:
                            nb_ = (i_ % 4 + 1) * 128
                            c0 = (i_ // 4) * 512
                            ee = e_p.next(); rr = r_p.next()
                            k.op("act", lambda e, pl=pl, ee=ee, nb_=nb_: e.activation(out=ee[:, :nb_], in_=pl[:, :nb_], func=AF.Exp), reads=[pl], writes=[ee])
                            pl2 = psl.next()
                            mmul(pl2[:, :nb_], ones16[:], ee[:, :nb_], True, True, [ones16, ee], [pl2])
                            k.op("dve", lambda e, pl2=pl2, rr=rr, nb_=nb_: e.reciprocal(out=rr[:, :nb_], in_=pl2[:, :nb_]), reads=[pl2], writes=[rr])
                            k.op("dve", lambda e, ee=ee, rr=rr, nb_=nb_, c0=c0: e.tensor_tensor(out=aff_all[:, c0:c0 + nb_], in0=ee[:, :nb_], in1=rr[:, :nb_], op=ALU.mult), reads=[ee, rr], writes=[aff_all])

            pend = None
            NTL = nn // 128
            for i0 in range(min(3, NTL)):
                stage0(i0)
            for i in range(NTL):
                if i + 3 < NTL:
                    stage0(i + 3)
                cur = stage1(i)
                if pend is not None:
                    stage2(pend)
                pend = cur
            if pend is not None:
                stage2(pend)
            if kind == 2:
                k.dma("sp", S.affD.ap.ap(), aff_all[:], reads=[aff_all], writes=[S.affD])

    def inproj_phase(specs, l):
        with k.phase():
            wb = k.sbuf("ip_w", [128, 8, IN_COLS], BF16)
            for c in range(8):
                k.dma("pool", wb[:, c, :], w_in.ap[l, c * 128:(c + 1) * 128, :], reads=[w_in], writes=[wb], acc_w=True)
            wp_ = k.sbuf("ip_wp", [128, 8, 512], BF16)
            k.dma("pool", wp_[:], w_qkp.ap[l].rearrange("(c p) n -> p c n", p=128), reads=[w_qkp], writes=[wp_])
            csb = k.sbuf("ip_cs", [128, 256], BF16)
            k.dma("sp", csb[:], cs_c.ap.ap(), reads=[cs_c], writes=[csb])
            lg = k.sbuf("ip_lg", [128, W], F32); lb = k.sbuf("ip_lb", [128, W], F32)
            bc_load(lg, sgu_g, sgu_g.ap[l:l + 1, :], W); bc_load(lb, sgu_b, sgu_b.ap[l:l + 1, :], W)
            for S, kv_only in specs:
              nn, si = S.n, S.si
              TT = 512 if nn >= 512 else nn
              with k.phase():
                hp = Pool(k, "ip_h", 2, [128, 8, TT], BF16)
                ps = Pool(k, "ip_ps", 8, [128, 512], F32, "psum")
                ev = Pool(k, "ip_ev", 4, [128, TT], BF16)
                f32p = Pool(k, "ip_f32", 4, [128, TT], F32)
                rp = Pool(k, "ip_rope", 2, [128, 4, TT], F32)
                zbp = Pool(k, "ip_zb", 2, [128, 2, TT], BF16)
                tmv = Pool(k, "ip_tmv", 3, [128, 512], F32)
                tmb = Pool(k, "ip_tmb", 3, [128, 512], BF16)
                stp = Pool(k, "ip_st", 3, [128, 8], F32)

                def fm(wt, col0, h, reads_w):
                    p = ps.next()
                    for c in range(8):
                        mmul(p[:, :TT], wt[:, c, col0:col0 + 128], h[:, c, :], c == 0, c == 7, [reads_w, h], [p])
                    return p

                def gelu_to(dst_ap, p, width, reads_extra, writes):
                    sq = f32p.next(); t2 = f32p.next()
                    k.op("act", lambda e: e.activation(out=sq[:, :width], in_=p[:, :width], func=AF.Square), reads=[p], writes=[sq])
                    k.op("dve", lambda e: e.tensor_scalar(out=sq[:, :width], in0=sq[:, :width], scalar1=0.044715, scalar2=1.0, op0=ALU.mult, op1=ALU.add), reads=[sq], writes=[sq])
                    k.op("dve", lambda e: e.tensor_tensor(out=t2[:, :width], in0=p[:, :width], in1=sq[:, :width], op=ALU.mult), reads=[p, sq], writes=[t2])
                    k.op("act", lambda e: e.activation(out=t2[:, :width], in_=t2[:, :width], func=AF.Sigmoid, scale=GELU_C), reads=[t2], writes=[t2])
                    k.op("dve", lambda e: e.tensor_tensor(out=dst_ap, in0=p[:, :width], in1=t2[:, :width], op=ALU.mult), reads=[p, t2], writes=writes)

                def ip_loads(j):
                    tk = slice(j * TT, (j + 1) * TT)
                    h = hp.next()
                    k.dma("sp", h[:], S.hT.ap.ap().rearrange("(c p) t -> p c t", p=128)[:, :, tk], reads=[S.hT], writes=[h])
                    rt = None
                    if si == 0:
                        rt = rp.next()
                        k.dma("sp", rt[:], rope.ap.ap().rearrange("f p t -> p f t")[:, :, tk], reads=[rope], writes=[rt])
                    return h, rt
                nxt = ip_loads(0)
                for j in range(nn // TT):
                    tk = slice(j * TT, (j + 1) * TT)
                    h, rt = nxt
                    if j + 1 < nn // TT:
                        nxt = ip_loads(j + 1)
                    fmv = lambda T_: T_.ap.ap().rearrange("(c p) t -> p c t", p=128)
                    if not kv_only:
                        zb = zbp.next()
                        for ch in range(2):
                            p = fm(wb, A_END + ch * 128, h, wb)
                            k.op("act", lambda e, p=p, zb=zb, ch=ch, TT=TT: e.activation(out=zb[:, ch, :], in_=p[:, :TT], func=AF.Copy), reads=[p], writes=[zb])
                    for s_ in range(TT // 128):
                        trow = slice(j * TT + s_ * 128, j * TT + (s_ + 1) * 128)
                        hs = lambda c: h[:, c, s_ * 128:(s_ + 1) * 128]
                        p = ps.next()
                        for c in range(8):
                            mmul(p[:, 0:256], hs(c), wb[:, c, K_END:V_END], c == 0, c == 7, [h, wb], [p])
                        if not kv_only:
                            for c in range(8):
                                mmul(p[:, 256:512], hs(c), wb[:, c, 256:512], c == 0, c == 7, [h, wb], [p])
                        ov = tmb.next()
                        k.op("act", lambda e, ov=ov, p=p: e.activation(out=ov[:, 0:256], in_=p[:, 0:256], func=AF.Copy), reads=[p], writes=[ov])
                        k.dma("sp", S.v.ap[trow, :], ov[:, 0:256], reads=[ov], writes=[S.v], acc_w=True)
                        if kv_only:
                            continue
                        gv = tmv.next(); s8 = stp.next(); ol = tmb.next()
                        sq = f32p.next(); t2 = f32p.next()
                        pv = p
                        k.op("act", lambda e, sq=sq, pv=pv: e.activation(out=sq[:, :256], in_=pv[:, 256:512], func=AF.Square), reads=[pv], writes=[sq])
                        k.op("dve", lambda e, sq=sq: e.tensor_scalar(out=sq[:, :256], in0=sq[:, :256], scalar1=0.044715, scalar2=1.0, op0=ALU.mult, op1=ALU.add), reads=[sq], writes=[sq])
                        k.op("dve", lambda e, sq=sq, t2=t2, pv=pv: e.tensor_tensor(out=t2[:, :256], in0=pv[:, 256:512], in1=sq[:, :256], op=ALU.mult), reads=[pv, sq], writes=[t2])
                        k.op("act", lambda e, t2=t2: e.activation(out=t2[:, :256], in_=t2[:, :256], func=AF.Sigmoid, scale=GELU_C), reads=[t2], writes=[t2])
                        k.op("dve", lambda e, gv=gv, t2=t2, pv=pv: e.tensor_tensor(out=gv[:, :256], in0=pv[:, 256:512], in1=t2[:, :256], op=ALU.mult), reads=[pv, t2], writes=[gv])
                        k.op("dve", lambda e, gv=gv, s8=s8: e.bn_stats(out=s8[:, 0:6], in_=gv[:, :256]), reads=[gv], writes=[s8])
                        k.op("dve", lambda e, s8=s8: e.bn_aggr(out=s8[:, 6:8], in_=s8[:, 0:6]), reads=[s8], writes=[s8])
                        k.op("act", lambda e, s8=s8: e.activation(out=s8[:, 0:1], in_=s8[:, 7:8], func=AF.Sqrt, bias=1e-6, scale=1.0), reads=[s8], writes=[s8])
                        k.op("dve", lambda e, s8=s8: e.reciprocal(out=s8[:, 1:2], in_=s8[:, 0:1]), reads=[s8], writes=[s8])
                        k.op("dve", lambda e, gv=gv, s8=s8: e.tensor_scalar(out=gv[:, :256], in0=gv[:, :256], scalar1=s8[:, 6:7], scalar2=s8[:, 1:2], op0=ALU.subtract, op1=ALU.mult), reads=[gv, s8], writes=[gv])
                        k.op("pool", lambda e, gv=gv: e.tensor_tensor(out=gv[:, :256], in0=gv[:, :256], in1=lg[:], op=ALU.mult), reads=[gv, lg], writes=[gv])
                        k.op("pool", lambda e, gv=gv, ol=ol: e.tensor_tensor(out=ol[:, :256], in0=gv[:, :256], in1=lb[:], op=ALU.add), reads=[gv, lb], writes=[ol])
                        k.dma("sp", S.vln.ap[trow, :], ol[:, :256], reads=[ol], writes=[S.vln], acc_w=True)
                        pz = ps.next()
                        for ch in range(2):
                            mmul(pz[:, ch * 256:(ch + 1) * 256], zb[:, ch, s_ * 128:(s_ + 1) * 128], csb[:], True, True, [zb, csb], [pz])
                        oz = tmb.next()
                        k.op("act", lambda e, oz=oz, pz=pz: e.activation(out=oz[:], in_=pz[:], func=AF.Copy), reads=[pz], writes=[oz])
                        ozv = oz[:].rearrange("p (c r f) -> p c r f", c=2, r=2)
                        k.dma("sp", S.Zr.ap[trow, :].rearrange("t (c f) -> t c f", c=2), ozv[:, :, 0, :], reads=[oz], writes=[S.Zr], acc_w=True)
                        k.dma("sp", S.Zi.ap[trow, :].rearrange("t (c f) -> t c f", c=2), ozv[:, :, 1, :], reads=[oz], writes=[S.Zi], acc_w=True)
                    if not kv_only:
                        for ch in range(2):
                            p = fm(wb, ch * 128, h, wb); o = ev.next()
                            gelu_to(o[:], p, TT, [], [o])
                            k.dma("sp", fmv(S.uT)[:, ch, tk], o[:], reads=[o], writes=[S.uT], acc_w=True)
                        for ch in range(2):
                            pa = fm(wb, B_END + ch * 128, h, wb); pg = fm(wb, B_END + 256 + ch * 128, h, wb)
                            sg = f32p.next(); o = ev.next()
                            k.op("act", lambda e, sg=sg, pg=pg, TT=TT: e.activation(out=sg[:], in_=pg[:, :TT], func=AF.Sigmoid), reads=[pg], writes=[sg])
                            k.op("dve", lambda e, o=o, pa=pa, sg=sg, TT=TT: e.tensor_tensor(out=o[:], in0=pa[:, :TT], in1=sg[:], op=ALU.mult), reads=[pa, sg], writes=[o])
                            k.dma("sp", fmv(S.yT)[:, ch, tk], o[:], reads=[o], writes=[S.yT], acc_w=True)
                    for which, col0, pc0, dstr, dstp in ((0, C_END, 0, S.qrT, S.qpT), (1, Q_END, 256, S.krT, None)):
                        if kv_only and which == 0:
                            continue
                        for ch in range(2):
                            p = fm(wb, col0 + ch * 128, h, wb)
                            if si == 0:
                                pp_ = fm(wp_, pc0 + ch * 128, h, wp_)
                                t1 = f32p.next(); t2 = f32p.next(); o = ev.next()
                                k.op("dve", lambda e, t1=t1, p=p, rt=rt, which=which, TT=TT: e.tensor_tensor(out=t1[:], in0=p[:, :TT], in1=rt[:, 2 * which, :], op=ALU.mult), reads=[p, rt], writes=[t1])
                                k.op("dve", lambda e, t2=t2, pp_=pp_, rt=rt, which=which, TT=TT: e.tensor_tensor(out=t2[:], in0=pp_[:, :TT], in1=rt[:, 2 * which + 1, :], op=ALU.mult), reads=[pp_, rt], writes=[t2])
                                k.op("pool", lambda e, o=o, t1=t1, t2=t2: e.tensor_tensor(out=o[:], in0=t1[:], in1=t2[:], op=ALU.add), reads=[t1, t2], writes=[o])
                                k.dma("sp", fmv(dstr)[:, ch, tk], o[:], reads=[o], writes=[dstr], acc_w=True)
                                if which == 0:
                                    o2 = ev.next()
                                    k.op("act", lambda e, o2=o2, p=p, TT=TT: e.activation(out=o2[:], in_=p[:, :TT], func=AF.Copy, scale=0.125), reads=[p], writes=[o2])
                                    k.dma("sp", fmv(dstp)[:, ch, tk], o2[:], reads=[o2], writes=[dstp], acc_w=True)
                            else:
                                o = ev.next()
                                if which == 0:
                                    k.op("act", lambda e, o=o, p=p, TT=TT: e.activation(out=o[:], in_=p[:, :TT], func=AF.Copy, scale=0.125), reads=[p], writes=[o])
                                    k.dma("sp", fmv(S.qpT)[:, ch, tk], o[:], reads=[o], writes=[S.qpT], acc_w=True)
                                else:
                                    k.op("act", lambda e, o=o, p=p, TT=TT: e.activation(out=o[:], in_=p[:, :TT], func=AF.Copy), reads=[p], writes=[o])
                                    k.dma("sp", fmv(S.krT)[:, ch, tk], o[:], reads=[o], writes=[S.krT], acc_w=True)
                    if not kv_only:
                        for ch in range(32):
                            p = fm(wb, V_END + ch * 128, h, wb); o = ev.next()
                            k.op("act", lambda e, p=p, o=o, TT=TT: e.activation(out=o[:], in_=p[:, :TT], func=AF.Sigmoid), reads=[p], writes=[o])
                            k.dma("sp", S.gT.ap.ap().rearrange("(c p) t -> p c t", p=128)[:, ch, tk], o[:], reads=[o], writes=[S.gT], acc_w=True)

    def gmlp_phase(S, l):
        nn = S.n
        TT = 512 if nn >= 512 else nn
        NCH = TT // 128
        fmv = lambda T_: T_.ap.ap().rearrange("(c p) t -> p c t", p=128)
        with k.phase():
            ws = k.sbuf("gm_ws", [128, 4, 128], BF16)
            k.dma("pool", ws[:], wsT.ap[l], reads=[wsT], writes=[ws])
            bs = k.sbuf("gm_bs", [1, 4, 128], BF16)
            k.dma("pool", bs[:], bsp.ap[l:l + 1], reads=[bsp], writes=[bs])
            ones = k.sbuf("gm_ones", [1, 128], BF16)
            k.op("dve", lambda e: e.memset(ones[:], 1.0), writes=[ones])
            vp = Pool(k, "gm_v", 2, [128, NCH, W], BF16)
            up = Pool(k, "gm_u", 2, [128, 2, TT], BF16)
            ap_ = Pool(k, "gm_a", 2, [128, 2, TT], BF16)
            pp = Pool(k, "gm_ps", 8, [128, NCH, 128], F32, "psum")
            def gm_loads(j):
                tk = slice(j * TT, (j + 1) * TT)
                vt = vp.next(); ut = up.next()
                k.dma("sp", vt[:], S.vln.ap[tk, :].rearrange("(c p) f -> p c f", p=128), reads=[S.vln], writes=[vt])
                k.dma("sp", ut[:], fmv(S.uT)[:, :, tk], reads=[S.uT], writes=[ut])
                return vt, ut
            nxt = gm_loads(0)
            for j in range(nn // TT):
                tk = slice(j * TT, (j + 1) * TT)
                vt, ut = nxt
                if j + 1 < nn // TT:
                    nxt = gm_loads(j + 1)
                at = ap_.next()
                for hf in range(2):
                    for gi in range(2):
                        g = 2 * hf + gi
                        p = pp.next()
                        for cc in range(NCH):
                            mmul(p[:, cc, :], vt[:, cc, hf * 128:(hf + 1) * 128], ws[:, g, :], True, False, [vt, ws], [p])
                            mmul(p[:, cc, :], ones[:], bs[:, g, :], False, True, [ones, bs], [p])
                        pr = slice(gi * 64, (gi + 1) * 64)
                        k.op("dve", lambda e, at=at, p=p, ut=ut, pr=pr, hf=hf: e.tensor_tensor(out=at[pr, hf, :].rearrange("p (c q) -> p c q", q=128), in0=p[pr, :, :], in1=ut[pr, hf, :].rearrange("p (c q) -> p c q", q=128), op=ALU.mult), reads=[p, ut], writes=[at])
                k.dma("sp", fmv(S.aT)[:, :, tk], at[:], reads=[at], writes=[S.aT], acc_w=True)

    def conv_phase(S, l):
        nn = S.n
        TT = 512 if nn >= 512 else nn
        fmv = lambda T_: T_.ap.ap().rearrange("(c p) t -> p c t", p=128)
        with k.phase():
            identf, = load_ident(False, True)
            cw = k.sbuf("cv_w", [128, 2, 31], F32)
            k.dma("sp", cw[:], conv_wT.ap[l].rearrange("(h p) j -> p h j", p=128), reads=[conv_wT], writes=[cw])
            prm = k.sbuf("cv_prm", [128, 3, 2], F32)
            for i_, src in enumerate((conv_b, cln_g, cln_b)):
                for hf in range(2):
                    k.dma("sp", prm[:, i_, hf:hf + 1], src.ap[l, hf * 128:(hf + 1) * 128, :], reads=[src], writes=[prm], acc_w=True)
            dg = k.sbuf("cv_diag", [128, 2, 31, 128], BF16)
            for hf in range(2):
                for j in range(31):
                    k.op("dve", lambda e, hf=hf, j=j: e.tensor_scalar(out=dg[:, hf, j, :], in0=identf[:], scalar1=cw[:, hf, j:j + 1], scalar2=None, op0=ALU.mult), reads=[identf, cw], writes=[dg])
            onesf = k.sbuf("cv_ones", [128, 128], F32)
            k.op("dve", lambda e: e.memset(onesf[:], 1.0 / W), writes=[onesf])
            yp = Pool(k, "cv_y", 2, [128, 2, TT + 30], BF16)
            pc = Pool(k, "cv_pc", 4, [128, TT], F32, "psum")
            pst = Pool(k, "cv_pst", 2, [128, 2, TT], F32, "psum")
            y2p = Pool(k, "cv_y2", 3, [128, 2, TT], F32)
            sqp = Pool(k, "cv_sq", 3, [128, 2, TT], F32)
            stp = Pool(k, "cv_st", 2, [128, 2, TT], F32)
            op_ = Pool(k, "cv_o", 2, [128, 2, TT], BF16)
            def cv_a(j):
                    t0 = j * TT
                    yt = yp.next()
                    lo, hi = max(0, t0 - 15), min(nn, t0 + TT + 15)
                    if lo > t0 - 15 or hi < t0 + TT + 15:
                        k.op("pool", lambda e, yt=yt: e.memset(yt[:], 0.0), writes=[yt])
                    k.dma("sp", yt[:, :, lo - (t0 - 15):hi - (t0 - 15)], fmv(S.yT)[:, :, lo:hi], reads=[S.yT], writes=[yt])
                    y2 = y2p.next(); sq = sqp.next()
                    for hf in range(2):
                        p = pc.next()
                        for jj in range(31):
                            mmul(p[:], dg[:, hf, jj, :], yt[:, hf, jj:jj + TT], jj == 0, jj == 30, [dg, yt], [p])
                        k.op("act", lambda e, y2=y2, p=p, hf=hf: e.activation(out=y2[:, hf, :], in_=p[:], func=AF.Identity, bias=prm[:, 0, hf:hf + 1], scale=1.0), reads=[p, prm], writes=[y2])
                        k.op("act", lambda e, y2=y2, sq=sq, hf=hf: e.activation(out=sq[:, hf, :], in_=y2[:, hf, :], func=AF.Square), reads=[y2], writes=[sq])
                    return (t0, y2, sq)
            def cv_b(ctx_):
                    t0, y2, sq = ctx_
                    ps_ = pst.next()
                    for hf in range(2):
                        mmul(ps_[:, 0, :], onesf[:], y2[:, hf, :], hf == 0, hf == 1, [onesf, y2], [ps_])
                    for hf in range(2):
                        mmul(ps_[:, 1, :], onesf[:], sq[:, hf, :], hf == 0, hf == 1, [onesf, sq], [ps_])
                    stt = stp.next(); ot = op_.next()
                    k.op("act", lambda e, stt=stt, ps_=ps_: e.activation(out=stt[:, 0, :], in_=ps_[:, 0, :], func=AF.Copy), reads=[ps_], writes=[stt])
                    k.op("dve", lambda e, stt=stt: e.tensor_tensor(out=stt[:, 1, :], in0=stt[:, 0, :], in1=stt[:, 0, :], op=ALU.mult), reads=[stt], writes=[stt])
                    k.op("dve", lambda e, stt=stt, ps_=ps_: e.tensor_tensor(out=stt[:, 1, :], in0=ps_[:, 1, :], in1=stt[:, 1, :], op=ALU.subtract), reads=[ps_, stt], writes=[stt])
                    k.op("act", lambda e, stt=stt: e.activation(out=stt[:, 1, :], in_=stt[:, 1, :], func=AF.Sqrt, bias=1e-6, scale=1.0), reads=[stt], writes=[stt])
                    k.op("dve", lambda e, stt=stt: e.reciprocal(out=stt[:, 1, :], in_=stt[:, 1, :]), reads=[stt], writes=[stt])
                    for hf in range(2):
                        k.op("dve", lambda e, y2=y2, stt=stt, hf=hf: e.tensor_tensor(out=y2[:, hf, :], in0=y2[:, hf, :], in1=stt[:, 0, :], op=ALU.subtract), reads=[y2, stt], writes=[y2])
                        k.op("pool", lambda e, y2=y2, stt=stt, hf=hf: e.tensor_tensor(out=y2[:, hf, :], in0=y2[:, hf, :], in1=stt[:, 1, :], op=ALU.mult), reads=[y2, stt], writes=[y2])
                        k.op("act", lambda e, y2=y2, ot=ot, hf=hf: e.activation(out=ot[:, hf, :], in_=y2[:, hf, :], func=AF.Silu, bias=prm[:, 2, hf:hf + 1], scale=prm[:, 1, hf:hf + 1]), reads=[y2, prm], writes=[ot])
                    k.dma("sp", fmv(S.cT)[:, :, t0:t0 + TT], ot[:], reads=[ot], writes=[S.cT], acc_w=True)
            pend = None
            for j in range(nn // TT):
                cur = cv_a(j)
                if pend is not None:
                    cv_b(pend)
                pend = cur
            cv_b(pend)

    def fourier_phase(S, l):
        nn, NN1 = S.n, S.N1
        fmv = lambda T_: T_.ap.ap().rearrange("(c p) t -> p c t", p=128)
        CB = 4096
        with k.phase():
            f1sb = k.sbuf("ff_f1", [NN1, 3, NN1], BF16)
            k.dma("sp", f1sb[:], S.f1.ap.ap().rearrange("m c q -> c m q"), reads=[S.f1], writes=[f1sb])
            zp = Pool(k, "ff_z", 4, [NN1, CB], BF16)
            yp = Pool(k, "ff_y", 4, [NN1, CB], BF16)
            pp = Pool(k, "ff_ps", 4, [NN1, 512], F32, "psum")
            Zrv = S.Zr.ap.ap().rearrange("(c p) f -> c (p f)", p=128)
            Ziv = S.Zi.ap.ap().rearrange("(c p) f -> c (p f)", p=128)
            def ff_loads(blk):
                cb = slice(blk * CB, (blk + 1) * CB)
                zr = zp.next(); zi = zp.next()
                k.dma("sp", zr[:], Zrv[:, cb], reads=[S.Zr], writes=[zr])
                k.dma("sp", zi[:], Ziv[:, cb], reads=[S.Zi], writes=[zi])
                return zr, zi
            nxt = ff_loads(0)
            for blk in range(128 * W // CB):
                cb = slice(blk * CB, (blk + 1) * CB)
                zr, zi = nxt
                if blk + 1 < 128 * W // CB:
                    nxt = ff_loads(blk + 1)
                yr = yp.next(); yi = yp.next()
                for sub in range(CB // 512):
                    cs_ = slice(sub * 512, (sub + 1) * 512)
                    pr = pp.next(); pi = pp.next()
                    mmul(pr[:], f1sb[:, 0, :], zr[:, cs_], True, False, [f1sb, zr], [pr])
                    mmul(pr[:], f1sb[:, 1, :], zi[:, cs_], False, True, [f1sb, zi], [pr])
                    mmul(pi[:], f1sb[:, 0, :], zi[:, cs_], True, False, [f1sb, zi], [pi])
                    mmul(pi[:], f1sb[:, 2, :], zr[:, cs_], False, True, [f1sb, zr], [pi])
                    k.op("act", lambda e, yr=yr, pr=pr, cs_=cs_: e.activation(out=yr[:, cs_], in_=pr[:], func=AF.Copy), reads=[pr], writes=[yr])
                    k.op("dve", lambda e, yi=yi, pi=pi, cs_=cs_: e.tensor_copy(out=yi[:, cs_], in_=pi[:]), reads=[pi], writes=[yi])
                k.dma("sp", S.Y1r.ap[:, cb], yr[:], reads=[yr], writes=[S.Y1r], acc_w=True)
                k.dma("sp", S.Y1i.ap[:, cb], yi[:], reads=[yi], writes=[S.Y1i], acc_w=True)
        with k.phase():
            KB = min(8, NN1)
            G = min(4, KB)
            f2sb = k.sbuf("ff_f2", [128, 2, 128], BF16)
            k.dma("sp", f2sb[:], f2.ap.ap().rearrange("m p q -> p m q"), reads=[f2], writes=[f2sb])
            tw = k.sbuf("ff_tw", [128, 2, NN1], F32)
            k.dma("sp", tw[:], S.tw.ap.ap().rearrange("m p q -> p m q"), reads=[S.tw], writes=[tw])
            outT = k.sbuf("ff_out", [128, 2, nn], BF16)
            yrp = Pool(k, "ff_yr", 2, [128, KB, W], BF16)
            yip = Pool(k, "ff_yi", 2, [128, KB, W], BF16)
            ypr = Pool(k, "ff_ypr", 2, [128, KB, W], BF16)
            ypi = Pool(k, "ff_ypi", 2, [128, KB, W], BF16)
            tp = Pool(k, "ff_t", 6, [128, W], F32)
            po = Pool(k, "ff_po", 4, [128, G, 128], F32, "psum")
            Y1rv = S.Y1r.ap.ap().rearrange("q (p f) -> p q f", p=128)
            Y1iv = S.Y1i.ap.ap().rearrange("q (p f) -> p q f", p=128)
            def f2_loads(kb):
                ks = slice(kb * KB, (kb + 1) * KB)
                yr = yrp.next(); yi = yip.next()
                k.dma("sp", yr[:], Y1rv[:, ks, :], reads=[S.Y1r], writes=[yr])
                k.dma("sp", yi[:], Y1iv[:, ks, :], reads=[S.Y1i], writes=[yi])
                return yr, yi
            nxt = f2_loads(0)
            for kb in range(NN1 // KB):
                ks = slice(kb * KB, (kb + 1) * KB)
                yr, yi = nxt
                if kb + 1 < NN1 // KB:
                    nxt = f2_loads(kb + 1)
                qr = ypr.next(); qi = ypi.next()
                for kk in range(KB):
                    k1 = kb * KB + kk
                    t1 = tp.next(); t2 = tp.next()
                    k.op("act", lambda e, t1=t1, yi=yi, kk=kk, k1=k1: e.activation(out=t1[:], in_=yi[:, kk, :], func=AF.Copy, scale=tw[:, 1, k1:k1 + 1]), reads=[yi, tw], writes=[t1])
                    k.op("dve", lambda e, t1=t1, yr=yr, qr=qr, kk=kk, k1=k1: e.scalar_tensor_tensor(out=qr[:, kk, :], in0=yr[:, kk, :], scalar=tw[:, 0, k1:k1 + 1], in1=t1[:], op0=ALU.mult, op1=ALU.subtract), reads=[yr, tw, t1], writes=[qr])
                    k.op("act", lambda e, t2=t2, yi=yi, kk=kk, k1=k1: e.activation(out=t2[:], in_=yi[:, kk, :], func=AF.Copy, scale=tw[:, 0, k1:k1 + 1]), reads=[yi, tw], writes=[t2])
                    k.op("dve", lambda e, t2=t2, yr=yr, qi=qi, kk=kk, k1=k1: e.scalar_tensor_tensor(out=qi[:, kk, :], in0=yr[:, kk, :], scalar=tw[:, 1, k1:k1 + 1], in1=t2[:], op0=ALU.mult, op1=ALU.add), reads=[yr, tw, t2], writes=[qi])
                for fh in range(2):
                    fs = slice(fh * 128, (fh + 1) * 128)
                    for g0 in range(0, KB, G):
                        p = po.next()
                        for gg in range(G):
                            kk = g0 + gg
                            mmul(p[:, gg, :], qr[:, kk, fs], f2sb[:, 0, :], True, False, [qr, f2sb], [p])
                            mmul(p[:, gg, :], qi[:, kk, fs], f2sb[:, 1, :], False, True, [qi, f2sb], [p])
                        k1_0 = kb * KB + g0
                        dst = outT[:, fh, :].rearrange("p (a b) -> p b a", b=NN1)[:, k1_0:k1_0 + G, :]
                        k.op("dve", lambda e, dst=dst, p=p: e.tensor_copy(out=dst, in_=p[:]), reads=[p], writes=[outT])
            k.dma("sp", fmv(S.bT), outT[:], reads=[outT], writes=[S.bT])

    def attention_phase(S, SCx, l):
        nn, si = S.n, S.si
        NT = nn // 128
        fmv = lambda T_: T_.ap.ap().rearrange("(c p) t -> p c t", p=128)
        with k.phase():
            identb, = load_ident(True, False)
            kc = k.sbuf("at_kc", [128, 2, NCTX], BF16)
            k.dma("sp", kc[:], fmv(SCx.krT), reads=[SCx.krT], writes=[kc])
            vc = k.sbuf("at_vc", [128, 2, 4, 65], BF16)
            k.op("dve", lambda e: e.memset(vc[:], 1.0), writes=[vc])
            for t_ in range(2):
                k.dma("sp", vc[:, t_, :, 0:64], SCx.v.ap[t_ * 128:(t_ + 1) * 128, :].rearrange("p (h d) -> p h d", h=4), reads=[SCx.v], writes=[vc], acc_w=True)
            if si == 0:
                kr = k.sbuf("at_kr", [128, 2, nn], BF16)
                k.dma("sp", kr[:], fmv(S.krT), reads=[S.krT], writes=[kr])
                vs = k.sbuf("at_v", [128, NT, 4, 65], BF16)
                k.op("dve", lambda e: e.memset(vs[:], 1.0), writes=[vs])
                for t_ in range(NT):
                    k.dma("sp", vs[:, t_, :, 0:64], S.v.ap[t_ * 128:(t_ + 1) * 128, :].rearrange("p (h d) -> p h d", h=4), reads=[S.v], writes=[vs], acc_w=True)
                bias = k.sbuf("at_bias", [128, 4 * U, 128], BF16)
                k.dma("sp", bias[:], biasT.ap[l], reads=[biasT], writes=[bias])
            qrp = Pool(k, "at_qr", 3, [128, 2, 128], BF16)
            qpp = Pool(k, "at_qp", 3, [128, 2, 128], BF16)
            psS = Pool(k, "at_S", 2, [128, 8, 128], F32, "psum")
            psO = Pool(k, "at_O", 2, [128, 128], F32, "psum")
            psT = Pool(k, "at_T", 1, [128, 2, 128], BF16, "psum")
            Ep = Pool(k, "at_E", 3, [128, 8, 128], BF16)
            rcp = Pool(k, "at_rc", 4, [128, 1], F32)
            otp = Pool(k, "at_ot", 3, [128, W], BF16)
            dtp = Pool(k, "at_dt", 2, [128, 2, 128], BF16)
            items = [(qt, h) for qt in range(NT) for h in range(4)]
            qst = {}

            def stage_s(qt, h):
                qs = slice(qt * 128, (qt + 1) * 128)
                if h == 0:
                    qp_t = qpp.next()
                    k.dma("sp", qp_t[:], fmv(S.qpT)[:, :, qs], reads=[S.qpT], writes=[qp_t])
                    qr_t = None
                    lst = []
                    if si == 0:
                        qr_t = qrp.next()
                        k.dma("sp", qr_t[:], fmv(S.qrT)[:, :, qs], reads=[S.qrT], writes=[qr_t])
                        lst = qtiles[qt]
                    qst[qt] = (qp_t, qr_t, lst)
                qp_t, qr_t, lst = qst[qt]
                nw = len(lst); nt = nw + 2
                ch, pr = h // 2, slice((h % 2) * 64, (h % 2) * 64 + 64)
                Sp = psS.next(); E = Ep.next()
                for j_, (kt, u) in enumerate(lst):
                    mmul(Sp[:, j_, :], identb[:], bias[:, h * U + u, :], True, False, [identb, bias], [Sp])
                    mmul(Sp[:, j_, :], kr[pr, ch, kt * 128:(kt + 1) * 128], qr_t[pr, ch, :], False, True, [kr, qr_t], [Sp])
                for cj in range(2):
                    mmul(Sp[:, nw + cj, :], kc[pr, ch, cj * 128:(cj + 1) * 128], qp_t[pr, ch, :], True, True, [kc, qp_t], [Sp])
                k.op("act", lambda e, E=E, Sp=Sp, nt=nt: e.activation(out=E[:, 0:nt, :], in_=Sp[:, 0:nt, :], func=AF.Exp), reads=[Sp], writes=[E])
                return E

            cur_ot = {}

            def stage_pv(qt, h, E):
                qs = slice(qt * 128, (qt + 1) * 128)
                qp_t, qr_t, lst = qst[qt]
                nw = len(lst); nt = nw + 2
                if h == 0:
                    cur_ot[qt] = otp.next()
                ot = cur_ot[qt]
                O = psO.next(); rc = rcp.next()
                for j_ in range(nt):
                    rhs = vs[:, lst[j_][0], h, :] if j_ < nw else vc[:, j_ - nw, h, :]
                    mmul(O[:, 0:65], E[:, j_, :], rhs, j_ == 0, j_ == nt - 1, [E, vc] + ([vs] if si == 0 else []), [O])
                k.op("dve", lambda e, rc=rc, O=O: e.reciprocal(out=rc[:], in_=O[:, 64:65]), reads=[O], writes=[rc])
                k.op("dve", lambda e, ot=ot, O=O, rc=rc, h=h: e.tensor_scalar(out=ot[:, h * 64:(h + 1) * 64], in0=O[:, 0:64], scalar1=rc[:, 0:1], scalar2=None, op0=ALU.mult), reads=[O, rc], writes=[ot])
                if h == 3:
                    pt = psT.next(); dt = dtp.next()
                    for c in range(2):
                        k.op("pe", lambda e, c=c, pt=pt, ot=ot: e.transpose(out=pt[:, c, :], in_=ot[:, c * 128:(c + 1) * 128], identity=identb[:]), reads=[ot, identb], writes=[pt], signal=(c == 1))
                    k.op("act", lambda e, pt=pt, dt=dt: e.activation(out=dt[:], in_=pt[:], func=AF.Copy), reads=[pt], writes=[dt])
                    k.dma("sp", fmv(S.dT)[:, :, qs], dt[:], reads=[dt], writes=[S.dT], acc_w=True)

            Es = {0: stage_s(*items[0])}
            for i_, (qt, h) in enumerate(items):
                if i_ + 1 < len(items):
                    Es[i_ + 1] = stage_s(*items[i_ + 1])
                stage_pv(qt, h, Es.pop(i_))

    def merge_phase(S, l, Xsrc, Xdst):
        nn, si = S.n, S.si
        TT = 512 if nn >= 512 else nn
        fmv = lambda T_: T_.ap.ap().rearrange("(c p) t -> p c t", p=128)
        with k.phase():
            wbr = []
            for br in range(4):
                t = k.sbuf("mg_wbr", [128, 2, D], BF16)
                k.dma("pool", t[:], w_br[br].ap[l].rearrange("(c p) n -> p c n", p=128), reads=[w_br[br]], writes=[t])
                wbr.append(t)
            wo = k.sbuf("mg_wo", [128, 8, D], BF16)
            k.dma("pool", wo[:], w_out.ap[l].rearrange("(c p) n -> p c n", p=128), reads=[w_out], writes=[wo])
            identb, = load_ident(True, False)
            gtb = k.sbuf("mg_gt", [128, D], F32)
            bc_load(gtb, modD[l], modD[l].ap[si:si + 1, 2 * D:3 * D], D)
            brp = [Pool(k, "mg_br%d" % i, 2, [128, 2, TT], BF16) for i in range(4)]
            gp = Pool(k, "mg_g", 2, [128, 32, TT], BF16)
            ps = Pool(k, "mg_ps", 8, [128, 512], F32, "psum")
            tp = Pool(k, "mg_t", 8, [128, TT], BF16)
            mp = Pool(k, "mg_m", 2, [128, 8, TT], BF16)
            xp = Pool(k, "mg_x", 2, [128, D], F32)
            tmp = Pool(k, "mg_tmp", 2, [128, D], F32)
            xop = Pool(k, "mg_xo", 2, [128, D], F32)
            srcs = [S.aT, S.bT, S.cT, S.dT]
            def out_group(j, mT, g_, st_):
                s_, half = g_ // 2, g_ % 2
                rows = slice(j * TT + s_ * 128, j * TT + (s_ + 1) * 128)
                if half == 0:
                    st_["xt"] = xp.next(); st_["tm"] = tmp.next(); st_["xo"] = xop.next()
                    k.dma("sp", st_["xt"][:], Xsrc.ap[rows, :], reads=[Xsrc], writes=[st_["xt"]])
                xt, tm, xo = st_["xt"], st_["tm"], st_["xo"]
                hs = slice(half * 512, (half + 1) * 512)
                p = ps.next()
                for kc in range(8):
                    mmul(p[:], mT[:, kc, s_ * 128:(s_ + 1) * 128], wo[:, kc, hs], kc == 0, kc == 7, [mT, wo], [p])
                k.op("dve", lambda e, tm=tm, p=p, hs=hs: e.tensor_tensor(out=tm[:, hs], in0=p[:], in1=gtb[:, hs], op=ALU.mult), reads=[p, gtb], writes=[tm])
                if half == 1:
                    k.op("pool", lambda e, xo=xo, tm=tm, xt=xt: e.tensor_tensor(out=xo[:], in0=tm[:], in1=xt[:], op=ALU.add), reads=[tm, xt], writes=[xo])
                    k.dma("sp", Xdst.ap[rows, :], xo[:], reads=[xo], writes=[Xdst], acc_w=True)

            NG = (TT // 128) * 2
            prev = None
            def mg_loads(j):
                tk = slice(j * TT, (j + 1) * TT)
                bts = []
                for br in range(4):
                    t = brp[br].next()
                    k.dma("sp", t[:], fmv(srcs[br])[:, :, tk], reads=[srcs[br]], writes=[t])
                    bts.append(t)
                g = gp.next()
                for q4 in range(4):
                    k.dma("sp", g[:, q4 * 8:(q4 + 1) * 8, :], S.gT.ap.ap().rearrange("(c p) t -> p c t", p=128)[:, q4 * 8:(q4 + 1) * 8, tk], reads=[S.gT], writes=[g], acc_w=True)
                return bts, g
            nxt = mg_loads(0)
            for j in range(nn // TT):
                tk = slice(j * TT, (j + 1) * TT)
                bts, g = nxt
                if j + 1 < nn // TT:
                    nxt = mg_loads(j + 1)
                mT = mp.next()
                st_ = {}
                for oc in range(8):
                    ts = []
                    for br in range(4):
                        p = ps.next()
                        for kc in range(2):
                            mmul(p[:, :TT], wbr[br][:, kc, oc * 128:(oc + 1) * 128], bts[br][:, kc, :], kc == 0, kc == 1, [wbr[br], bts[br]], [p])
                        t = tp.next()
                        k.op("dve", lambda e, t=t, p=p, g=g, br=br, oc=oc: e.tensor_tensor(out=t[:], in0=p[:, :TT], in1=g[:, br * 8 + oc, :], op=ALU.mult), reads=[p, g], writes=[t])
                        ts.append(t)
                    if prev is not None and oc < NG:
                        out_group(prev[0], prev[1], oc, st_)
                    pm = ps.next()
                    for br in range(4):
                        mmul(pm[:, :TT], identb[:], ts[br][:], br == 0, br == 3, [identb, ts[br]], [pm])
                    k.op("act", lambda e, pm=pm, mT=mT, oc=oc: e.activation(out=mT[:, oc, :], in_=pm[:, :TT], func=AF.Copy), reads=[pm], writes=[mT])
                prev = (j, mT)
            st_ = {}
            for g_ in range(NG):
                out_group(prev[0], prev[1], g_, st_)

    def route_phase(S, l):
        nn, cap_ = S.n, S.cap
        Tseg = nn // 8
        NCH = nn // 128
        if not hasattr(S, "slot2D"):
            S.slot2D = scr(S.name + "_slot2D", [NE, nn], F32)
        seg = lambda T_: T_.ap.ap().rearrange("e (s t) -> (e s) t", s=8)
        with k.phase():
            blk = k.sbuf("rt_blk", [128, 128], F32); low = k.sbuf("rt_low", [128, 128], F32)
            k.dma("sp", blk[:], r_blk.ap.ap(), reads=[r_blk], writes=[blk])
            k.dma("sp", low[:], r_low.ap.ap(), reads=[r_low], writes=[low])
            A = k.sbuf("rt_A", [128, Tseg], F32)
            k.dma("sp", A[:], seg(S.affD), reads=[S.affD], writes=[A])
            junk = k.sbuf("rt_junk", [128, Tseg], F32)
            sv = k.sbuf("rt_sv", [128, 8], F32)
            k.op("dve", lambda e: e.memset(sv[:], 0.0), writes=[sv])
            k.op("dve", lambda e: e.memset(sv[:, 1:2], 1.0), reads=[sv], writes=[sv])
            pc = Pool(k, "rt_pc", 2, [128, 1], F32, "psum")
            for it in range(30):
                step = 2.0 ** -(it + 1)
                k.op("dve", lambda e, step=step: e.tensor_scalar(out=sv[:, 2:3], in0=sv[:, 0:1], scalar1=step, scalar2=None, op0=ALU.add), reads=[sv], writes=[sv])
                k.op("dve", lambda e: e.tensor_scalar(out=junk[:], in0=A[:], scalar1=sv[:, 2:3], scalar2=0.0, op0=ALU.is_ge, op1=ALU.add, accum_out=sv[:, 3:4]), reads=[A, sv], writes=[junk, sv])
                p = pc.next()
                mmul(p[:], blk[:], sv[:, 3:4], True, True, [blk, sv], [p])
                k.op("dve", lambda e, p=p, step=step: e.tensor_scalar(out=sv[:, 4:5], in0=p[:], scalar1=float(cap_), scalar2=step, op0=ALU.is_ge, op1=ALU.mult), reads=[p, sv], writes=[sv])
                k.op("dve", lambda e: e.tensor_tensor(out=sv[:, 0:1], in0=sv[:, 0:1], in1=sv[:, 4:5], op=ALU.add), reads=[sv], writes=[sv])
            mask = k.sbuf("rt_mask", [128, Tseg], F32)
            k.op("dve", lambda e: e.tensor_scalar(out=mask[:], in0=A[:], scalar1=sv[:, 0:1], scalar2=None, op0=ALU.is_ge), reads=[A, sv], writes=[mask])
            k.op("pool", lambda e: e.memset(junk[:], 0.0), writes=[junk])
            cs = k.sbuf("rt_cs", [128, Tseg], F32)
            k.op("dve", lambda e: e.tensor_tensor_scan(out=cs[:], data0=mask[:], data1=junk[:], initial=0.0, op0=ALU.add, op1=ALU.add), reads=[mask, junk], writes=[cs])
            k.op("dve", lambda e: e.tensor_copy(out=sv[:, 6:7], in_=cs[:, Tseg - 1:Tseg]), reads=[cs, sv], writes=[sv])
            p = pc.next()
            mmul(p[:], low[:], sv[:, 6:7], True, True, [low, sv], [p])
            k.op("dve", lambda e, p=p: e.tensor_copy(out=sv[:, 7:8], in_=p[:]), reads=[p, sv], writes=[sv])
            INV = 2016.0
            k.op("dve", lambda e: e.tensor_scalar(out=cs[:], in0=cs[:], scalar1=sv[:, 7:8], scalar2=-(INV + 1.0), op0=ALU.add, op1=ALU.add), reads=[cs, sv], writes=[cs])
            k.op("dve", lambda e: e.tensor_tensor(out=cs[:], in0=cs[:], in1=mask[:], op=ALU.mult), reads=[cs, mask], writes=[cs])
            k.op("dve", lambda e: e.tensor_scalar(out=cs[:], in0=cs[:], scalar1=INV, scalar2=None, op0=ALU.add), reads=[cs], writes=[cs])
            si_ = k.sbuf("rt_si", [128, Tseg], I32); s2 = k.sbuf("rt_s2", [128, Tseg], I32)
            k.op("dve", lambda e: e.tensor_copy(out=si_[:], in_=cs[:]), reads=[cs], writes=[si_])
            k.op("dve", lambda e: e.tensor_scalar(out=s2[:], in0=si_[:], scalar1=5, scalar2=None, op0=ALU.arith_shift_right), reads=[si_], writes=[s2])
            k.op("dve", lambda e: e.tensor_copy(out=mask[:], in_=s2[:]), reads=[s2], writes=[mask])
            k.dma("sp", seg(S.slotD), mask[:], reads=[mask], writes=[S.slotD])
            s3 = k.sbuf("rt_s3", [128, Tseg], I32)
            k.op("dve", lambda e: e.tensor_scalar(out=s3[:], in0=si_[:], scalar1=31, scalar2=None, op0=ALU.bitwise_and), reads=[si_], writes=[s3])
            k.op("dve", lambda e: e.tensor_copy(out=junk[:], in_=s3[:]), reads=[s3], writes=[junk])
            k.dma("sp", seg(S.slot2D), junk[:], reads=[junk], writes=[S.slot2D])
        with k.phase():
            identf, = load_ident(False, True)
            iota = k.sbuf("rt_iota", [128, NE, 32], F32)
            k.dma("sp", iota[:], r_iota.ap.ap(), reads=[r_iota], writes=[iota])
            tid = k.sbuf("rt_tid", [128, NCH], F32)
            k.dma("sp", tid[:], S.tid.ap.ap(), reads=[S.tid], writes=[tid])
            rows3 = []
            for nm, src in (("hi", S.slotD), ("lo", S.slot2D), ("af", S.affD)):
                t = k.sbuf("rt_" + nm, [NE, nn], F32)
                k.dma("sp", t[:], src.ap.ap(), reads=[src], writes=[t])
                rows3.append(t)
            pT = Pool(k, "rt_pT", 3, [128, 3, NE], F32, "psum")
            tmp_ = Pool(k, "rt_tm", 3, [128, 3, NE], F32)
            Ap = Pool(k, "rt_Ap", 3, [128, NE, 32], F32)
            Bp = Pool(k, "rt_Bp", 3, [128, NE, 32], F32)
            AGp = Pool(k, "rt_AG", 3, [128, NE, 2, 32], F32)
            accp = Pool(k, "rt_acc", 2, [64, NE, 32], F32, "psum")
            res = k.sbuf("rt_res", [64, NE, 32], F32)
            k.op("dve", lambda e: e.memset(res[:], 0.0), writes=[res])
            for c in range(NCH):
                cs_ = slice(c * 128, (c + 1) * 128)
                pt = pT.next(); tm = tmp_.next(); Aa = Ap.next(); Bb = Bp.next(); AG = AGp.next()
                for i_ in range(3):
                    k.op("pe", lambda e, i_=i_, pt=pt, cs_=cs_: e.transpose(out=pt[:, i_, :], in_=rows3[i_][:, cs_], identity=identf[0:NE, 0:NE]), reads=[rows3[i_], identf], writes=[pt], signal=(i_ == 2))
                k.op("act", lambda e, pt=pt, tm=tm: e.activation(out=tm[:], in_=pt[:], func=AF.Copy), reads=[pt], writes=[tm])
                b0, b1, b2 = [tm[:, i_, :].unsqueeze(2).to_broadcast([128, NE, 32]) for i_ in range(3)]
                k.op("dve", lambda e, Aa=Aa, b0=b0: e.tensor_tensor(out=Aa[:], in0=iota[:], in1=b0, op=ALU.is_equal), reads=[iota, tm], writes=[Aa])
                k.op("dve", lambda e, Bb=Bb, b1=b1: e.tensor_tensor(out=Bb[:], in0=iota[:], in1=b1, op=ALU.is_equal), reads=[iota, tm], writes=[Bb])
                k.op("dve", lambda e, Aa=Aa, AG=AG, b2=b2: e.tensor_tensor(out=AG[:, :, 1, :], in0=Aa[:], in1=b2, op=ALU.mult), reads=[Aa, tm], writes=[AG])
                k.op("act", lambda e, Aa=Aa, AG=AG, c=c: e.activation(out=AG[:, :, 0, :], in_=Aa[:], func=AF.Copy, scale=tid[:, c:c + 1]), reads=[Aa, tid], writes=[AG])
                acc = accp.next()
                for e_ in range(NE):
                    mmul(acc[:, e_, :], AG[:, e_, :, :], Bb[:, e_, :], True, True, [AG, Bb], [acc])
                k.op("dve", lambda e, acc=acc: e.tensor_tensor(out=res[:], in0=acc[:], in1=res[:], op=ALU.add), reads=[acc, res], writes=[res])
            resi = k.sbuf("rt_resi", [32, NE, 32], I32)
            k.op("dve", lambda e: e.tensor_copy(out=resi[:], in_=res[0:32]), reads=[res], writes=[resi])
            na = max(1, cap_ // 32)
            k.dma("sp", S.idxD.ap.ap().rearrange("e (a b) -> a e b", b=32), resi[0:na], reads=[resi], writes=[S.idxD])
            k.dma("sp", S.gateD.ap.ap().rearrange("e (a b) -> a e b", b=32), res[32:32 + na], reads=[res], writes=[S.gateD])

    def experts_phase(l, streams):
        with k.phase():
            identb, = load_ident(True, False)
            gtb = {}
            for S in streams:
                t = k.sbuf("ex_gt", [128, D], F32)
                bc_load(t, modD[l], modD[l].ap[S.si:S.si + 1, 5 * D:6 * D], D)
                gtb[S.si] = t
            wgp = Pool(k, "ex_wg", 2, [128, 8, D], BF16)
            wup = Pool(k, "ex_wu", 2, [128, 8, D], BF16)
            wdp = Pool(k, "ex_wd", 2, [128, 8, D], BF16)
            CAPM = max(S.cap for S in streams)
            xeTp = Pool(k, "ex_xeT", 2, [128, 8, CAPM], BF16)
            hT = k.sbuf("ex_hT", [128, 8, CAPM], BF16)
            xep = Pool(k, "ex_xe", 3, [128, D], BF16)
            yp = Pool(k, "ex_y", 3, [128, D], F32)
            sap = Pool(k, "ex_sa", 2, [128, 512], F32)
            ixp = Pool(k, "ex_ix", 3, [128, 8], I32)
            gap = Pool(k, "ex_ga", 3, [128, 8], F32)
            psT = Pool(k, "ex_psT", 1, [128, 8, 128], BF16, "psum")
            psA = Pool(k, "ex_psA", 2, [128, 512], F32, "psum")
            psU = Pool(k, "ex_psU", 2, [128, 512], F32, "psum")
            psY = Pool(k, "ex_psY", 3, [128, 512], F32, "psum")
            items = [(e_, S) for e_ in range(NE) for S in streams]
            wts = {}

            def load_w(e_):
                wg = wgp.next(); wu = wup.next(); wd = wdp.next()
                for wt, src in ((wg, w_gate), (wu, w_up), (wd, w_down)):
                    k.dma("pool", wt[:], src.ap[l, e_].rearrange("(c p) f -> p c f", p=128), reads=[src], writes=[wt])
                wts[e_] = (wg, wu, wd)

            def stage_g(e_, S):
                cap_ = S.cap
                P_ = min(128, cap_)
                ntl = max(1, cap_ // 128)
                ix = ixp.next(); ga = gap.next(); xeT = xeTp.next()
                k.dma("sp", ix[:P_, :ntl], S.idxD.ap[e_, :].rearrange("(p t) -> p t", t=ntl), reads=[S.idxD], writes=[ix])
                k.dma("sp", ga[:P_, :ntl], S.gateD.ap[e_, :].rearrange("(p t) -> p t", t=ntl), reads=[S.gateD], writes=[ga])
                k.op("dve", lambda e, ix=ix, P_=P_, ntl=ntl, hi_=S.n - 1: e.tensor_scalar(out=ix[:P_, :ntl], in0=ix[:P_, :ntl], scalar1=hi_, scalar2=0, op0=ALU.min, op1=ALU.max), reads=[ix], writes=[ix])
                for t_ in range(ntl):
                    xe = xep.next()
                    k.dma_raw("pool", lambda e, xe=xe, ix=ix, t_=t_, S=S, P_=P_: e.indirect_dma_start(out=xe[:P_, :], out_offset=None, in_=S.h2D.ap.ap(), in_offset=bass.IndirectOffsetOnAxis(ap=ix[:P_, t_:t_ + 1], axis=0)), reads=[S.h2D, ix], writes=[xe])
                    pt = psT.next()
                    for c in range(8):
                        k.op("pe", lambda e, c=c, pt=pt, xe=xe, P_=P_: e.transpose(out=pt[:, c, :P_], in_=xe[:P_, c * 128:(c + 1) * 128], identity=identb[:P_, :P_]), reads=[xe, identb], writes=[pt], signal=(c == 7))
                    k.op("act", lambda e, pt=pt, t_=t_, P_=P_, xeT=xeT: e.activation(out=xeT[:, :, t_ * 128:t_ * 128 + P_], in_=pt[:, :, :P_], func=AF.Copy), reads=[pt], writes=[xeT])
                return (ix, ga, xeT)

            def stage_u(e_, S, ctx_):
                ix, ga, xeT = ctx_
                wg, wu, wd = wts[e_]
                cap_ = S.cap
                NB = min(512, cap_)
                for fc in range(8):
                    fs = slice(fc * 128, (fc + 1) * 128)
                    for nb in range(cap_ // NB):
                        ns = slice(nb * NB, (nb + 1) * NB)
                        pa = psA.next(); pu = psU.next(); sa = sap.next()
                        for c in range(8):
                            mmul(pa[:, :NB], wg[:, c, fs], xeT[:, c, ns], c == 0, c == 7, [wg, xeT], [pa])
                        for c in range(8):
                            mmul(pu[:, :NB], wu[:, c, fs], xeT[:, c, ns], c == 0, c == 7, [wu, xeT], [pu])
                        k.op("act", lambda e, sa=sa, pa=pa, NB=NB: e.activation(out=sa[:, :NB], in_=pa[:, :NB], func=AF.Silu), reads=[pa], writes=[sa])
                        k.op("dve", lambda e, sa=sa, pu=pu, fc=fc, ns=ns, NB=NB: e.tensor_tensor(out=hT[:, fc, ns], in0=pu[:, :NB], in1=sa[:, :NB], op=ALU.mult), reads=[pu, sa], writes=[hT])

            def stage_d(e_, S, ctx_):
                ix, ga, xeT = ctx_
                wg, wu, wd = wts[e_]
                cap_ = S.cap
                P_ = min(128, cap_)
                ntl = max(1, cap_ // 128)
                Xn = S.Xnext
                for t_ in range(ntl):
                    y = yp.next()
                    for half in range(2):
                        hs = slice(half * 512, (half + 1) * 512)
                        py = psY.next()
                        for fc in range(8):
                            mmul(py[:P_, :], hT[:, fc, t_ * 128:t_ * 128 + P_], wd[:, fc, hs], fc == 0, fc == 7, [hT, wd], [py])
                        k.op("dve", lambda e, y=y, py=py, ga=ga, t_=t_, hs=hs, S=S, P_=P_: e.scalar_tensor_tensor(out=y[:P_, hs], in0=py[:P_, :], scalar=ga[:P_, t_:t_ + 1], in1=gtb[S.si][:P_, hs], op0=ALU.mult, op1=ALU.mult), reads=[py, ga, gtb[S.si]], writes=[y])
                    k.dma_raw("pool", lambda e, y=y, ix=ix, t_=t_, Xn=Xn, P_=P_: e.indirect_dma_start(out=Xn.ap.ap(), out_offset=bass.IndirectOffsetOnAxis(ap=ix[:P_, t_:t_ + 1], axis=0), in_=y[:P_, :], in_offset=None, compute_op=ALU.add), reads=[y, ix], writes=[Xn], acc_w=(t_ > 0))
                    if t_ == 0:
                        Xn.wb = []

            load_w(0)
            ctxs = {0: stage_g(*items[0])}
            for i_, (e_, S) in enumerate(items):
                if S is streams[0] and e_ + 1 < NE:
                    load_w(e_ + 1)
                stage_u(e_, S, ctxs[i_])
                if i_ + 1 < len(items):
                    ctxs[i_ + 1] = stage_g(*items[i_ + 1])
                stage_d(e_, S, ctxs[i_])

    PH = set(dbg_phases) if dbg_phases is not None else None
    def on(name):
        return PH is None or name in PH
    SX.Xcur, SC.Xcur = x_in, ctx_in
    for l in range(L):
        last = l == L - 1
        for S_ in (SX, SC):
            S_.Xin = S_.Xcur
            S_.Xnext = S_.XA if S_.Xcur is not S_.XA else S_.XB
        if on("mod"):
            mod_phase(l)
        if on("norm1"):
            norm_phase(SX, l, 1); norm_phase(SC, l, 1)
        if on("inproj"):
            inproj_phase([(SX, False), (SC, last)], l)
        if on("gmlp"):
            gmlp_phase(SX, l)
            if not last:
                gmlp_phase(SC, l)
        if on("conv"):
            conv_phase(SX, l)
            if not last:
                conv_phase(SC, l)
        if on("fourier"):
            fourier_phase(SX, l)
            if not last:
                fourier_phase(SC, l)
        if on("attn"):
            attention_phase(SX, SC, l)
            if not last:
                attention_phase(SC, SC, l)
        if on("merge"):
            merge_phase(SX, l, SX.Xcur, SX.Xnext)
            if not last:
                merge_phase(SC, l, SC.Xcur, SC.Xnext)
        if on("ffn"):
            strs = [SX] if last else [SX, SC]
            for S_ in strs:
                S_.Xin = S_.Xnext
                norm_phase(S_, l, 2)
                route_phase(S_, l)
            if on("experts"):
                experts_phase(l, strs)
        for S_ in (SX, SC):
            S_.Xcur = S_.Xnext
        if PH is not None:
            break
    SX.Xin = SX.Xcur if PH is None else x_in
    norm_phase(SX, 0, 3)
    k.finish([OUT])
    k.emit()
    st.close()
    return nc, list(ins.keys())


def _prep_inputs(inp, n, L=2):
    f = lambda a: np.ascontiguousarray(np.asarray(a, dtype=np.float32))
    ulist, qtiles = _attn_geometry(n)
    cs, f1x, f2, twx = _dft_consts(n)
    _, f1c, _, twc = _dft_consts(NCTX)
    blk, low, iota = _route_consts()
    w_in = f(inp["w_in"])
    i = np.arange(256)
    perm = np.where((i % 32) < 16, i + 16, i - 16)
    w_qkp = np.concatenate([w_in[:, :, C_END:Q_END][:, :, perm], w_in[:, :, Q_END:K_END][:, :, perm]], axis=2)
    pp = np.arange(128)
    common = {
        "w_mod": f(inp["w_mod"]), "b_mod": f(inp["b_mod"]), "norm1_g": f(inp["norm1_g"]), "norm2_g": f(inp["norm2_g"]),
        "final_norm_g": f(inp["final_norm_g"]).reshape(1, D), "w_in": w_in, "w_qkp": np.ascontiguousarray(w_qkp),
        "sgu_ln_g": f(inp["sgu_ln_g"]), "sgu_ln_b": f(inp["sgu_ln_b"]),
        "wsT": np.ascontiguousarray(f(inp["w_spatial"]).transpose(0, 3, 1, 2)),
        "b_spatial": f(inp["b_spatial"]),
        "w_a_out": f(inp["w_a_out"]), "w_b_out": f(inp["w_b_out"]), "w_c_out": f(inp["w_c_out"]), "w_d_out": f(inp["w_d_out"]),
        "conv_wT": np.ascontiguousarray(f(inp["conv_w"]).transpose(0, 2, 1)), "conv_b_c": f(inp["conv_b"])[:, :, None].copy(),
        "conv_ln_g_c": f(inp["conv_ln_g"])[:, :, None].copy(), "conv_ln_b_c": f(inp["conv_ln_b"])[:, :, None].copy(),
        "w_out": f(inp["w_out"]), "w_router": f(inp["w_router"]),
        "w_gate_e": f(inp["w_gate_e"]), "w_up_e": f(inp["w_up_e"]), "w_down_e": f(inp["w_down_e"]),
        "id_bf": np.eye(128).astype(bf16_np), "id_f": np.eye(128, dtype=np.float32),
        "rope": _rope_tables(n),
        "biasT": np.stack([_bias_tiles(f(inp["rpb"])[l], ulist) for l in range(L)]),
        "dft_cs": cs, "dft_f1x": f1x, "dft_f1c": f1c, "dft_f2": f2, "dft_twx": twx, "dft_twc": twc,
        "r_blk": blk, "r_low": low, "r_iota": iota,
        "tid_x": (np.arange(n // 128)[None, :] * 128 + pp[:, None]).astype(np.float32),
        "tid_c": (np.arange(2)[None, :] * 128 + pp[:, None]).astype(np.float32),
        "cctx_pk": np.ascontiguousarray(f(inp["c_ctx"]).reshape(8, 128).T),
    }
    maps = []
    B = inp["x"].shape[0]
    for b in range(B):
        m = dict(common)
        m["x"] = f(inp["x"][b]); m["ctx"] = f(inp["ctx"][b])
        m["c_pk"] = np.ascontiguousarray(f(inp["c"][b]).reshape(8, 128).T)
        maps.append(m)
    return maps


_CACHE = {}


def kernel(**inputs):
    n = inputs["x"].shape[1]
    B = inputs["x"].shape[0]
    if n not in _CACHE:
        _CACHE[n] = build_program(n)
    nc, names = _CACHE[n]
    maps = _prep_inputs(inputs, n)
    maps = [{kk: m[kk] for kk in names} for m in maps]
    res = run_bass_kernel_spmd(nc, maps, core_ids=list(range(B)))
    return np.stack([np.asarray(r["out"], dtype=np.float32) for r in res.results], axis=0)
```
